# Optimizing a Trainium2 kernel written in Bass

```python
import math
import jax, jax.numpy as jnp
from jax import lax
import numpy as np

D_MODEL = 1024
BATCH = 4
SEQ = 4096
DEPTH = 1

N_MEM = 256
D_MIX = D_MODEL
FOX_HEADS = 8
FOX_HEAD_DIM = 64
FOX_WIDTH = FOX_HEADS * FOX_HEAD_DIM
QUERY_BLOCK = 128
FOX_FORGET_BIAS = 3.0
MLSTM_HEADS = 4
MLSTM_QK_DIM = 64
MLSTM_V_DIM = 128
MLSTM_QK_WIDTH = MLSTM_HEADS * MLSTM_QK_DIM
MLSTM_V_WIDTH = MLSTM_HEADS * MLSTM_V_DIM
MLSTM_CHUNK = 64
MLSTM_FORGET_BIAS = 3.0
CONV_WIDTH = 4
IN_WIDTHS = (FOX_WIDTH, FOX_WIDTH, FOX_WIDTH, FOX_HEADS,
             2 * MLSTM_QK_WIDTH, MLSTM_V_WIDTH, MLSTM_HEADS, MLSTM_HEADS, MLSTM_V_WIDTH)
IN_WIDTH = 3088
XATTN_HEADS = 4
XATTN_HEAD_DIM = D_MODEL // XATTN_HEADS
N_EXPERTS = 32
TOP_K = 4
D_EXPERT = D_MODEL
SWIGLU_LIMIT = 7.0
SWIGLU_ALPHA = 1.702
EXPERT_BLOCK = 256
DEEPNORM_ALPHA = (2 * DEPTH) ** 0.25
DEEPNORM_BETA = (8 * DEPTH) ** -0.25
LN_EPS = 1e-5
RMS_EPS = 1e-6

kernel_name = "fox_mlstm_hymba_deepnorm_moe"


def layer_norm(x, g, b):
    xf = x.astype(jnp.float32)
    mu = jnp.mean(xf, -1, keepdims=True)
    xc = xf - mu
    var = jnp.mean(xc * xc, -1, keepdims=True)
    return (xc * lax.rsqrt(var + LN_EPS) * g + b).astype(x.dtype)


def head_rmsnorm(h, gain):
    B, S = h.shape[0], h.shape[1]
    hf = h.astype(jnp.float32)
    hf = hf * lax.rsqrt(jnp.mean(hf * hf, -1, keepdims=True) + RMS_EPS)
    return hf.reshape(B, S, -1) * gain


def causal_conv(x, w):
    S = x.shape[1]
    xp = jnp.pad(x, ((0, 0), (CONV_WIDTH - 1, 0), (0, 0)))
    out = xp[:, 0:S] * w[0]
    for j in range(1, CONV_WIDTH):
        out = out + xp[:, j:j + S] * w[j]
    return out


def fox_attention(q, k, v, log_f):
    S = q.shape[2]
    c = jnp.cumsum(log_f, axis=-1)
    scale = FOX_HEAD_DIM ** -0.5
    outs = []
    for blk in range(S // QUERY_BLOCK):
        q0 = blk * QUERY_BLOCK
        q1 = q0 + QUERY_BLOCK
        s = jnp.einsum('bhqd,bhkd->bhqk', q[:, :, q0:q1], k[:, :, :q1]).astype(jnp.float32) * scale
        s = s + c[:, :, q0:q1, None] - c[:, :, None, :q1]
        causal = (q0 + jnp.arange(QUERY_BLOCK))[:, None] >= jnp.arange(q1)[None, :]
        p = jax.nn.softmax(jnp.where(causal, s, -jnp.inf), axis=-1)
        outs.append(jnp.einsum('bhqk,bhkd->bhqd', p.astype(v.dtype), v[:, :, :q1]))
    return jnp.concatenate(outs, axis=2)


def mlstm_chunkwise(q, k, v, i_pre, f_pre):
    B, H, S, Dk = q.shape
    Dv = v.shape[-1]
    L = MLSTM_CHUNK
    NC = S // L
    f32 = jnp.float32
    q = q.astype(f32).reshape(B, H, NC, L, Dk)
    k = (k.astype(f32) * Dk ** -0.5).reshape(B, H, NC, L, Dk)
    v = v.astype(f32).reshape(B, H, NC, L, Dv)
    log_i = i_pre.astype(f32).reshape(B, H, NC, L)
    log_f = jax.nn.log_sigmoid(f_pre.astype(f32)).reshape(B, H, NC, L)
    b = jnp.cumsum(log_f, axis=-1)
    g = b[..., -1]
    tri = jnp.tril(jnp.ones((L, L), dtype=bool))
    d = jnp.where(tri, b[..., :, None] - b[..., None, :] + log_i[..., None, :], -jnp.inf)
    a = g[..., None] - b + log_i
    m_loc = jnp.max(a, axis=-1)
    wa = jnp.exp(a - m_loc[..., None])
    kv_chunk = jnp.einsum('bhcl,bhcld,bhcle->bhcde', wa, k, v)
    n_chunk = jnp.einsum('bhcl,bhcld->bhcd', wa, k)

    def step(carry, inp):
        C, n, m = carry
        kv_c, n_c, g_c, m_c = inp
        m_new = jnp.maximum(g_c + m, m_c)
        decay = jnp.exp(g_c + m - m_new)
        scale_in = jnp.exp(m_c - m_new)
        C_new = decay[..., None, None] * C + scale_in[..., None, None] * kv_c
        n_new = decay[..., None] * n + scale_in[..., None] * n_c
        return (C_new, n_new, m_new), (C, n, m)

    init = (jnp.zeros((B, H, Dk, Dv), f32), jnp.zeros((B, H, Dk), f32), jnp.zeros((B, H), f32))
    xs = (jnp.moveaxis(kv_chunk, 2, 0), jnp.moveaxis(n_chunk, 2, 0),
          jnp.moveaxis(g, 2, 0), jnp.moveaxis(m_loc, 2, 0))
    _, (C_prev, n_prev, m_prev) = lax.scan(step, init, xs)
    C_prev = jnp.moveaxis(C_prev, 0, 2)
    n_prev = jnp.moveaxis(n_prev, 0, 2)
    m_prev = jnp.moveaxis(m_prev, 0, 2)

    inter_log = b + m_prev[..., None]
    m_t = jnp.maximum(inter_log, jnp.max(d, axis=-1))
    w_inter = jnp.exp(inter_log - m_t)
    p = jnp.exp(d - m_t[..., None]) * jnp.einsum('bhcld,bhcsd->bhcls', q, k)
    num = (w_inter[..., None] * jnp.einsum('bhcld,bhcde->bhcle', q, C_prev)
           + jnp.einsum('bhcls,bhcse->bhcle', p, v))
    den = w_inter * jnp.einsum('bhcld,bhcd->bhcl', q, n_prev) + jnp.sum(p, axis=-1)
    h = num / jnp.maximum(jnp.abs(den), jnp.exp(-m_t))[..., None]
    return h.reshape(B, H, S, Dv)


def hybrid_mixer(x, w_in, fox_f_bias, conv_w, i_bias, f_bias, fox_g, mlstm_g, w_out):
    B, S, _ = x.shape
    u = x @ w_in
    points = np.cumsum(IN_WIDTHS)[:-1].tolist()
    fq, fk, fv, ff, mqk, mv, mi, mf, mo = jnp.split(u, points, axis=-1)

    def heads(t, h):
        return t.reshape(B, S, h, -1).transpose(0, 2, 1, 3)

    log_f = jax.nn.log_sigmoid((ff + fox_f_bias).astype(jnp.float32)).transpose(0, 2, 1)
    fo = fox_attention(heads(fq, FOX_HEADS), heads(fk, FOX_HEADS), heads(fv, FOX_HEADS), log_f)
    fo = head_rmsnorm(fo.transpose(0, 2, 1, 3), fox_g)

    mqk = jax.nn.silu(causal_conv(mqk, conv_w))
    mq, mk = jnp.split(mqk, [MLSTM_QK_WIDTH], axis=-1)
    h = mlstm_chunkwise(heads(mq, MLSTM_HEADS), heads(mk, MLSTM_HEADS), heads(mv, MLSTM_HEADS),
                        (mi + i_bias).transpose(0, 2, 1), (mf + f_bias).transpose(0, 2, 1))
    mo_out = head_rmsnorm(h.transpose(0, 2, 1, 3), mlstm_g) * jax.nn.sigmoid(mo.astype(jnp.float32))

    mixed = jnp.concatenate([fo, mo_out], axis=-1).astype(x.dtype)
    return mixed @ w_out


def memory_cross_attention(x, mem, w_q, w_k, w_v, w_o):
    B, S, D = x.shape
    M = mem.shape[1]
    q = (x @ w_q).reshape(B, S, XATTN_HEADS, XATTN_HEAD_DIM)
    k = (mem @ w_k).reshape(B, M, XATTN_HEADS, XATTN_HEAD_DIM)
    v = (mem @ w_v).reshape(B, M, XATTN_HEADS, XATTN_HEAD_DIM)
    s = jnp.einsum('bqhd,bkhd->bhqk', q, k).astype(jnp.float32) * XATTN_HEAD_DIM ** -0.5
    p = jax.nn.softmax(s, axis=-1)
    o = jnp.einsum('bhqk,bkhd->bqhd', p.astype(v.dtype), v).reshape(B, S, D)
    return o @ w_o


def clamped_swiglu(h):
    gate, lin = h[..., :D_EXPERT], h[..., D_EXPERT:]
    gate = jnp.minimum(gate, SWIGLU_LIMIT)
    lin = jnp.clip(lin, -SWIGLU_LIMIT, SWIGLU_LIMIT)
    return gate * jax.nn.sigmoid(SWIGLU_ALPHA * gate) * (lin + 1)


def moe_ffn(x, w_router, b_router, w_gu, b_gu, w_down, b_down):
    B, S, D = x.shape
    N = B * S
    G = EXPERT_BLOCK
    xf = x.reshape(N, D)
    logits = (xf @ w_router + b_router).astype(jnp.float32)
    top_vals, top_idx = lax.top_k(logits, TOP_K)
    gates = jax.nn.softmax(top_vals, axis=-1)
    e_flat = top_idx.reshape(-1).astype(jnp.int32)
    tok_flat = jnp.repeat(jnp.arange(N, dtype=jnp.int32), TOP_K)
    g_flat = gates.reshape(-1)
    order = jnp.argsort(e_flat)
    e_sorted = e_flat[order]
    counts = jnp.bincount(e_flat, length=N_EXPERTS).astype(jnp.int32)
    padded = ((counts + G - 1) // G) * G
    start = jnp.cumsum(counts) - counts
    pad_end = jnp.cumsum(padded)
    pad_start = pad_end - padded
    rank = jnp.arange(N * TOP_K, dtype=jnp.int32) - start[e_sorted]
    dest = pad_start[e_sorted] + rank
    P = N * TOP_K + N_EXPERTS * G
    NB = P // G
    row_tok = jnp.full((P,), N, jnp.int32).at[dest].set(tok_flat[order])
    row_gate = jnp.zeros((P,), jnp.float32).at[dest].set(g_flat[order])
    blk_e = jnp.minimum(jnp.searchsorted(pad_end, jnp.arange(NB, dtype=jnp.int32) * G, side='right'),
                        N_EXPERTS - 1)
    x_pad = jnp.concatenate([xf, jnp.zeros((1, D), xf.dtype)], axis=0)
    xb = x_pad[row_tok].reshape(NB, G, D)

    def expert_block(args):
        xblk, e = args
        hid = clamped_swiglu(xblk @ w_gu[e] + b_gu[e])
        return hid @ w_down[e] + b_down[e]

    yb = lax.map(expert_block, (xb, blk_e)).reshape(P, D)
    y = jnp.zeros((N + 1, D), yb.dtype).at[row_tok].add(yb * row_gate[:, None].astype(yb.dtype))
    return y[:N].reshape(B, S, D)


def setup_inputs(seed: int = 0) -> dict:
    key = jax.random.key(seed)
    ks = iter(jax.random.split(key, 32))
    L, D, E, F = DEPTH, D_MODEL, N_EXPERTS, D_EXPERT

    def nrm(shape, scale):
        return jax.random.normal(next(ks), shape, jnp.float32) * scale

    return {
        "x": nrm((BATCH, SEQ, D), 1.0),
        "mem": nrm((BATCH, N_MEM, D), 1.0),
        "w_in": nrm((L, D, IN_WIDTH), D ** -0.5),
        "fox_f_bias": FOX_FORGET_BIAS + nrm((L, FOX_HEADS), 0.1),
        "mlstm_conv_w": nrm((L, CONV_WIDTH, 2 * MLSTM_QK_WIDTH), CONV_WIDTH ** -0.5),
        "mlstm_i_bias": nrm((L, MLSTM_HEADS), 0.1),
        "mlstm_f_bias": MLSTM_FORGET_BIAS + nrm((L, MLSTM_HEADS), 0.1),
        "fox_norm_g": 1.0 + nrm((L, FOX_WIDTH), 0.02),
        "mlstm_norm_g": 1.0 + nrm((L, MLSTM_V_WIDTH), 0.02),
        "w_mix_out": nrm((L, D_MIX, D), D_MIX ** -0.5 * DEEPNORM_BETA),
        "ln1_g": 1.0 + nrm((L, D), 0.02),
        "ln1_b": nrm((L, D), 0.02),
        "w_xq": nrm((L, D, D), D ** -0.5),
        "w_xk": nrm((L, D, D), D ** -0.5),
        "w_xv": nrm((L, D, D), D ** -0.5),
        "w_xo": nrm((L, D, D), D ** -0.5 * DEEPNORM_BETA),
        "ln2_g": 1.0 + nrm((L, D), 0.02),
        "ln2_b": nrm((L, D), 0.02),
        "w_router": nrm((L, D, E), D ** -0.5),
        "b_router": nrm((L, E), 0.01),
        "w_gate_up": nrm((L, E, D, 2 * F), D ** -0.5),
        "b_gate_up": nrm((L, E, 2 * F), 0.02),
        "w_down": nrm((L, E, F, D), F ** -0.5 * DEEPNORM_BETA),
        "b_down": nrm((L, E, D), 0.02),
        "ln3_g": 1.0 + nrm((L, D), 0.02),
        "ln3_b": nrm((L, D), 0.02),
    }


def reference(x, mem, w_in, fox_f_bias, mlstm_conv_w, mlstm_i_bias, mlstm_f_bias, fox_norm_g,
              mlstm_norm_g, w_mix_out, ln1_g, ln1_b, w_xq, w_xk, w_xv, w_xo, ln2_g, ln2_b,
              w_router, b_router, w_gate_up, b_gate_up, w_down, b_down, ln3_g, ln3_b):
    for l in range(DEPTH):
        mix = hybrid_mixer(x, w_in[l], fox_f_bias[l], mlstm_conv_w[l], mlstm_i_bias[l],
                           mlstm_f_bias[l], fox_norm_g[l], mlstm_norm_g[l], w_mix_out[l])
        x = layer_norm(DEEPNORM_ALPHA * x + mix.astype(x.dtype), ln1_g[l], ln1_b[l])
        xa = memory_cross_attention(x, mem, w_xq[l], w_xk[l], w_xv[l], w_xo[l])
        x = layer_norm(DEEPNORM_ALPHA * x + xa.astype(x.dtype), ln2_g[l], ln2_b[l])
        ff = moe_ffn(x, w_router[l], b_router[l], w_gate_up[l], b_gate_up[l], w_down[l], b_down[l])
        x = layer_norm(DEEPNORM_ALPHA * x + ff.astype(x.dtype), ln3_g[l], ln3_b[l])
    return x
```

```python
import numpy as np
from contextlib import ExitStack
import concourse.bass as bass
import concourse.mybir as mybir
from concourse.bass_utils import run_bass_kernel_spmd

F32 = mybir.dt.float32
BF16 = mybir.dt.bfloat16
I32 = mybir.dt.int32
AF = mybir.ActivationFunctionType
ALU = mybir.AluOpType
AX = mybir.AxisListType
ESZ = {F32: 4, BF16: 2, I32: 4}

D = 1024
NTOK = 2048
NALL = 4096
NE = 32
CAP = 384
ALPHA = 2.0 ** 0.25
NEG = -30000.0
PAGE = 2048
_DBG = {}


class V:
    def __init__(self, ap, key, lo, hi, es):
        self.ap, self.key, self.lo, self.hi, self.es = ap, key, lo, hi, es

    def __getitem__(self, sl):
        a, b = sl.start or 0, sl.stop
        return V(self.ap[:, a:b], self.key, self.lo + a * self.es, self.lo + b * self.es, self.es)

    def p(self, p0, p1):
        return V(self.ap[p0:p1], self.key, self.lo, self.hi, self.es)

    def re(self, pat, **kw):
        return V(self.ap.rearrange(pat, **kw), self.key, self.lo, self.hi, self.es)

    def w(self, ap):
        return V(ap, self.key, self.lo, self.hi, self.es)


class Op:
    __slots__ = ("eng", "fn", "deps", "dma", "inc", "val", "sem", "idx", "prewait")


class Prog:
    ENGS = ["pe", "dve", "act", "pool", "sp"]

    def __init__(self, nc, sbuf_bytes, dma_pool=20):
        self.nc = nc
        self.ops = []
        self.recs = {}
        self.sb_off = 0
        self.sbuf_bytes = sbuf_bytes
        self.dma_pool = dma_pool
        self.bank = 0

    def setup(self, stack):
        nc = self.nc
        self.sb = stack.enter_context(nc.sbuf_tensor("sb_all", [128, self.sbuf_bytes // 4], F32))
        self.ps = stack.enter_context(nc.psum_tensor("ps_all", [128, 4096], F32))

    def alloc(self, ncols, dt=F32):
        nbytes = ((ncols * ESZ[dt] + 63) // 64) * 64
        off = self.sb_off
        self.sb_off += nbytes
        assert self.sb_off <= self.sbuf_bytes, f"SBUF overflow {self.sb_off}"
        ap = self.sb[:, off // 4:(off + nbytes) // 4]
        if dt != F32:
            ap = ap.bitcast(dt)
        ap = ap[:, 0:ncols]
        return V(ap, "S", off, off + ncols * ESZ[dt], ESZ[dt])

    def mark(self):
        return self.sb_off

    def release(self, m):
        self.sb_off = m

    def psum(self, bank, col0=0, ncols=512, dt=F32):
        off = bank * 2048 + col0 * ESZ[dt]
        nb = ncols * ESZ[dt]
        assert col0 * ESZ[dt] + nb <= 2048
        ap = self.ps[:, bank * 512:(bank + 1) * 512]
        if dt != F32:
            ap = ap.bitcast(dt)
        ap = ap[:, col0:col0 + ncols]
        return V(ap, "P", off, off + nb, ESZ[dt])

    nbanks = 8

    def nb(self):
        b = self.bank % self.nbanks
        self.bank = (b + 1) % self.nbanks
        return b

    def _deps(self, idx, eng, isdma, reads, writes):
        deps = set()
        ops = self.ops
        for (key, lo, hi) in reads:
            for pg in range(lo // PAGE, (hi - 1) // PAGE + 1):
                a, b = max(lo, pg * PAGE), min(hi, (pg + 1) * PAGE)
                lst = self.recs.setdefault((key, pg), [])
                found = False
                for r in lst:
                    if r[2] == "w":
                        if r[0] < b and a < r[1]:
                            deps.add(r[3])
                    elif (not found) and r[0] == a and r[1] == b and not isdma:
                        od = ops[r[3]]
                        if od.eng == eng and not od.dma:
                            r[3] = idx
                            found = True
                if not found:
                    lst.append([a, b, "r", idx])
        for (key, lo, hi) in writes:
            for pg in range(lo // PAGE, (hi - 1) // PAGE + 1):
                a, b = max(lo, pg * PAGE), min(hi, (pg + 1) * PAGE)
                lst = self.recs.get((key, pg), [])
                keep = []
                for r in lst:
                    if r[3] != idx and r[0] < b and a < r[1]:
                        deps.add(r[3])
                        if r[0] >= a and r[1] <= b:
                            continue
                    keep.append(r)
                keep.append([a, b, "w", idx])
                self.recs[(key, pg)] = keep
        return deps

    @staticmethod
    def _reg(x):
        if isinstance(x, V):
            if x.key == "P":
                return (x.key, (x.lo // PAGE) * PAGE, ((x.hi - 1) // PAGE + 1) * PAGE)
            return (x.key, x.lo, x.hi)
        return x

    def op(self, eng, fn, reads=(), writes=(), dma=False):
        o = Op()
        o.eng, o.fn, o.dma, o.inc, o.val, o.sem, o.prewait = eng, fn, dma, False, None, None, None
        o.idx = len(self.ops)
        self.ops.append(o)
        deps = self._deps(o.idx, eng, dma, [self._reg(r) for r in reads], [self._reg(w) for w in writes])
        deps.discard(o.idx)
        fin = set()
        for d in deps:
            od = self.ops[d]
            if od.eng == eng and eng == "pe" and not od.dma and not dma:
                continue
            fin.add(d)
        o.deps = fin
        return o

    def emit(self, stack):
        nc = self.nc
        ops = self.ops
        for o in ops:
            for d in o.deps:
                ops[d].inc = True
        esem = {e: stack.enter_context(nc.semaphore("sem_" + e)) for e in self.ENGS}
        dq = ("sp", "pool", "act")
        dsem = {e: [stack.enter_context(nc.semaphore(f"dsem_{e}_{i}")) for i in range(self.dma_pool)] for e in dq}
        cnt = {e: 0 for e in self.ENGS}
        dcnt = {e: 0 for e in dq}
        for o in ops:
            if o.dma:
                k = dcnt[o.eng]
                dcnt[o.eng] += 1
                s = k % self.dma_pool
                o.sem = dsem[o.eng][s]
                o.val = 16 * (k // self.dma_pool + 1)
                o.prewait = (o.sem, o.val - 16) if o.val > 16 else None
            elif o.inc:
                cnt[o.eng] += 1
                o.sem = esem[o.eng]
                o.val = cnt[o.eng]
        block = stack.enter_context(nc.Block())
        engobj = {"pe": "tensor", "dve": "vector", "act": "scalar", "pool": "gpsimd", "sp": "sync"}

        def make(ename):
            mine = [o for o in ops if o.eng == ename]

            def body(e):
                waited = {}

                def wait(sem, val):
                    k = id(sem)
                    if waited.get(k, 0) >= val:
                        return
                    waited[k] = val
                    e.wait_ge(sem, val)

                for o in mine:
                    if o.prewait is not None:
                        wait(*o.prewait)
                    need = {}
                    for d in o.deps:
                        od = ops[d]
                        k = id(od.sem)
                        if k not in need or need[k][1] < od.val:
                            need[k] = (od.sem, od.val)
                    for sem, val in need.values():
                        wait(sem, val)
                    ins = o.fn(e)
                    if o.dma:
                        ins.then_inc(o.sem, 16)
                    elif o.inc:
                        ins.then_inc(o.sem, 1)
                if ename in dsem:
                    last = {}
                    for o in mine:
                        if o.dma:
                            last[id(o.sem)] = (o.sem, o.val)
                    for sem, val in last.values():
                        wait(sem, val)
            return body

        for ename in self.ENGS:
            getattr(block, engobj[ename])(make(ename))


def build_program(dbg=False, stop=99):
    nc = bass.Bass("TRN2", target_bir_lowering=False)

    state = {"dumped": False}

    def dump_mixed():
        if not dbg or state["dumped"] or "mixed" not in state:
            return
        state["dumped"] = True
        mixed_ = state["mixed"]
        mtmp = P.alloc(1024)
        for qb in range(16):
            P.op("dve", lambda e, qb=qb: e.tensor_copy(out=mtmp.ap, in_=mixed_.ap[:, qb * 1024:(qb + 1) * 1024]),
                 reads=[mixed_[qb * 1024:(qb + 1) * 1024]], writes=[mtmp])
            P.op("sp", lambda e, qb=qb: e.dma_start(out=dbg_mixed[qb * 128:(qb + 1) * 128, :], in_=mtmp.ap), reads=[mtmp], dma=True)

    def finish():
        dump_mixed()
        P.emit(st)
        st.close()
        return nc

    def din(name, shape, dt=F32):
        return nc.dram_tensor(name, list(shape), dt, kind="ExternalInput").ap()

    xcat = din("xcat", [NALL, D])
    memb = din("memb", [256, D])
    kmask_d = din("kmask", [128, 1])
    consts_d = din("consts", [128, 800])
    w_in = din("w_in", [D, 3088])
    gbias_d = din("gbias", [1, 512])
    convT_d = din("convT", [128, 16])
    foxg_d = din("foxg", [1, 512])
    mlg_d = din("mlg", [1, 512])
    w_mix = din("w_mix", [D, D])
    ln_d = din("lnp", [1, 6 * D])
    w_xq = din("w_xq", [D, D])
    w_xk = din("w_xk", [D, D])
    w_xv = din("w_xv", [D, D])
    w_xo = din("w_xo", [D, D])
    w_r = din("w_r", [D, NE])
    b_r = din("b_r", [1, NE])
    NEd = NE if stop >= 5 else 1
    w_gu = din("w_gu", [NEd, D, 2 * D])
    bguT_d = din("bguT", [128, NE * 16])
    w_dn = din("w_dn", [NEd, D, D])
    b_dn = din("b_dn", [NE, D])
    out_d = nc.dram_tensor("out", [NTOK, D], F32, kind="ExternalOutput").ap()
    if dbg:
        dbg_mixed = nc.dram_tensor("dbg_mixed", [NTOK, D], F32, kind="ExternalOutput").ap()
        dbg_x1 = nc.dram_tensor("dbg_x1", [NTOK, D], F32, kind="ExternalOutput").ap()
        dbg_x2 = nc.dram_tensor("dbg_x2", [NTOK, D], F32, kind="ExternalOutput").ap()

    st = ExitStack()
    P = Prog(nc, 200 * 1024)
    P.setup(st)
    Xg = nc.dram_tensor("Xg", [NE * CAP, D], BF16).ap()
    Yg = nc.dram_tensor("Yg", [NE * CAP, D], BF16).ap()
    X2 = nc.dram_tensor("X2s", [NTOK, D], F32).ap()

    def wview(w, c0, c1):
        return w.rearrange("(c p) n -> p c n", p=128)[:, :, c0:c1]

    def mm(ps, lhsT, rhs, start, stop, rd):
        P.op("pe", lambda e: e.matmul(ps.ap, lhsT, rhs, start=start, stop=stop), reads=rd, writes=[ps])

    def tr(ps, in_v, in_ap, ident_v, ident_ap):
        P.op("pe", lambda e: e.transpose(ps.ap, in_ap, ident_ap), reads=[in_v, ident_v], writes=[ps])

    ev_rr = [0]

    def evac(out_v, out_ap, in_v, in_ap, eng=None):
        if eng is None:
            eng = ("dve", "act")[ev_rr[0] % 2]
            ev_rr[0] += 1
        if eng == "act":
            P.op("act", lambda e: e.copy(out=out_ap, in_=in_ap), reads=[in_v], writes=[out_v])
        else:
            P.op(eng, lambda e: e.tensor_copy(out=out_ap, in_=in_ap), reads=[in_v], writes=[out_v])

    def dma(eng, out_ap, in_ap, reads=(), writes=()):
        P.op(eng, lambda e: e.dma_start(out=out_ap, in_=in_ap), reads=reads, writes=writes, dma=True)

    def ts(eng, out, in0, s1, s2, op0, op1=None, rd=(), wr=()):
        if s2 is None:
            P.op(eng, lambda e: e.tensor_scalar(out=out, in0=in0, scalar1=s1, scalar2=None, op0=op0), reads=rd, writes=wr)
        else:
            P.op(eng, lambda e: e.tensor_scalar(out=out, in0=in0, scalar1=s1, scalar2=s2, op0=op0, op1=op1), reads=rd, writes=wr)

    def tt(eng, out, in0, in1, op, rd=(), wr=()):
        P.op(eng, lambda e: e.tensor_tensor(out=out, in0=in0, in1=in1, op=op), reads=rd, writes=wr)

    def stt(eng, out, in0, sc, in1, op0, op1, rd=(), wr=(), acc=None):
        if acc is None:
            P.op(eng, lambda e: e.scalar_tensor_tensor(out=out, in0=in0, scalar=sc, in1=in1, op0=op0, op1=op1), reads=rd, writes=wr)
        else:
            P.op(eng, lambda e: e.scalar_tensor_tensor(out=out, in0=in0, scalar=sc, in1=in1, op0=op0, op1=op1, accum_out=acc),
                 reads=rd, writes=wr)

    def act(out, in_, func, rd=(), wr=(), bias=None, scale=None):
        kw = {}
        if bias is not None:
            kw["bias"] = bias
        if scale is not None:
            kw["scale"] = scale
        P.op("act", lambda e: e.activation(out=out, in_=in_, func=func, **kw), reads=rd, writes=wr)

    cst = P.alloc(800)
    dma("sp", cst.ap, consts_d, writes=[cst])
    ident = cst[0:128]
    Uincl = cst[128:256]
    ones = cst[384:512]
    maskneg = cst[512:640]
    Ult = cst[640:768]
    ebase = cst[768:800]
    identb = P.alloc(128, BF16)
    evac(identb, identb.ap, ident, ident.ap, "dve")
    causb = P.alloc(128, BF16)
    evac(causb, causb.ap, Uincl, Uincl.ap, "dve")
    kmask = P.alloc(1)
    dma("sp", kmask.ap, kmask_d, writes=[kmask])
    mixed = P.alloc(16 * 1024, BF16)
    state["mixed"] = mixed
    P.op("pool", lambda e: e.memset(mixed.ap, 0.0), writes=[mixed])
    junk = P.alloc(128)
    dest_all = P.alloc(64, I32)
    gate_all = P.alloc(64)
    gfull = P.alloc(16 * 32)
    lnp_box = [None, 0]
    m_gates = P.mark()
    gbias = P.alloc(512)
    dma("sp", gbias.ap, gbias_d.partition_broadcast(128), writes=[gbias])
    gpre = P.alloc(512)
    glog = P.alloc(512)
    cs = P.alloc(512)
    tot = P.alloc(512)

    def load_x_block(tb, xs):
        for i in range(4):
            r0 = tb * 512 + i * 128
            dma("sp", xs[i].ap, xcat[r0:r0 + 128, :], writes=[xs[i]])

    def transpose_block(xs, xT):
        for c in range(8):
            b = P.nb()
            for i in range(4):
                ps = P.psum(b, i * 128, 128)
                tr(ps, xs[i][c * 128:(c + 1) * 128], xs[i].ap[:, c * 128:(c + 1) * 128], ident, ident.ap)
            psf = P.psum(b)
            evac(xT[c * 512:(c + 1) * 512], xT.ap[:, c * 512:(c + 1) * 512], psf, psf.ap)

    def rstd_from_ss(ss, n, eps_col, outv):
        ts("dve", outv.ap, ss.ap, 1.0 / n, (1e-5, 1e-6)[eps_col], ALU.mult, ALU.add, rd=[ss], wr=[outv])
        act(outv.ap, outv.ap, AF.Sqrt, rd=[outv], wr=[outv])
        P.op("dve", lambda e: e.reciprocal(out=outv.ap, in_=outv.ap), reads=[outv], writes=[outv])

    def layer_norm(y, gi, outv, tmp):
        lnp, base = lnp_box
        g = lnp[(gi - base) * 1024:(gi - base + 1) * 1024]
        b = lnp[(gi - base + 1) * 1024:(gi - base + 2) * 1024]
        s1 = tmp[0:1]
        nm = tmp[1:2]
        ss = tmp[2:3]
        rs = tmp[3:4]
        P.op("dve", lambda e: e.tensor_reduce(out=s1.ap, in_=y.ap, axis=AX.X, op=ALU.add), reads=[y], writes=[s1])
        ts("dve", nm.ap, s1.ap, -1.0 / D, None, ALU.mult, rd=[s1], wr=[nm])
        act(y.ap, y.ap, AF.Identity, rd=[y, nm], wr=[y], bias=nm.ap)
        P.op("act", lambda e: e.activation(out=outv.ap, in_=y.ap, func=AF.Square, accum_out=ss.ap), reads=[y], writes=[outv, ss])
        rstd_from_ss(ss, D, 0, rs)
        stt("dve", outv.ap, y.ap, rs.ap, g.ap, ALU.mult, ALU.mult, rd=[y, rs, g], wr=[outv])
        tt("dve" if gi == 4 else "pool", outv.ap, outv.ap, b.ap, ALU.add, rd=[outv, b], wr=[outv])

    m_phase1 = P.mark()
    zt = P.alloc(2048, BF16)
    P.op("pool", lambda e: e.memset(zt.ap, 0.0), writes=[zt])
    for r in range(0, NE * CAP, 256):
        P.op("act", lambda e, r=r: e.dma_start(out=Xg[r:r + 256, :].rearrange("(p a) n -> p (a n)", a=2), in_=zt.ap),
             reads=[zt], writes=[("Xg", r, r + 256)], dma=True)

    KT = P.alloc(4 * NALL, BF16)
    QT = P.alloc(4 * NTOK, BF16)
    Vaug = P.alloc(32 * 8 * 65, BF16)
    P.op("pool", lambda e: e.memset(Vaug.ap, 1.0), writes=[Vaug])
    m_proj = P.mark()
    wq = P.alloc(8 * 512, BF16)
    wk = P.alloc(8 * 512, BF16)
    wv = P.alloc(8 * 512, BF16)
    wg = P.alloc(8 * 16, BF16)
    dma("pool", wk.re("p (c n) -> p c n", c=8).ap, wview(w_in, 512, 1024), writes=[wk])
    dma("pool", wv.re("p (c n) -> p c n", c=8).ap, wview(w_in, 1024, 1536), writes=[wv])
    dma("pool", wg.re("p (c n) -> p c n", c=8).ap[:, :, 0:8], wview(w_in, 1536, 1544), writes=[wg])
    dma("pool", wg.re("p (c n) -> p c n", c=8).ap[:, :, 8:16], wview(w_in, 2568, 2576), writes=[wg])
    dma("pool", wq.re("p (c n) -> p c n", c=8).ap, wview(w_in, 0, 512), writes=[wq])
    xs_a = [P.alloc(1024) for _ in range(4)]
    xT2 = [P.alloc(8 * 512, BF16) for _ in range(2)]
    for tb in range(8):
        xT = xT2[tb % 2]
        load_x_block(tb, xs_a)
        transpose_block(xs_a, xT)
        for hp in range(4):
            ps = P.psum(P.nb())
            for c in range(8):
                mm(ps, wk.ap[:, c * 512 + hp * 128:c * 512 + (hp + 1) * 128], xT.ap[:, c * 512:(c + 1) * 512],
                   c == 0, c == 7, [wk, xT])
            o = KT[hp * NALL + tb * 512:hp * NALL + (tb + 1) * 512]
            evac(o, o.ap, ps, ps.ap)
        if tb >= 4:
            for hp in range(4):
                ps = P.psum(P.nb())
                for c in range(8):
                    mm(ps, wq.ap[:, c * 512 + hp * 128:c * 512 + (hp + 1) * 128], xT.ap[:, c * 512:(c + 1) * 512],
                       c == 0, c == 7, [wq, xT])
                o = QT[hp * NTOK + (tb - 4) * 512:hp * NTOK + (tb - 3) * 512]
                P.op("act", lambda e, o=o, ps=ps: e.activation(out=o.ap, in_=ps.ap, func=AF.Copy, scale=0.125),
                     reads=[ps], writes=[o])
        for i in range(4):
            blk = tb * 4 + i
            ps = P.psum(P.nb())
            for c in range(8):
                mm(ps, xT.ap[:, c * 512 + i * 128:c * 512 + (i + 1) * 128], wv.ap[:, c * 512:(c + 1) * 512],
                   c == 0, c == 7, [xT, wv])
            o = Vaug[blk * 520:(blk + 1) * 520]
            evac(o, o.ap.rearrange("p (h d) -> p h d", d=65)[:, :, 0:64], ps, ps.ap.rearrange("p (h d) -> p h d", d=64))
            ps2 = P.psum(P.nb(), 0, 16)
            for c in range(8):
                mm(ps2, xT.ap[:, c * 512 + i * 128:c * 512 + (i + 1) * 128], wg.ap[:, c * 16:(c + 1) * 16],
                   c == 0, c == 7, [xT, wg])
            o2 = gpre[blk * 16:(blk + 1) * 16]
            evac(o2, o2.ap, ps2, ps2.ap, "dve")
    P.release(m_proj)
    if stop < 1:
        return finish()

    tt("dve", gpre.ap, gpre.ap, gbias.ap, ALU.add, rd=[gpre, gbias], wr=[gpre])
    act(glog.ap, gpre.ap, AF.Sigmoid, rd=[gpre], wr=[glog])
    act(glog.ap, glog.ap, AF.Ln, rd=[glog], wr=[glog])
    ps = P.psum(P.nb())
    mm(ps, Uincl.ap, glog.ap, True, True, [Uincl, glog])
    evac(cs, cs.ap, ps, ps.ap, "dve")
    ps = P.psum(P.nb())
    mm(ps, ones.ap, glog.ap, True, True, [ones, glog])
    evac(tot, tot.ap, ps, ps.ap, "dve")
    m_fox = P.mark()
    pa = P.alloc(512)
    pb = P.alloc(512)
    evac(pa, pa.ap, tot, tot.ap, "dve")
    cur, oth = pa, pb
    for k in range(5):
        s = 16 * (2 ** k)
        evac(oth[0:s], oth.ap[:, 0:s], cur[0:s], cur.ap[:, 0:s], "dve")
        tt("dve", oth.ap[:, s:512], cur.ap[:, s:512], cur.ap[:, 0:512 - s], ALU.add, rd=[cur], wr=[oth[s:512]])
        cur, oth = oth, cur
    pincl = cur
    pexcl = oth
    tt("dve", pexcl.ap, pincl.ap, tot.ap, ALU.subtract, rd=[pincl, tot], wr=[pexcl])
    negc = P.alloc(512)
    stt("dve", negc.ap, cs.ap, -1.0, pexcl.ap, ALU.mult, ALU.subtract, rd=[cs, pexcl], wr=[negc])
    biasT = P.alloc(4 * 32 * 8)
    for G in range(4):
        for h in range(8):
            col = (16 + 4 * G) * 16 + h
            o = biasT[G * 256:(G + 1) * 256]
            ts("dve", o.ap.rearrange("p (b h) -> p b h", h=8)[:, :, h], negc.ap.rearrange("p (b g) -> p b g", g=16)[:, :, h],
               pexcl.ap[:, col:col + 1], None, ALU.add, rd=[negc, pexcl], wr=[o])
        o = biasT[G * 256:G * 256 + 128]
        ts("dve", o.ap, o.ap, kmask.ap, None, ALU.add, rd=[o, kmask], wr=[o])
    foxg = P.alloc(512)
    dma("sp", foxg.ap, foxg_d.partition_broadcast(128), writes=[foxg])
    if stop < 1.2:
        return finish()

    P.nbanks = 6
    PT3 = [P.alloc(1024, BF16) for _ in range(3)]
    Vs = [P.alloc(65, BF16) for _ in range(8)]
    wexp = P.alloc(4 * 32 * 8)
    ts("dve", wexp.ap, biasT.ap, 80.0, None, ALU.min, rd=[biasT], wr=[wexp])
    act(wexp.ap, wexp.ap, AF.Exp, rd=[wexp], wr=[wexp])
    ot_sb = [P.alloc(512) for _ in range(2)]
    for t_ in ot_sb:
        P.op("pool", lambda e, t_=t_: e.memset(t_.ap, 0.0), writes=[t_])
    rden_t = P.alloc(128)
    ssraw = P.alloc(128)
    rstd_t = P.alloc(128)
    o_sb = [P.alloc(64) for _ in range(2)]
    its = []
    for h in range(8):
        for G in range(4):
            if stop < 1.4 and (h, G) != (0, 0):
                continue
            nk = 20 + 4 * G
            for kp in range(nk // 2):
                its.append((h, G, kp, nk))
    S_of = {}
    vi = [0]

    def fox_S(n):
        h, G, kp, nk = its[n]
        hp, po = h // 2, (h % 2) * 64
        b = (0, 2, 4)[n % 3]
        for t in range(2):
            kb = 2 * kp + t
            sps = P.psum(b + t)
            mm(sps, KT.ap[po:po + 64, hp * NALL + kb * 128:hp * NALL + (kb + 1) * 128],
               QT.ap[po:po + 64, hp * NTOK + G * 512:hp * NTOK + (G + 1) * 512], True, True, [KT, QT])
        S_of[n] = b

    def fox_rest(n):
        h, G, kp, nk = its[n]
        b = S_of.pop(n)
        ob = 6
        pt = PT3[n % 3]
        spair = V(P.ps[:, b * 512:(b + 2) * 512], "P", b * 2048, (b + 2) * 2048, 4)
        act(pt.ap, spair.ap, AF.Exp, rd=[spair], wr=[pt])
        for t in range(2):
            kb = 2 * kp + t
            j0 = max(0, kb - (16 + 4 * G))
            col0 = 128 * j0
            ncols = 512 - col0
            ptk = pt[t * 512 + col0:(t + 1) * 512]
            if kb >= 16 + 4 * G:
                pd = pt[t * 512 + col0:t * 512 + col0 + 128]
                tt("pool", pd.ap, pd.ap, causb.ap, ALU.mult, rd=[pd, causb], wr=[pd])
            vs = Vs[vi[0] % 8]
            vi[0] += 1
            bcol = G * 256 + kb * 8 + h
            vsl = Vaug[kb * 520 + h * 65:kb * 520 + (h + 1) * 65]
            ts("dve", vs.ap, vsl.ap, wexp.ap[:, bcol:bcol + 1], None, ALU.mult, rd=[vsl, wexp], wr=[vs])
            ops_ = P.psum(ob, col0, ncols)
            P.op("pe", lambda e, ops_=ops_, vs=vs, ptk=ptk, kb=kb: e.matmul(ops_.ap[0:65, :], vs.ap, ptk.ap, start=(kb == 0), stop=(kb == nk - 1)),
                 reads=[vs, ptk], writes=[ops_])
        if 2 * kp + 1 != nk - 1 or stop < 1.3:
            return
        opsf = P.psum(ob)
        osb = ot_sb[(h * 4 + G) % 2]
        evac(osb, osb.ap[0:65, :], opsf, opsf.ap[0:65, :], "dve")
        tb_ = 7
        for j in range(4):
            ps = P.psum(tb_, j * 128, 128)
            tr(ps, osb, osb.ap[:, j * 128:(j + 1) * 128], ident, ident.ap)
        for j in range(4):
            qb = G * 4 + j
            c_ = qb * 8 + h
            ps = P.psum(tb_, j * 128, 128)
            P.op("dve", lambda e, ps=ps, c_=c_: e.reciprocal(out=rden_t.ap[:, c_:c_ + 1], in_=ps.ap[:, 64:65]),
                 reads=[ps], writes=[rden_t[c_:c_ + 1]])
            mo = mixed[qb * 1024 + h * 64:qb * 1024 + (h + 1) * 64]
            ts("dve", mo.ap, ps.ap[:, 0:64], rden_t.ap[:, c_:c_ + 1], None, ALU.mult, rd=[ps, rden_t[c_:c_ + 1]], wr=[mo])
            osj = o_sb[j % 2]
            ts("dve", osj.ap, ps.ap[:, 0:64], rden_t.ap[:, c_:c_ + 1], None, ALU.mult, rd=[ps, rden_t[c_:c_ + 1]], wr=[osj])
            stt("dve", junk.ap[:, 0:64], osj.ap, 1.0, osj.ap, ALU.mult, ALU.mult, rd=[osj],
                wr=[junk[0:64], ssraw[c_:c_ + 1]], acc=ssraw.ap[:, c_:c_ + 1])

    LA = 1
    for n in range(len(its) + LA):
        if n < len(its):
            fox_S(n)
        if n >= LA:
            fox_rest(n - LA)
    P.nbanks = 8
    if stop >= 1.4:
        rstd_from_ss(ssraw, 64, 1, rstd_t)
        for qb in range(16):
            for h in range(8):
                c_ = qb * 8 + h
                mo = mixed[qb * 1024 + h * 64:qb * 1024 + (h + 1) * 64]
                stt("dve", mo.ap, mo.ap, rstd_t.ap[:, c_:c_ + 1], foxg.ap[:, h * 64:(h + 1) * 64], ALU.mult, ALU.mult,
                    rd=[mo, rstd_t, foxg], wr=[mo])
    P.release(m_phase1)
    if stop < 2:
        return finish()

    m1b = P.mark()
    wmq = P.alloc(8 * 256, BF16)
    wmk = P.alloc(8 * 256, BF16)
    wmv = P.alloc(8 * 512, BF16)
    wmo = P.alloc(8 * 512, BF16)
    dma("pool", wmk.re("p (c n) -> p c n", c=8).ap, wview(w_in, 1800, 2056), writes=[wmk])
    dma("pool", wmv.re("p (c n) -> p c n", c=8).ap, wview(w_in, 2056, 2568), writes=[wmv])
    dma("pool", wmq.re("p (c n) -> p c n", c=8).ap, wview(w_in, 1544, 1800), writes=[wmq])
    dma("pool", wmo.re("p (c n) -> p c n", c=8).ap, wview(w_in, 2576, 3088), writes=[wmo])
    convT = P.alloc(16)
    dma("sp", convT.ap, convT_d, writes=[convT])
    mlg = P.alloc(512)
    dma("sp", mlg.ap, mlg_d.partition_broadcast(128), writes=[mlg])
    xs_b = [P.alloc(1024) for _ in range(4)]
    xTb = [P.alloc(8 * 512, BF16) for _ in range(2)]
    kbuf = [P.alloc(515) for _ in range(2)]
    qbuf = [P.alloc(515) for _ in range(2)]
    for t_ in kbuf + qbuf:
        P.op("pool", lambda e, t_=t_: e.memset(t_.ap, 0.0), writes=[t_])
    cacc = P.alloc(512)
    ksil2 = [[P.alloc(512, BF16) for _ in range(2)] for _ in range(2)]
    qsil2 = [[P.alloc(512, BF16) for _ in range(2)] for _ in range(2)]
    ktok2 = [[P.alloc(256) for _ in range(4)] for _ in range(2)]
    vaug2 = [[P.alloc(4 * 129, BF16) for _ in range(4)] for _ in range(2)]
    for t_ in vaug2[0] + vaug2[1]:
        P.op("pool", lambda e, t_=t_: e.memset(t_.ap, 1.0), writes=[t_])
    osig2 = [[P.alloc(512) for _ in range(4)] for _ in range(2)]
    Cst = [P.alloc(129) for _ in range(4)]
    Cbf = [P.alloc(129, BF16) for _ in range(4)]
    for t_ in Cst + Cbf:
        P.op("pool", lambda e, t_=t_: e.memset(t_.ap, 0.0), writes=[t_])
    g4s = [P.alloc(32) for _ in range(4)]
    diagb4 = [P.alloc(128) for _ in range(4)]
    DT4 = [P.alloc(128) for _ in range(4)]
    PTm4 = [P.alloc(128, BF16) for _ in range(4)]
    kw0s = [P.alloc(64, BF16) for _ in range(2)]
    kw1s = [P.alloc(128, BF16) for _ in range(2)]
    for t_ in kw1s:
        P.op("pool", lambda e, t_=t_: e.memset(t_.ap, 0.0), writes=[t_])
    hun4 = [P.alloc(129) for _ in range(4)]
    hstore = P.alloc(16 * 128)
    ssm = P.alloc(16)
    rsm = P.alloc(16)
    dn4 = P.alloc(4)

    def conv_silu(buf, psrc, chunk, outv, scale):
        evac(buf[3:515], buf.ap[:, 3:515], psrc, psrc.ap)
        ts("dve", cacc.ap, buf.ap[:, 0:512], convT.ap[:, chunk * 4:chunk * 4 + 1], None, ALU.mult, rd=[buf, convT], wr=[cacc])
        for j in range(1, 4):
            stt("dve", cacc.ap, buf.ap[:, j:j + 512], convT.ap[:, chunk * 4 + j:chunk * 4 + j + 1], cacc.ap, ALU.mult, ALU.add,
                rd=[buf, convT, cacc], wr=[cacc])
        evac(buf[0:3], buf.ap[:, 0:3], buf[512:515], buf.ap[:, 512:515], "pool")
        if scale is None:
            act(outv.ap, cacc.ap, AF.Silu, rd=[cacc], wr=[outv])
        else:
            act(cacc.ap, cacc.ap, AF.Silu, rd=[cacc], wr=[cacc])
            ts("dve", outv.ap, cacc.ap, scale, None, ALU.mult, rd=[cacc], wr=[outv])

    def ml_proj(tb):
        own = tb >= 4
        xT = xTb[tb % 2]
        ksil, qsil, ktok, vaug, osig = ksil2[tb % 2], qsil2[tb % 2], ktok2[tb % 2], vaug2[tb % 2], osig2[tb % 2]
        load_x_block(tb, xs_b)
        transpose_block(xs_b, xT)
        for cc in range(2):
            ps = P.psum(P.nb())
            for c in range(8):
                mm(ps, wmk.ap[:, c * 256 + cc * 128:c * 256 + (cc + 1) * 128], xT.ap[:, c * 512:(c + 1) * 512],
                   c == 0, c == 7, [wmk, xT])
            conv_silu(kbuf[cc], ps, 2 + cc, ksil[cc], 0.125)
            if tb >= 3:
                ps = P.psum(P.nb())
                for c in range(8):
                    mm(ps, wmq.ap[:, c * 256 + cc * 128:c * 256 + (cc + 1) * 128], xT.ap[:, c * 512:(c + 1) * 512],
                       c == 0, c == 7, [wmq, xT])
                conv_silu(qbuf[cc], ps, cc, qsil[cc], None)
        for i in range(4):
            for cc in range(2):
                ps = P.psum(P.nb(), 0, 128, BF16)
                tr(ps, ksil[cc][i * 128:(i + 1) * 128], ksil[cc].ap[:, i * 128:(i + 1) * 128], identb, identb.ap)
                o = ktok[i][cc * 128:(cc + 1) * 128]
                evac(o, o.ap, ps, ps.ap)
            ps = P.psum(P.nb())
            for c in range(8):
                mm(ps, xT.ap[:, c * 512 + i * 128:c * 512 + (i + 1) * 128], wmv.ap[:, c * 512:(c + 1) * 512],
                   c == 0, c == 7, [xT, wmv])
            evac(vaug[i], vaug[i].ap.rearrange("p (h d) -> p h d", d=129)[:, :, 0:128], ps,
                 ps.ap.rearrange("p (h d) -> p h d", d=128))
            if own:
                ps = P.psum(P.nb())
                for c in range(8):
                    mm(ps, xT.ap[:, c * 512 + i * 128:c * 512 + (i + 1) * 128], wmo.ap[:, c * 512:(c + 1) * 512],
                       c == 0, c == 7, [xT, wmo])
                act(osig[i].ap, ps.ap, AF.Sigmoid, rd=[ps], wr=[osig[i]])
    def ml_chunks(tb):
        own = tb >= 4
        ksil, qsil, ktok, vaug, osig = ksil2[tb % 2], qsil2[tb % 2], ktok2[tb % 2], vaug2[tb % 2], osig2[tb % 2]
        for i in range(4):
            blk = tb * 4 + i
            qb = blk - 16
            fcol = blk * 16 + 12
            icol = blk * 16 + 8
            g4 = g4s[i]
            tt("dve", g4.ap[:, 16:20], tot.ap[:, fcol:fcol + 4], cs.ap[:, fcol:fcol + 4], ALU.subtract, rd=[tot, cs], wr=[g4[16:20]])
            tt("dve", g4.ap[:, 16:20], g4.ap[:, 16:20], gpre.ap[:, icol:icol + 4], ALU.add, rd=[g4[16:20], gpre], wr=[g4[16:20]])
            act(g4.ap[:, 4:8], g4.ap[:, 16:20], AF.Exp, rd=[g4[16:20]], wr=[g4[4:8]])
            act(g4.ap[:, 8:12], tot.ap[:, fcol:fcol + 4], AF.Exp, rd=[tot], wr=[g4[8:12]])
            if own:
                act(g4.ap[:, 0:4], cs.ap[:, fcol:fcol + 4], AF.Exp, rd=[cs], wr=[g4[0:4]])
                tt("dve", g4.ap[:, 12:16], gpre.ap[:, icol:icol + 4], cs.ap[:, fcol:fcol + 4], ALU.subtract, rd=[gpre, cs], wr=[g4[12:16]])
            H4 = range(4)
            vas = [vaug[i][hh * 129:(hh + 1) * 129] for hh in H4]
            ccs = [hh // 2 for hh in H4]
            pos = [(hh % 2) * 64 for hh in H4]
            if own:
                for hh in H4:
                    ts("dve", diagb4[hh].ap, ident.ap, cs.ap[:, fcol + hh:fcol + hh + 1], None, ALU.mult, rd=[ident, cs], wr=[diagb4[hh]])
                eps4 = []
                for hh in H4:
                    eps_ = P.psum(P.nb(), 0, 128)
                    mm(eps_, ones.ap, diagb4[hh].ap, True, False, [ones, diagb4[hh]])
                    mm(eps_, ident.ap, maskneg.ap, False, True, [ident, maskneg])
                    eps4.append(eps_)
                for hh in H4:
                    act(DT4[hh].ap, eps4[hh].ap, AF.Exp, rd=[eps4[hh], g4[12:16]], wr=[DT4[hh]], bias=g4.ap[:, 12 + hh:13 + hh])
                sps4 = []
                for hh in H4:
                    cc, po = ccs[hh], pos[hh]
                    sps = P.psum(P.nb(), 0, 128)
                    mm(sps, ksil[cc].ap[po:po + 64, i * 128:(i + 1) * 128], qsil[cc].ap[po:po + 64, i * 128:(i + 1) * 128],
                       True, True, [ksil[cc], qsil[cc]])
                    sps4.append(sps)
                for hh in H4:
                    tt("dve", PTm4[hh].ap, DT4[hh].ap, sps4[hh].ap, ALU.mult, rd=[DT4[hh], sps4[hh]], wr=[PTm4[hh]])
                n12 = []
                for hh in H4:
                    cc, po = ccs[hh], pos[hh]
                    n2 = P.psum(P.nb(), 0, 129)
                    mm(n2, qsil[cc].ap[po:po + 64, i * 128:(i + 1) * 128], Cbf[hh].ap[po:po + 64, :], True, True, [qsil[cc], Cbf[hh]])
                    n12.append(n2)
                for hh in H4:
                    ts("dve", hun4[hh].ap, n12[hh].ap, g4.ap[:, hh:hh + 1], None, ALU.mult, rd=[n12[hh], g4[0:4]], wr=[hun4[hh]])
                n11 = []
                for hh in H4:
                    n1 = P.psum(P.nb(), 0, 129)
                    mm(n1, PTm4[hh].ap, vas[hh].ap, True, True, [PTm4[hh], vas[hh]])
                    n11.append(n1)
                for hh in H4:
                    tt("dve", hun4[hh].ap, hun4[hh].ap, n11[hh].ap, ALU.add, rd=[hun4[hh], n11[hh]], wr=[hun4[hh]])
                for hh in H4:
                    u = i * 4 + hh
                    hs = hstore[u * 128:(u + 1) * 128]
                    d1 = dn4[hh:hh + 1]
                    stt("dve", d1.ap, hun4[hh].ap[:, 128:129], -1.0, hun4[hh].ap[:, 128:129], ALU.mult, ALU.max, rd=[hun4[hh]], wr=[d1])
                    ts("dve", d1.ap, d1.ap, 1.0, None, ALU.max, rd=[d1], wr=[d1])
                    P.op("dve", lambda e, d1=d1: e.reciprocal(out=d1.ap, in_=d1.ap), reads=[d1], writes=[d1])
                    ts("dve", hs.ap, hun4[hh].ap[:, 0:128], d1.ap, None, ALU.mult, rd=[hun4[hh], d1], wr=[hs])
                    stt("dve", junk.ap[:, 0:128], hs.ap, 1.0, hs.ap, ALU.mult, ALU.mult, rd=[hs], wr=[junk[0:128], ssm[u:u + 1]],
                        acc=ssm.ap[:, u:u + 1])
            dps4 = []
            for hh in H4:
                po = pos[hh]
                dps = P.psum(P.nb(), 0, 129)
                if po == 0:
                    kw0 = kw0s[hh // 2]
                    ts("dve", kw0.ap, ktok[i].ap[:, hh * 64:(hh + 1) * 64], g4.ap[:, 4 + hh:5 + hh], None, ALU.mult,
                       rd=[ktok[i], g4[4:8]], wr=[kw0])
                    P.op("pe", lambda e, dps=dps, va=vas[hh], kw0=kw0: e.matmul(dps.ap[0:64, :], kw0.ap, va.ap, start=True, stop=True),
                         reads=[kw0, vas[hh]], writes=[dps])
                else:
                    kw1 = kw1s[hh // 2]
                    ts("dve", kw1.ap[:, 64:128], ktok[i].ap[:, hh * 64:(hh + 1) * 64], g4.ap[:, 4 + hh:5 + hh], None, ALU.mult,
                       rd=[ktok[i], g4[4:8]], wr=[kw1])
                    P.op("pe", lambda e, dps=dps, va=vas[hh], kw1=kw1: e.matmul(dps.ap, kw1.ap, va.ap, start=True, stop=True),
                         reads=[kw1, vas[hh]], writes=[dps])
                dps4.append(dps)
            for hh in H4:
                po = pos[hh]
                stt("dve", Cst[hh].ap[po:po + 64, :], Cst[hh].ap[po:po + 64, :], g4.ap[po:po + 64, 8 + hh:9 + hh], dps4[hh].ap[po:po + 64, :],
                    ALU.mult, ALU.add, rd=[Cst[hh], g4[8:12], dps4[hh]], wr=[Cst[hh]])
            for hh in H4:
                po = pos[hh]
                evac(Cbf[hh], Cbf[hh].ap[po:po + 64, :], Cst[hh], Cst[hh].ap[po:po + 64, :], "pool")
        if own:
            rstd_from_ss(ssm, 128, 1, rsm)
            for i in range(4):
                qb = tb * 4 + i - 16
                for hh in range(4):
                    u = i * 4 + hh
                    hs = hstore[u * 128:(u + 1) * 128]
                    stt("dve", hs.ap, hs.ap, rsm.ap[:, u:u + 1], mlg.ap[:, hh * 128:(hh + 1) * 128], ALU.mult, ALU.mult,
                        rd=[hs, rsm, mlg], wr=[hs])
                    mo = mixed[qb * 1024 + 512 + hh * 128:qb * 1024 + 512 + (hh + 1) * 128]
                    tt("pool", mo.ap, hs.ap, osig[i].ap[:, hh * 128:(hh + 1) * 128], ALU.mult, rd=[hs, osig[i]], wr=[mo])
    ml_proj(0)
    for tb in range(8):
        if tb + 1 < 8:
            ml_proj(tb + 1)
        ml_chunks(tb)
    P.release(m1b)
    P.release(m_gates)
    if stop < 3:
        return finish()

    mdm = P.mark()
    dump_mixed()
    P.release(mdm)

    m2 = P.mark()
    lnp2 = P.alloc(4 * 1024)
    dma("sp", lnp2.ap, ln_d[:, 0:4096].partition_broadcast(128), writes=[lnp2])
    lnp_box[0], lnp_box[1] = lnp2, 0
    wmix = P.alloc(8 * 1024, BF16)
    wxq = P.alloc(8 * 1024, BF16)
    wxo = P.alloc(8 * 1024, BF16)
    dma("pool", wmix.re("p (c n) -> p c n", c=8).ap, wview(w_mix, 0, 1024), writes=[wmix])
    wr_sb = P.alloc(8 * 32)
    dma("sp", wr_sb.re("p (c n) -> p c n", c=8).ap, wview(w_r, 0, 32), writes=[wr_sb])
    br_bc = P.alloc(32)
    dma("sp", br_bc.ap, b_r.partition_broadcast(128), writes=[br_bc])
    KmT = P.alloc(8 * 256, BF16)
    Vm = P.alloc(2 * 4 * 257, BF16)
    P.op("pool", lambda e: e.memset(Vm.ap, 1.0), writes=[Vm])
    macc = P.alloc(32)
    P.op("pool", lambda e: e.memset(macc.ap, 0.0), writes=[macc])
    m2a = P.mark()
    wtmp = P.alloc(8 * 1024, BF16)
    mems = [P.alloc(1024) for _ in range(2)]
    memT = P.alloc(8 * 256, BF16)
    for mt in range(2):
        dma("sp", mems[mt].ap, memb[mt * 128:(mt + 1) * 128, :], writes=[mems[mt]])
    for c in range(8):
        b = P.nb()
        for mt in range(2):
            ps = P.psum(b, mt * 128, 128)
            tr(ps, mems[mt][c * 128:(c + 1) * 128], mems[mt].ap[:, c * 128:(c + 1) * 128], ident, ident.ap)
        psf = P.psum(b, 0, 256)
        evac(memT[c * 256:(c + 1) * 256], memT.ap[:, c * 256:(c + 1) * 256], psf, psf.ap)
    dma("pool", wtmp.re("p (c n) -> p c n", c=8).ap, wview(w_xk, 0, 1024), writes=[wtmp])
    for jc in range(8):
        ps = P.psum(P.nb(), 0, 256)
        for c in range(8):
            mm(ps, wtmp.ap[:, c * 1024 + jc * 128:c * 1024 + (jc + 1) * 128], memT.ap[:, c * 256:(c + 1) * 256],
               c == 0, c == 7, [wtmp, memT])
        o = KmT[jc * 256:(jc + 1) * 256]
        evac(o, o.ap, ps, ps.ap)
    dma("pool", wtmp.re("p (c n) -> p c n", c=8).ap, wview(w_xv, 0, 1024), reads=[], writes=[wtmp])
    for mt in range(2):
        for half in range(2):
            ps = P.psum(P.nb())
            for c in range(8):
                mm(ps, memT.ap[:, c * 256 + mt * 128:c * 256 + (mt + 1) * 128], wtmp.ap[:, c * 1024 + half * 512:c * 1024 + (half + 1) * 512],
                   c == 0, c == 7, [memT, wtmp])
            o = Vm[mt * 1028 + half * 514:mt * 1028 + (half + 1) * 514]
            evac(o, o.ap.rearrange("p (h d) -> p h d", d=257)[:, :, 0:256], ps, ps.ap.rearrange("p (h d) -> p h d", d=256))
    P.release(m2a)
    dma("pool", wxq.re("p (c n) -> p c n", c=8).ap, wview(w_xq, 0, 1024), writes=[wxq])
    dma("pool", wxo.re("p (c n) -> p c n", c=8).ap, wview(w_xo, 0, 1024), writes=[wxo])

    mT = P.alloc(8 * 128, BF16)
    x1s = [[P.alloc(1024) for _ in range(4)] for _ in range(2)]
    ytmpA = P.alloc(1024)
    ytmpC = P.alloc(1024)
    ltA = P.alloc(8)
    ltC = P.alloc(8)
    xres = P.alloc(1024)
    x1T = P.alloc(8 * 512, BF16)
    oT = P.alloc(8 * 512, BF16)
    qT = P.alloc(8 * 512, BF16)
    PTx = [P.alloc(512, BF16) for _ in range(2)]
    oatt = [P.alloc(1024, BF16) for _ in range(4)]
    x2 = P.alloc(1024)
    x2b = P.alloc(1024, BF16)
    x2T = ytmpC
    lg = P.alloc(32)
    m8 = P.alloc(8)
    msk = P.alloc(32)
    ex = P.alloc(32)
    rt = P.alloc(8)
    rtB = P.alloc(8)
    Dm = P.alloc(32)
    d8 = P.alloc(8)

    def stageA(grp):
        x1 = x1s[grp % 2]
        for i in range(4):
            qb = grp * 4 + i
            b = P.nb()
            for c in range(8):
                ps = P.psum(b, c * 128, 128, BF16)
                msl = mixed[qb * 1024 + c * 128:qb * 1024 + (c + 1) * 128]
                tr(ps, msl, msl.ap, identb, identb.ap)
            psf = P.psum(b, 0, 1024, BF16)
            evac(mT, mT.ap, psf, psf.ap)
            dma("sp", xres.ap, xcat[NTOK + qb * 128:NTOK + (qb + 1) * 128, :], writes=[xres])
            for half in range(2):
                ps = P.psum(P.nb())
                for c in range(8):
                    mm(ps, mT.ap[:, c * 128:(c + 1) * 128], wmix.ap[:, c * 1024 + half * 512:c * 1024 + (half + 1) * 512],
                       c == 0, c == 7, [mT, wmix])
                stt("dve", ytmpA.ap[:, half * 512:(half + 1) * 512], xres.ap[:, half * 512:(half + 1) * 512], ALPHA, ps.ap,
                    ALU.mult, ALU.add, rd=[xres, ps], wr=[ytmpA[half * 512:(half + 1) * 512]])
            layer_norm(ytmpA, 0, x1[i], ltA)
            if dbg:
                dma("sp", dbg_x1[qb * 128:(qb + 1) * 128, :], x1[i].ap, reads=[x1[i]])

    def stageB1(grp):
        x1 = x1s[grp % 2]
        for c in range(8):
            b = P.nb()
            for i in range(4):
                ps = P.psum(b, i * 128, 128)
                tr(ps, x1[i][c * 128:(c + 1) * 128], x1[i].ap[:, c * 128:(c + 1) * 128], ident, ident.ap)
            psf = P.psum(b)
            evac(x1T[c * 512:(c + 1) * 512], x1T.ap[:, c * 512:(c + 1) * 512], psf, psf.ap)

    def stageB2(grp):
        for jc in range(8):
            ps = P.psum(P.nb())
            for c in range(8):
                mm(ps, wxq.ap[:, c * 1024 + jc * 128:c * 1024 + (jc + 1) * 128], x1T.ap[:, c * 512:(c + 1) * 512],
                   c == 0, c == 7, [wxq, x1T])
            o = qT[jc * 512:(jc + 1) * 512]
            evac(o, o.ap, ps, ps.ap)

    def stageB3(grp):
        for hx in range(4):
            pts = []
            for mt in range(2):
                ps = P.psum(P.nb())
                for k2 in range(2):
                    jc = hx * 2 + k2
                    mm(ps, KmT.ap[:, jc * 256 + mt * 128:jc * 256 + (mt + 1) * 128], qT.ap[:, jc * 512:(jc + 1) * 512],
                       k2 == 0, k2 == 1, [KmT, qT])
                pt = PTx[mt]
                act(pt.ap, ps.ap, AF.Exp, rd=[ps], wr=[pt], scale=1.0 / 16.0)
                pts.append(pt)
            for i in range(4):
                ps = P.psum(P.nb(), 0, 257)
                for mt in range(2):
                    vsl = Vm[mt * 1028 + hx * 257:mt * 1028 + (hx + 1) * 257]
                    mm(ps, pts[mt].ap[:, i * 128:(i + 1) * 128], vsl.ap, mt == 0, mt == 1, [pts[mt], vsl])
                P.op("dve", lambda e, ps=ps: e.reciprocal(out=rtB.ap[:, 0:1], in_=ps.ap[:, 256:257]), reads=[ps], writes=[rtB[0:1]])
                o = oatt[i][hx * 256:(hx + 1) * 256]
                ts("dve", o.ap, ps.ap[:, 0:256], rtB.ap[:, 0:1], None, ALU.mult, rd=[ps, rtB[0:1]], wr=[o])

    def stageB4(grp):
        for c in range(8):
            b = P.nb()
            for i in range(4):
                ps = P.psum(b, i * 128, 128, BF16)
                tr(ps, oatt[i][c * 128:(c + 1) * 128], oatt[i].ap[:, c * 128:(c + 1) * 128], identb, identb.ap)
            psf = P.psum(b, 0, 512, BF16)
            evac(oT[c * 512:(c + 1) * 512], oT.ap[:, c * 512:(c + 1) * 512], psf, psf.ap)

    def stageC1(grp, i):
        x1 = x1s[grp % 2]
        qb = grp * 4 + i
        for half in range(2):
            ps = P.psum(P.nb())
            for c in range(8):
                mm(ps, oT.ap[:, c * 512 + i * 128:c * 512 + (i + 1) * 128], wxo.ap[:, c * 1024 + half * 512:c * 1024 + (half + 1) * 512],
                   c == 0, c == 7, [oT, wxo])
            stt("dve", ytmpC.ap[:, half * 512:(half + 1) * 512], x1[i].ap[:, half * 512:(half + 1) * 512], ALPHA, ps.ap,
                ALU.mult, ALU.add, rd=[x1[i], ps], wr=[ytmpC[half * 512:(half + 1) * 512]])
        layer_norm(ytmpC, 2, x2, ltC)
        dma("sp", X2[qb * 128:(qb + 1) * 128, :], x2.ap, reads=[x2], writes=[("X2", qb * 128, (qb + 1) * 128)])
        if dbg:
            dma("sp", dbg_x2[qb * 128:(qb + 1) * 128, :], x2.ap, reads=[x2])
        evac(x2b, x2b.ap, x2, x2.ap, "pool")

    def stageC2(grp, i):
        qb = grp * 4 + i
        for half in range(2):
            b = P.nb()
            for c4 in range(4):
                c = half * 4 + c4
                ps = P.psum(b, c4 * 128, 128)
                tr(ps, x2[c * 128:(c + 1) * 128], x2.ap[:, c * 128:(c + 1) * 128], ident, ident.ap)
            psf = P.psum(b)
            evac(x2T[half * 512:(half + 1) * 512], x2T.ap[:, half * 512:(half + 1) * 512], psf, psf.ap)
        ps = P.psum(P.nb(), 0, 32)
        for c in range(8):
            mm(ps, x2T.ap[:, c * 128:(c + 1) * 128], wr_sb.ap[:, c * 32:(c + 1) * 32], c == 0, c == 7, [x2T, wr_sb])
        tt("dve", lg.ap, ps.ap, br_bc.ap, ALU.add, rd=[ps, br_bc], wr=[lg])
        P.op("dve", lambda e: e.max(out=m8.ap, in_=lg.ap), reads=[lg], writes=[m8])
        ts("dve", msk.ap, lg.ap, m8.ap[:, 3:4], None, ALU.is_ge, rd=[lg, m8], wr=[msk])
        ts("dve", rt.ap[:, 1:2], m8.ap[:, 0:1], -1.0, None, ALU.mult, rd=[m8], wr=[rt[1:2]])
        act(ex.ap, lg.ap, AF.Exp, rd=[lg, rt[1:2]], wr=[ex], bias=rt.ap[:, 1:2])
        stt("dve", ex.ap, ex.ap, 1.0, msk.ap, ALU.mult, ALU.mult, rd=[ex, msk], wr=[ex, rt[2:3]], acc=rt.ap[:, 2:3])
        P.op("dve", lambda e: e.reciprocal(out=rt.ap[:, 2:3], in_=rt.ap[:, 2:3]), reads=[rt[2:3]], writes=[rt[2:3]])
        gf = gfull[qb * 32:(qb + 1) * 32]
        ts("dve", gf.ap, ex.ap, rt.ap[:, 2:3], None, ALU.mult, rd=[ex, rt[2:3]], wr=[gf])
        ps = P.psum(P.nb(), 0, 32)
        mm(ps, Ult.ap, msk.ap, True, False, [Ult, msk])
        mm(ps, ones.ap, macc.ap, False, True, [ones, macc])
        ts("dve", Dm.ap, ps.ap, float(CAP - 1), None, ALU.min, rd=[ps], wr=[Dm])
        tt("dve", Dm.ap, Dm.ap, ebase.ap, ALU.add, rd=[Dm, ebase], wr=[Dm])
        tt("dve", Dm.ap, Dm.ap, msk.ap, ALU.mult, rd=[Dm, msk], wr=[Dm])
        tt("pool", macc.ap, macc.ap, msk.ap, ALU.add, rd=[macc, msk], wr=[macc])
        P.op("dve", lambda e: e.max(out=d8.ap, in_=Dm.ap), reads=[Dm], writes=[d8])
        de = dest_all[qb * 4:(qb + 1) * 4]
        ts("dve", de.ap, d8.ap[:, 0:4], -1.0, None, ALU.add, rd=[d8], wr=[de])
        for k in range(4):
            ga = gate_all[qb * 4 + k:qb * 4 + k + 1]
            stt("dve", junk.ap[:, 0:32], Dm.ap, d8.ap[:, k:k + 1], gf.ap, ALU.is_equal, ALU.mult, rd=[Dm, d8, gf],
                wr=[junk[0:32], ga], acc=ga.ap)
            P.op("pool", lambda e, k=k, de=de: e.indirect_dma_start(
                out=Xg, out_offset=bass.IndirectOffsetOnAxis(ap=de.ap[:, k:k + 1], axis=0), in_=x2b.ap, in_offset=None),
                reads=[x2b, de], writes=[("Xg", 0, NE * CAP)], dma=True)

    stageA(0)
    stageB1(0)
    stageB2(0)
    stageB3(0)
    stageB4(0)
    for grp in range(4):
        nxt = grp + 1 < 4
        if nxt:
            stageA(grp + 1)
        Bs = [stageB1, stageB2, stageB3, stageB4]
        for i in range(4):
            stageC1(grp, i)
            if nxt:
                Bs[i](grp + 1)
            stageC2(grp, i)
    P.release(m2)
    if stop < 4:
        return finish()

    m3 = P.mark()
    bguT = P.alloc(NE * 16)
    dma("sp", bguT.ap, bguT_d, writes=[bguT])
    bguS = P.alloc(NE * 16)
    ts("dve", bguS.ap, bguT.ap, 1.0 / 1.702, None, ALU.mult, rd=[bguT], wr=[bguS])
    wgu2 = [P.alloc(8 * 2048, BF16) for _ in range(2)]
    wd2 = [P.alloc(8 * 1024, BF16) for _ in range(2)]
    Xe2 = [[P.alloc(1024, BF16) for _ in range(3)] for _ in range(2)]
    XeT2 = [P.alloc(8 * CAP, BF16) for _ in range(2)]
    hidT = P.alloc(8 * CAP, BF16)
    gt2 = [P.alloc(CAP) for _ in range(2)]
    sg2 = [P.alloc(CAP) for _ in range(2)]
    ln2 = [P.alloc(CAP) for _ in range(2)]
    Ysb = [P.alloc(1024, BF16) for _ in range(2)]

    def load_wgu(en):
        if en >= NE:
            return
        wgu_ = wgu2[en % 2]
        for c4 in range(4):
            sl_ = wgu_[c4 * 4096:(c4 + 1) * 4096]
            dma("pool", sl_.ap.rearrange("p (c n) -> p c n", c=2),
                w_gu[en, c4 * 256:(c4 + 1) * 256, :].rearrange("(c p) n -> p c n", p=128), writes=[sl_])

    def load_wd(en):
        if en >= NE:
            return
        wd_ = wd2[en % 2]
        for c4 in range(2):
            sl_ = wd_[c4 * 4096:(c4 + 1) * 4096]
            dma("pool", sl_.ap.rearrange("p (c n) -> p c n", c=4),
                w_dn[en, c4 * 512:(c4 + 1) * 512, :].rearrange("(c p) n -> p c n", p=128), writes=[sl_])

    def load_xe(en):
        if en >= NE:
            return
        for s in range(3):
            r0 = en * CAP + s * 128
            dma("sp", Xe2[en % 2][s].ap, Xg[r0:r0 + 128, :], reads=[("Xg", r0, r0 + 128)], writes=[Xe2[en % 2][s]])

    def xe_transposes(en):
        if en >= NE:
            return
        Xe, XeT = Xe2[en % 2], XeT2[en % 2]
        for c in range(8):
            b = P.nb()
            for s in range(3):
                ps = P.psum(b, s * 128, 128, BF16)
                tr(ps, Xe[s][c * 128:(c + 1) * 128], Xe[s].ap[:, c * 128:(c + 1) * 128], identb, identb.ap)
            psf = P.psum(b, 0, CAP, BF16)
            evac(XeT[c * CAP:(c + 1) * CAP], XeT.ap[:, c * CAP:(c + 1) * CAP], psf, psf.ap)

    load_wgu(0)
    load_wd(0)
    load_wgu(1)
    load_wd(1)
    load_xe(0)
    xe_transposes(0)
    yi = 0
    for ex_ in range(NE):
        wgu = wgu2[ex_ % 2]
        wd = wd2[ex_ % 2]
        XeT = XeT2[ex_ % 2]
        load_xe(ex_ + 1)
        for g in range(8):
            psg = P.psum(P.nb(), 0, CAP)
            for c in range(8):
                mm(psg, wgu.ap[:, c * 2048 + g * 128:c * 2048 + (g + 1) * 128], XeT.ap[:, c * CAP:(c + 1) * CAP],
                   c == 0, c == 7, [wgu[c * 2048:(c + 1) * 2048], XeT])
            psl = P.psum(P.nb(), 0, CAP)
            for c in range(8):
                mm(psl, wgu.ap[:, c * 2048 + 1024 + g * 128:c * 2048 + 1024 + (g + 1) * 128], XeT.ap[:, c * CAP:(c + 1) * CAP],
                   c == 0, c == 7, [wgu[c * 2048:(c + 1) * 2048], XeT])
            gt, sg, ln = gt2[g % 2], sg2[g % 2], ln2[g % 2]
            bg = bguT[ex_ * 16 + g:ex_ * 16 + g + 1]
            bl = bguT[ex_ * 16 + 8 + g:ex_ * 16 + 8 + g + 1]
            ts("dve", gt.ap, psg.ap, bg.ap, 7.0, ALU.add, ALU.min, rd=[psg, bg], wr=[gt])
            act(sg.ap, gt.ap, AF.Silu, rd=[gt], wr=[sg], scale=1.702)
            bls = bguS[ex_ * 16 + 8 + g:ex_ * 16 + 8 + g + 1]
            act(ln.ap, psl.ap, AF.Identity, rd=[psl, bls], wr=[ln], bias=bls.ap, scale=1.0 / 1.702)
            ts("dve", ln.ap, ln.ap, 7.0 / 1.702, -7.0 / 1.702, ALU.min, ALU.max, rd=[ln], wr=[ln])
            o = hidT[g * CAP:(g + 1) * CAP]
            stt("dve", o.ap, ln.ap, 1.0 / 1.702, sg.ap, ALU.add, ALU.mult, rd=[ln, sg], wr=[o])
        load_wgu(ex_ + 2)
        xe_transposes(ex_ + 1)
        for s in range(3):
            ysb = Ysb[yi % 2]
            yi += 1
            for half in range(2):
                ps = P.psum(P.nb())
                for g in range(8):
                    mm(ps, hidT.ap[:, g * CAP + s * 128:g * CAP + (s + 1) * 128], wd.ap[:, g * 1024 + half * 512:g * 1024 + (half + 1) * 512],
                       g == 0, g == 7, [hidT, wd[g * 1024:(g + 1) * 1024]])
                evac(ysb[half * 512:(half + 1) * 512], ysb.ap[:, half * 512:(half + 1) * 512], ps, ps.ap)
            r0 = ex_ * CAP + s * 128
            dma("sp", Yg[r0:r0 + 128, :], ysb.ap, reads=[ysb], writes=[("Yg", r0, r0 + 128)])
        load_wd(ex_ + 2)
    P.release(m3)
    if stop < 5:
        return finish()

    lnp4 = P.alloc(2 * 1024)
    dma("sp", lnp4.ap, ln_d[:, 4096:6144].partition_broadcast(128), writes=[lnp4])
    lnp_box[0], lnp_box[1] = lnp4, 4
    bdn = P.alloc(1024)
    dma("sp", bdn.ap[0:32, :], b_dn, writes=[bdn])
    Gk2 = [[P.alloc(1024, BF16) for _ in range(4)] for _ in range(2)]
    acc2 = [P.alloc(1024) for _ in range(2)]
    x2r2 = [P.alloc(1024) for _ in range(2)]
    gT = P.alloc(128)
    outt2 = [P.alloc(1024) for _ in range(2)]
    lt4 = P.alloc(8)

    def comb_loads(qb):
        de = dest_all[qb * 4:(qb + 1) * 4]
        Gk = Gk2[qb % 2]
        for k in range(4):
            P.op("pool", lambda e, k=k, de=de, Gk=Gk: e.indirect_dma_start(
                out=Gk[k].ap, out_offset=None, in_=Yg, in_offset=bass.IndirectOffsetOnAxis(ap=de.ap[:, k:k + 1], axis=0)),
                reads=[("Yg", 0, NE * CAP), de], writes=[Gk[k]], dma=True)
        dma("sp", x2r2[qb % 2].ap, X2[qb * 128:(qb + 1) * 128, :], reads=[("X2", qb * 128, (qb + 1) * 128)], writes=[x2r2[qb % 2]])

    comb_loads(0)
    for qb in range(16):
        if qb + 1 < 16:
            comb_loads(qb + 1)
        Gk, acc, x2r, outt = Gk2[qb % 2], acc2[qb % 2], x2r2[qb % 2], outt2[qb % 2]
        ga = gate_all[qb * 4:(qb + 1) * 4]
        ts("dve", acc.ap, Gk[0].ap, ga.ap[:, 0:1], None, ALU.mult, rd=[Gk[0], ga], wr=[acc])
        for k in range(1, 4):
            stt("dve", acc.ap, Gk[k].ap, ga.ap[:, k:k + 1], acc.ap, ALU.mult, ALU.add, rd=[Gk[k], ga, acc], wr=[acc])
        gf = gfull[qb * 32:(qb + 1) * 32]
        ps = P.psum(P.nb(), 0, 128)
        P.op("pe", lambda e, ps=ps, gf=gf: e.transpose(ps.ap[0:32, :], gf.ap, ident.ap), reads=[gf, ident], writes=[ps])
        evac(gT, gT.ap[0:32, :], ps, ps.ap[0:32, :], "dve")
        for half in range(2):
            ps = P.psum(P.nb())
            mm(ps, gT.ap[0:32, :], bdn.ap[0:32, half * 512:(half + 1) * 512], True, True, [gT, bdn])
            tt("dve", acc.ap[:, half * 512:(half + 1) * 512], acc.ap[:, half * 512:(half + 1) * 512], ps.ap, ALU.add,
               rd=[acc[half * 512:(half + 1) * 512], ps], wr=[acc[half * 512:(half + 1) * 512]])
        stt("dve", acc.ap, x2r.ap, ALPHA, acc.ap, ALU.mult, ALU.add, rd=[x2r, acc], wr=[acc])
        layer_norm(acc, 4, outt, lt4)
        dma("sp", out_d[qb * 128:(qb + 1) * 128, :], outt.ap, reads=[outt])

    return finish()


_CACHE = {}


def _consts():
    c = np.zeros((128, 800), np.float32)
    idx = np.arange(128)
    c[:, 0:128] = np.eye(128)
    c[:, 128:256] = (idx[:, None] <= idx[None, :])
    c[:, 256:384] = (idx[:, None] > idx[None, :])
    c[:, 384:512] = 1.0
    c[:, 512:640] = np.where(idx[:, None] <= idx[None, :], 0.0, NEG)
    c[:, 640:768] = (idx[:, None] < idx[None, :])
    c[:, 768:800] = (np.arange(NE) * CAP + 1)[None, :]
    return c


def make_in_maps(x, mem, w_in, fox_f_bias, mlstm_conv_w, mlstm_i_bias, mlstm_f_bias, fox_norm_g, mlstm_norm_g,
                 w_mix_out, ln1_g, ln1_b, w_xq, w_xk, w_xv, w_xo, ln2_g, ln2_b, w_router, b_router, w_gate_up,
                 b_gate_up, w_down, b_down, ln3_g, ln3_b):
    f = lambda a: np.ascontiguousarray(np.asarray(a, dtype=np.float32))
    x = f(x)
    mem = f(mem)
    gb = np.concatenate([f(fox_f_bias)[0], f(mlstm_i_bias)[0], f(mlstm_f_bias)[0]])
    shared = {
        "consts": _consts(),
        "w_in": f(w_in)[0],
        "gbias": np.ascontiguousarray(np.tile(gb, 32)[None, :]),
        "convT": np.ascontiguousarray(f(mlstm_conv_w)[0].reshape(4, 4, 128).transpose(2, 1, 0).reshape(128, 16)),
        "foxg": f(fox_norm_g),
        "mlg": f(mlstm_norm_g),
        "w_mix": f(w_mix_out)[0],
        "lnp": np.ascontiguousarray(np.concatenate([f(ln1_g)[0], f(ln1_b)[0], f(ln2_g)[0], f(ln2_b)[0], f(ln3_g)[0], f(ln3_b)[0]])[None, :]),
        "w_xq": f(w_xq)[0], "w_xk": f(w_xk)[0], "w_xv": f(w_xv)[0], "w_xo": f(w_xo)[0],
        "w_r": f(w_router)[0], "b_r": f(b_router),
        "w_gu": f(w_gate_up)[0],
        "bguT": np.ascontiguousarray(f(b_gate_up)[0].reshape(NE, 16, 128).transpose(2, 0, 1).reshape(128, NE * 16)),
        "w_dn": f(w_down)[0], "b_dn": f(b_down)[0],
    }
    maps = []
    for c in range(8):
        b, h = c // 2, c % 2
        if h == 0:
            xc = np.concatenate([np.zeros((NTOK, D), np.float32), x[b, :NTOK]], axis=0)
            km = np.full((128, 1), NEG, np.float32)
        else:
            xc = x[b]
            km = np.zeros((128, 1), np.float32)
        m = dict(shared)
        m["xcat"] = np.ascontiguousarray(xc)
        m["memb"] = mem[b]
        m["kmask"] = km
        maps.append(m)
    return maps


def kernel(**inputs):
    if "nc" not in _CACHE:
        _CACHE["nc"] = build_program(False)
    nc = _CACHE["nc"]
    maps = make_in_maps(**inputs)
    res = run_bass_kernel_spmd(nc, maps, core_ids=list(range(8)))
    out = np.zeros((4, 4096, D), np.float32)
    for c in range(8):
        b, h = c // 2, c % 2
        out[b, h * NTOK:(h + 1) * NTOK] = res.results[c]["out"]
    return out
```

```python
import numpy as np
from contextlib import ExitStack
import concourse.bass as bass
import concourse.mybir as mybir
from concourse.bass_utils import run_bass_kernel_spmd

F32 = mybir.dt.float32
BF16 = mybir.dt.bfloat16
I32 = mybir.dt.int32
AF = mybir.ActivationFunctionType
ALU = mybir.AluOpType
AX = mybir.AxisListType
ESZ = {F32: 4, BF16: 2, I32: 4}

D = 1024
NTOK = 2048
NALL = 4096
NE = 32
CAP = 384
ALPHA = 2.0 ** 0.25
NEG = -30000.0
PAGE = 2048
_DBG = {}


class V:
    def __init__(self, ap, key, lo, hi, es):
        self.ap, self.key, self.lo, self.hi, self.es = ap, key, lo, hi, es

    def __getitem__(self, sl):
        a, b = sl.start or 0, sl.stop
        return V(self.ap[:, a:b], self.key, self.lo + a * self.es, self.lo + b * self.es, self.es)

    def p(self, p0, p1):
        return V(self.ap[p0:p1], self.key, self.lo, self.hi, self.es)

    def re(self, pat, **kw):
        return V(self.ap.rearrange(pat, **kw), self.key, self.lo, self.hi, self.es)

    def w(self, ap):
        return V(ap, self.key, self.lo, self.hi, self.es)


class Op:
    __slots__ = ("eng", "fn", "deps", "dma", "inc", "val", "sem", "idx", "prewait")


class Prog:
    ENGS = ["pe", "dve", "act", "pool", "sp"]

    def __init__(self, nc, sbuf_bytes, dma_pool=20):
        self.nc = nc
        self.ops = []
        self.recs = {}
        self.sb_off = 0
        self.sbuf_bytes = sbuf_bytes
        self.dma_pool = dma_pool
        self.bank = 0

    def setup(self, stack):
        nc = self.nc
        self.sb = stack.enter_context(nc.sbuf_tensor("sb_all", [128, self.sbuf_bytes // 4], F32))
        self.ps = stack.enter_context(nc.psum_tensor("ps_all", [128, 4096], F32))

    def alloc(self, ncols, dt=F32):
        nbytes = ((ncols * ESZ[dt] + 63) // 64) * 64
        off = self.sb_off
        self.sb_off += nbytes
        assert self.sb_off <= self.sbuf_bytes, f"SBUF overflow {self.sb_off}"
        ap = self.sb[:, off // 4:(off + nbytes) // 4]
        if dt != F32:
            ap = ap.bitcast(dt)
        ap = ap[:, 0:ncols]
        return V(ap, "S", off, off + ncols * ESZ[dt], ESZ[dt])

    def mark(self):
        return self.sb_off

    def release(self, m):
        self.sb_off = m

    def psum(self, bank, col0=0, ncols=512, dt=F32):
        off = bank * 2048 + col0 * ESZ[dt]
        nb = ncols * ESZ[dt]
        assert col0 * ESZ[dt] + nb <= 2048
        ap = self.ps[:, bank * 512:(bank + 1) * 512]
        if dt != F32:
            ap = ap.bitcast(dt)
        ap = ap[:, col0:col0 + ncols]
        return V(ap, "P", off, off + nb, ESZ[dt])

    nbanks = 8

    def nb(self):
        b = self.bank % self.nbanks
        self.bank = (b + 1) % self.nbanks
        return b

    def _deps(self, idx, eng, isdma, reads, writes):
        deps = set()
        ops = self.ops
        for (key, lo, hi) in reads:
            for pg in range(lo // PAGE, (hi - 1) // PAGE + 1):
                a, b = max(lo, pg * PAGE), min(hi, (pg + 1) * PAGE)
                lst = self.recs.setdefault((key, pg), [])
                found = False
                for r in lst:
                    if r[2] == "w":
                        if r[0] < b and a < r[1]:
                            deps.add(r[3])
                    elif (not found) and r[0] == a and r[1] == b and not isdma:
                        od = ops[r[3]]
                        if od.eng == eng and not od.dma:
                            r[3] = idx
                            found = True
                if not found:
                    lst.append([a, b, "r", idx])
        for (key, lo, hi) in writes:
            for pg in range(lo // PAGE, (hi - 1) // PAGE + 1):
                a, b = max(lo, pg * PAGE), min(hi, (pg + 1) * PAGE)
                lst = self.recs.get((key, pg), [])
                keep = []
                for r in lst:
                    if r[3] != idx and r[0] < b and a < r[1]:
                        deps.add(r[3])
                        if r[0] >= a and r[1] <= b:
                            continue
                    keep.append(r)
                keep.append([a, b, "w", idx])
                self.recs[(key, pg)] = keep
        return deps

    @staticmethod
    def _reg(x):
        if isinstance(x, V):
            if x.key == "P":
                return (x.key, (x.lo // PAGE) * PAGE, ((x.hi - 1) // PAGE + 1) * PAGE)
            return (x.key, x.lo, x.hi)
        return x

    def op(self, eng, fn, reads=(), writes=(), dma=False):
        o = Op()
        o.eng, o.fn, o.dma, o.inc, o.val, o.sem, o.prewait = eng, fn, dma, False, None, None, None
        o.idx = len(self.ops)
        self.ops.append(o)
        deps = self._deps(o.idx, eng, dma, [self._reg(r) for r in reads], [self._reg(w) for w in writes])
        deps.discard(o.idx)
        fin = set()
        for d in deps:
            od = self.ops[d]
            if od.eng == eng and eng == "pe" and not od.dma and not dma:
                continue
            fin.add(d)
        o.deps = fin
        return o

    def emit(self, stack):
        nc = self.nc
        ops = self.ops
        for o in ops:
            for d in o.deps:
                ops[d].inc = True
        esem = {e: stack.enter_context(nc.semaphore("sem_" + e)) for e in self.ENGS}
        dq = ("sp", "pool", "act")
        dsem = {e: [stack.enter_context(nc.semaphore(f"dsem_{e}_{i}")) for i in range(self.dma_pool)] for e in dq}
        cnt = {e: 0 for e in self.ENGS}
        dcnt = {e: 0 for e in dq}
        for o in ops:
            if o.dma:
                k = dcnt[o.eng]
                dcnt[o.eng] += 1
                s = k % self.dma_pool
                o.sem = dsem[o.eng][s]
                o.val = 16 * (k // self.dma_pool + 1)
                o.prewait = (o.sem, o.val - 16) if o.val > 16 else None
            elif o.inc:
                cnt[o.eng] += 1
                o.sem = esem[o.eng]
                o.val = cnt[o.eng]
        block = stack.enter_context(nc.Block())
        engobj = {"pe": "tensor", "dve": "vector", "act": "scalar", "pool": "gpsimd", "sp": "sync"}

        def make(ename):
            mine = [o for o in ops if o.eng == ename]

            def body(e):
                waited = {}

                def wait(sem, val):
                    k = id(sem)
                    if waited.get(k, 0) >= val:
                        return
                    waited[k] = val
                    e.wait_ge(sem, val)

                for o in mine:
                    if o.prewait is not None:
                        wait(*o.prewait)
                    need = {}
                    for d in o.deps:
                        od = ops[d]
                        k = id(od.sem)
                        if k not in need or need[k][1] < od.val:
                            need[k] = (od.sem, od.val)
                    for sem, val in need.values():
                        wait(sem, val)
                    ins = o.fn(e)
                    if o.dma:
                        ins.then_inc(o.sem, 16)
                    elif o.inc:
                        ins.then_inc(o.sem, 1)
                if ename in dsem:
                    last = {}
                    for o in mine:
                        if o.dma:
                            last[id(o.sem)] = (o.sem, o.val)
                    for sem, val in last.values():
                        wait(sem, val)
            return body

        for ename in self.ENGS:
            getattr(block, engobj[ename])(make(ename))


def build_program(dbg=False, stop=99):
    nc = bass.Bass("TRN2", target_bir_lowering=False)

    state = {"dumped": False}

    def dump_mixed():
        if not dbg or state["dumped"] or "mixed" not in state:
            return
        state["dumped"] = True
        mixed_ = state["mixed"]
        mtmp = P.alloc(1024)
        for qb in range(16):
            P.op("dve", lambda e, qb=qb: e.tensor_copy(out=mtmp.ap, in_=mixed_.ap[:, qb * 1024:(qb + 1) * 1024]),
                 reads=[mixed_[qb * 1024:(qb + 1) * 1024]], writes=[mtmp])
            P.op("sp", lambda e, qb=qb: e.dma_start(out=dbg_mixed[qb * 128:(qb + 1) * 128, :], in_=mtmp.ap), reads=[mtmp], dma=True)

    def finish():
        dump_mixed()
        P.emit(st)
        st.close()
        return nc

    def din(name, shape, dt=F32):
        return nc.dram_tensor(name, list(shape), dt, kind="ExternalInput").ap()

    xcat = din("xcat", [NALL, D])
    memb = din("memb", [256, D])
    kmask_d = din("kmask", [128, 1])
    consts_d = din("consts", [128, 800])
    w_in = din("w_in", [D, 3088])
    gbias_d = din("gbias", [1, 512])
    convT_d = din("convT", [128, 16])
    foxg_d = din("foxg", [1, 512])
    mlg_d = din("mlg", [1, 512])
    w_mix = din("w_mix", [D, D])
    ln_d = din("lnp", [1, 6 * D])
    w_xq = din("w_xq", [D, D])
    w_xk = din("w_xk", [D, D])
    w_xv = din("w_xv", [D, D])
    w_xo = din("w_xo", [D, D])
    w_r = din("w_r", [D, NE])
    b_r = din("b_r", [1, NE])
    NEd = NE if stop >= 5 else 1
    w_gu = din("w_gu", [NEd, D, 2 * D])
    bguT_d = din("bguT", [128, NE * 16])
    w_dn = din("w_dn", [NEd, D, D])
    b_dn = din("b_dn", [NE, D])
    out_d = nc.dram_tensor("out", [NTOK, D], F32, kind="ExternalOutput").ap()
    if dbg:
        dbg_mixed = nc.dram_tensor("dbg_mixed", [NTOK, D], F32, kind="ExternalOutput").ap()
        dbg_x1 = nc.dram_tensor("dbg_x1", [NTOK, D], F32, kind="ExternalOutput").ap()
        dbg_x2 = nc.dram_tensor("dbg_x2", [NTOK, D], F32, kind="ExternalOutput").ap()

    st = ExitStack()
    P = Prog(nc, 200 * 1024)
    P.setup(st)
    Xg = nc.dram_tensor("Xg", [NE * CAP, D], BF16).ap()
    Yg = nc.dram_tensor("Yg", [NE * CAP, D], BF16).ap()
    X2 = nc.dram_tensor("X2s", [NTOK, D], F32).ap()

    def wview(w, c0, c1):
        return w.rearrange("(c p) n -> p c n", p=128)[:, :, c0:c1]

    def mm(ps, lhsT, rhs, start, stop, rd):
        P.op("pe", lambda e: e.matmul(ps.ap, lhsT, rhs, start=start, stop=stop), reads=rd, writes=[ps])

    def tr(ps, in_v, in_ap, ident_v, ident_ap):
        P.op("pe", lambda e: e.transpose(ps.ap, in_ap, ident_ap), reads=[in_v, ident_v], writes=[ps])

    ev_rr = [0]

    def evac(out_v, out_ap, in_v, in_ap, eng=None):
        if eng is None:
            eng = ("dve", "act")[ev_rr[0] % 2]
            ev_rr[0] += 1
        if eng == "act":
            P.op("act", lambda e: e.copy(out=out_ap, in_=in_ap), reads=[in_v], writes=[out_v])
        else:
            P.op(eng, lambda e: e.tensor_copy(out=out_ap, in_=in_ap), reads=[in_v], writes=[out_v])

    def dma(eng, out_ap, in_ap, reads=(), writes=()):
        P.op(eng, lambda e: e.dma_start(out=out_ap, in_=in_ap), reads=reads, writes=writes, dma=True)

    def ts(eng, out, in0, s1, s2, op0, op1=None, rd=(), wr=()):
        if s2 is None:
            P.op(eng, lambda e: e.tensor_scalar(out=out, in0=in0, scalar1=s1, scalar2=None, op0=op0), reads=rd, writes=wr)
        else:
            P.op(eng, lambda e: e.tensor_scalar(out=out, in0=in0, scalar1=s1, scalar2=s2, op0=op0, op1=op1), reads=rd, writes=wr)

    def tt(eng, out, in0, in1, op, rd=(), wr=()):
        P.op(eng, lambda e: e.tensor_tensor(out=out, in0=in0, in1=in1, op=op), reads=rd, writes=wr)

    def stt(eng, out, in0, sc, in1, op0, op1, rd=(), wr=(), acc=None):
        if acc is None:
            P.op(eng, lambda e: e.scalar_tensor_tensor(out=out, in0=in0, scalar=sc, in1=in1, op0=op0, op1=op1), reads=rd, writes=wr)
        else:
            P.op(eng, lambda e: e.scalar_tensor_tensor(out=out, in0=in0, scalar=sc, in1=in1, op0=op0, op1=op1, accum_out=acc),
                 reads=rd, writes=wr)

    def act(out, in_, func, rd=(), wr=(), bias=None, scale=None):
        kw = {}
        if bias is not None:
            kw["bias"] = bias
        if scale is not None:
            kw["scale"] = scale
        P.op("act", lambda e: e.activation(out=out, in_=in_, func=func, **kw), reads=rd, writes=wr)

    cst = P.alloc(800)
    dma("sp", cst.ap, consts_d, writes=[cst])
    ident = cst[0:128]
    Uincl = cst[128:256]
    ones = cst[384:512]
    maskneg = cst[512:640]
    Ult = cst[640:768]
    ebase = cst[768:800]
    identb = P.alloc(128, BF16)
    evac(identb, identb.ap, ident, ident.ap, "dve")
    causb = P.alloc(128, BF16)
    evac(causb, causb.ap, Uincl, Uincl.ap, "dve")
    kmask = P.alloc(1)
    dma("sp", kmask.ap, kmask_d, writes=[kmask])
    mixed = P.alloc(16 * 1024, BF16)
    state["mixed"] = mixed
    P.op("pool", lambda e: e.memset(mixed.ap, 0.0), writes=[mixed])
    junk = P.alloc(128)
    dest_all = P.alloc(64, I32)
    gate_all = P.alloc(64)
    gfull = P.alloc(16 * 32)
    lnp_box = [None, 0]
    m_gates = P.mark()
    gbias = P.alloc(512)
    dma("sp", gbias.ap, gbias_d.partition_broadcast(128), writes=[gbias])
    gpre = P.alloc(512)
    glog = P.alloc(512)
    cs = P.alloc(512)
    tot = P.alloc(512)

    def load_x_block(tb, xs):
        for i in range(4):
            r0 = tb * 512 + i * 128
            dma("sp", xs[i].ap, xcat[r0:r0 + 128, :], writes=[xs[i]])

    def transpose_block(xs, xT):
        for c in range(8):
            b = P.nb()
            for i in range(4):
                ps = P.psum(b, i * 128, 128)
                tr(ps, xs[i][c * 128:(c + 1) * 128], xs[i].ap[:, c * 128:(c + 1) * 128], ident, ident.ap)
            psf = P.psum(b)
            evac(xT[c * 512:(c + 1) * 512], xT.ap[:, c * 512:(c + 1) * 512], psf, psf.ap)

    def rstd_from_ss(ss, n, eps_col, outv):
        ts("dve", outv.ap, ss.ap, 1.0 / n, (1e-5, 1e-6)[eps_col], ALU.mult, ALU.add, rd=[ss], wr=[outv])
        act(outv.ap, outv.ap, AF.Sqrt, rd=[outv], wr=[outv])
        P.op("dve", lambda e: e.reciprocal(out=outv.ap, in_=outv.ap), reads=[outv], writes=[outv])

    def layer_norm(y, gi, outv, tmp):
        lnp, base = lnp_box
        g = lnp[(gi - base) * 1024:(gi - base + 1) * 1024]
        b = lnp[(gi - base + 1) * 1024:(gi - base + 2) * 1024]
        s1 = tmp[0:1]
        nm = tmp[1:2]
        ss = tmp[2:3]
        rs = tmp[3:4]
        P.op("dve", lambda e: e.tensor_reduce(out=s1.ap, in_=y.ap, axis=AX.X, op=ALU.add), reads=[y], writes=[s1])
        ts("dve", nm.ap, s1.ap, -1.0 / D, None, ALU.mult, rd=[s1], wr=[nm])
        act(y.ap, y.ap, AF.Identity, rd=[y, nm], wr=[y], bias=nm.ap)
        P.op("act", lambda e: e.activation(out=outv.ap, in_=y.ap, func=AF.Square, accum_out=ss.ap), reads=[y], writes=[outv, ss])
        rstd_from_ss(ss, D, 0, rs)
        stt("dve", outv.ap, y.ap, rs.ap, g.ap, ALU.mult, ALU.mult, rd=[y, rs, g], wr=[outv])
        tt("dve" if gi == 4 else "pool", outv.ap, outv.ap, b.ap, ALU.add, rd=[outv, b], wr=[outv])

    m_phase1 = P.mark()
    zt = P.alloc(2048, BF16)
    P.op("pool", lambda e: e.memset(zt.ap, 0.0), writes=[zt])
    for r in range(0, NE * CAP, 256):
        P.op("act", lambda e, r=r: e.dma_start(out=Xg[r:r + 256, :].rearrange("(p a) n -> p (a n)", a=2), in_=zt.ap),
             reads=[zt], writes=[("Xg", r, r + 256)], dma=True)

    KT = P.alloc(4 * NALL, BF16)
    QT = P.alloc(4 * NTOK, BF16)
    Vaug = P.alloc(32 * 8 * 65, BF16)
    P.op("pool", lambda e: e.memset(Vaug.ap, 1.0), writes=[Vaug])
    m_proj = P.mark()
    wq = P.alloc(8 * 512, BF16)
    wk = P.alloc(8 * 512, BF16)
    wv = P.alloc(8 * 512, BF16)
    wg = P.alloc(8 * 16, BF16)
    dma("pool", wk.re("p (c n) -> p c n", c=8).ap, wview(w_in, 512, 1024), writes=[wk])
    dma("pool", wv.re("p (c n) -> p c n", c=8).ap, wview(w_in, 1024, 1536), writes=[wv])
    dma("pool", wg.re("p (c n) -> p c n", c=8).ap[:, :, 0:8], wview(w_in, 1536, 1544), writes=[wg])
    dma("pool", wg.re("p (c n) -> p c n", c=8).ap[:, :, 8:16], wview(w_in, 2568, 2576), writes=[wg])
    dma("pool", wq.re("p (c n) -> p c n", c=8).ap, wview(w_in, 0, 512), writes=[wq])
    xs_a = [P.alloc(1024) for _ in range(4)]
    xT2 = [P.alloc(8 * 512, BF16) for _ in range(2)]
    for tb in range(8):
        xT = xT2[tb % 2]
        load_x_block(tb, xs_a)
        transpose_block(xs_a, xT)
        for hp in range(4):
            ps = P.psum(P.nb())
            for c in range(8):
                mm(ps, wk.ap[:, c * 512 + hp * 128:c * 512 + (hp + 1) * 128], xT.ap[:, c * 512:(c + 1) * 512],
                   c == 0, c == 7, [wk, xT])
            o = KT[hp * NALL + tb * 512:hp * NALL + (tb + 1) * 512]
            evac(o, o.ap, ps, ps.ap)
        if tb >= 4:
            for hp in range(4):
                ps = P.psum(P.nb())
                for c in range(8):
                    mm(ps, wq.ap[:, c * 512 + hp * 128:c * 512 + (hp + 1) * 128], xT.ap[:, c * 512:(c + 1) * 512],
                       c == 0, c == 7, [wq, xT])
                o = QT[hp * NTOK + (tb - 4) * 512:hp * NTOK + (tb - 3) * 512]
                P.op("act", lambda e, o=o, ps=ps: e.activation(out=o.ap, in_=ps.ap, func=AF.Copy, scale=0.125),
                     reads=[ps], writes=[o])
        for i in range(4):
            blk = tb * 4 + i
            ps = P.psum(P.nb())
            for c in range(8):
                mm(ps, xT.ap[:, c * 512 + i * 128:c * 512 + (i + 1) * 128], wv.ap[:, c * 512:(c + 1) * 512],
                   c == 0, c == 7, [xT, wv])
            o = Vaug[blk * 520:(blk + 1) * 520]
            evac(o, o.ap.rearrange("p (h d) -> p h d", d=65)[:, :, 0:64], ps, ps.ap.rearrange("p (h d) -> p h d", d=64))
            ps2 = P.psum(P.nb(), 0, 16)
            for c in range(8):
                mm(ps2, xT.ap[:, c * 512 + i * 128:c * 512 + (i + 1) * 128], wg.ap[:, c * 16:(c + 1) * 16],
                   c == 0, c == 7, [xT, wg])
            o2 = gpre[blk * 16:(blk + 1) * 16]
            evac(o2, o2.ap, ps2, ps2.ap, "dve")
    P.release(m_proj)
    if stop < 1:
        return finish()

    tt("dve", gpre.ap, gpre.ap, gbias.ap, ALU.add, rd=[gpre, gbias], wr=[gpre])
    act(glog.ap, gpre.ap, AF.Sigmoid, rd=[gpre], wr=[glog])
    act(glog.ap, glog.ap, AF.Ln, rd=[glog], wr=[glog])
    ps = P.psum(P.nb())
    mm(ps, Uincl.ap, glog.ap, True, True, [Uincl, glog])
    evac(cs, cs.ap, ps, ps.ap, "dve")
    ps = P.psum(P.nb())
    mm(ps, ones.ap, glog.ap, True, True, [ones, glog])
    evac(tot, tot.ap, ps, ps.ap, "dve")
    m_fox = P.mark()
    pa = P.alloc(512)
    pb = P.alloc(512)
    evac(pa, pa.ap, tot, tot.ap, "dve")
    cur, oth = pa, pb
    for k in range(5):
        s = 16 * (2 ** k)
        evac(oth[0:s], oth.ap[:, 0:s], cur[0:s], cur.ap[:, 0:s], "dve")
        tt("dve", oth.ap[:, s:512], cur.ap[:, s:512], cur.ap[:, 0:512 - s], ALU.add, rd=[cur], wr=[oth[s:512]])
        cur, oth = oth, cur
    pincl = cur
    pexcl = oth
    tt("dve", pexcl.ap, pincl.ap, tot.ap, ALU.subtract, rd=[pincl, tot], wr=[pexcl])
    negc = P.alloc(512)
    stt("dve", negc.ap, cs.ap, -1.0, pexcl.ap, ALU.mult, ALU.subtract, rd=[cs, pexcl], wr=[negc])
    biasT = P.alloc(4 * 32 * 8)
    for G in range(4):
        for h in range(8):
            col = (16 + 4 * G) * 16 + h
            o = biasT[G * 256:(G + 1) * 256]
            ts("dve", o.ap.rearrange("p (b h) -> p b h", h=8)[:, :, h], negc.ap.rearrange("p (b g) -> p b g", g=16)[:, :, h],
               pexcl.ap[:, col:col + 1], None, ALU.add, rd=[negc, pexcl], wr=[o])
        o = biasT[G * 256:G * 256 + 128]
        ts("dve", o.ap, o.ap, kmask.ap, None, ALU.add, rd=[o, kmask], wr=[o])
    foxg = P.alloc(512)
    dma("sp", foxg.ap, foxg_d.partition_broadcast(128), writes=[foxg])
    if stop < 1.2:
        return finish()

    P.nbanks = 6
    PT3 = [P.alloc(1024, BF16) for _ in range(3)]
    Vs = [P.alloc(128, BF16) for _ in range(8)]
    for t_ in Vs:
        P.op("pool", lambda e, t_=t_: e.memset(t_.ap, 0.0), writes=[t_])
    qz = [[P.alloc(512, BF16) for _ in range(2)] for _ in range(2)]
    for t_ in qz[0] + qz[1]:
        P.op("pool", lambda e, t_=t_: e.memset(t_.ap, 0.0), writes=[t_])
    qz_of = {}
    wexp = P.alloc(4 * 32 * 8)
    ts("dve", wexp.ap, biasT.ap, 80.0, None, ALU.min, rd=[biasT], wr=[wexp])
    act(wexp.ap, wexp.ap, AF.Exp, rd=[wexp], wr=[wexp])
    ot_sb = [P.alloc(512) for _ in range(2)]
    for t_ in ot_sb:
        P.op("pool", lambda e, t_=t_: e.memset(t_.ap, 0.0), writes=[t_])
    rden_t = P.alloc(128)
    ssraw = P.alloc(128)
    rstd_t = P.alloc(128)
    o_sb = [P.alloc(64) for _ in range(2)]
    its = []
    for h in range(8):
        for G in range(4):
            if stop < 1.4 and (h, G) != (0, 0):
                continue
            nk = 20 + 4 * G
            for kp in range(nk // 2):
                its.append((h, G, kp, nk))
    S_of = {}
    vi = [0]

    def fox_S(n):
        h, G, kp, nk = its[n]
        hp, po = h // 2, (h % 2) * 64
        b = (0, 2, 4)[n % 3]
        if (h, G) not in qz_of:
            qt_ = qz[h % 2][len(qz_of) // 2 % 2] if False else qz[h % 2][(h // 2 * 4 + G) % 2]
            qsrc = QT[hp * NTOK + G * 512:hp * NTOK + (G + 1) * 512]
            P.op("dve", lambda e: e.tensor_copy(out=qt_.ap[po:po + 64, :], in_=qsrc.ap[po:po + 64, :]), reads=[qsrc], writes=[qt_])
            qz_of[(h, G)] = qt_
        qt_ = qz_of[(h, G)]
        for t in range(2):
            kb = 2 * kp + t
            sps = P.psum(b + t)
            mm(sps, KT.ap[:, hp * NALL + kb * 128:hp * NALL + (kb + 1) * 128], qt_.ap, True, True, [KT, qt_])
        S_of[n] = b

    def fox_rest(n):
        h, G, kp, nk = its[n]
        b = S_of.pop(n)
        ob = 6
        pt = PT3[n % 3]
        spair = V(P.ps[:, b * 512:(b + 2) * 512], "P", b * 2048, (b + 2) * 2048, 4)
        act(pt.ap, spair.ap, AF.Exp, rd=[spair], wr=[pt])
        for t in range(2):
            kb = 2 * kp + t
            j0 = max(0, kb - (16 + 4 * G))
            col0 = 128 * j0
            ncols = 512 - col0
            ptk = pt[t * 512 + col0:(t + 1) * 512]
            if kb >= 16 + 4 * G:
                pd = pt[t * 512 + col0:t * 512 + col0 + 128]
                tt("pool", pd.ap, pd.ap, causb.ap, ALU.mult, rd=[pd, causb], wr=[pd])
            vs = Vs[vi[0] % 8]
            vi[0] += 1
            bcol = G * 256 + kb * 8 + h
            vsl = Vaug[kb * 520 + h * 65:kb * 520 + (h + 1) * 65]
            ts("dve", vs.ap[:, 0:65], vsl.ap, wexp.ap[:, bcol:bcol + 1], None, ALU.mult, rd=[vsl, wexp], wr=[vs])
            ops_ = P.psum(ob, col0, ncols)
            P.op("pe", lambda e, ops_=ops_, vs=vs, ptk=ptk, kb=kb: e.matmul(ops_.ap, vs.ap, ptk.ap, start=(kb == 0), stop=(kb == nk - 1)),
                 reads=[vs, ptk], writes=[ops_])
        if 2 * kp + 1 != nk - 1 or stop < 1.3:
            return
        opsf = P.psum(ob)
        osb = ot_sb[(h * 4 + G) % 2]
        evac(osb, osb.ap[0:65, :], opsf, opsf.ap[0:65, :], "dve")
        tb_ = 7
        for j in range(4):
            ps = P.psum(tb_, j * 128, 128)
            tr(ps, osb, osb.ap[:, j * 128:(j + 1) * 128], ident, ident.ap)
        for j in range(4):
            qb = G * 4 + j
            c_ = qb * 8 + h
            ps = P.psum(tb_, j * 128, 128)
            P.op("dve", lambda e, ps=ps, c_=c_: e.reciprocal(out=rden_t.ap[:, c_:c_ + 1], in_=ps.ap[:, 64:65]),
                 reads=[ps], writes=[rden_t[c_:c_ + 1]])
            mo = mixed[qb * 1024 + h * 64:qb * 1024 + (h + 1) * 64]
            ts("dve", mo.ap, ps.ap[:, 0:64], rden_t.ap[:, c_:c_ + 1], None, ALU.mult, rd=[ps, rden_t[c_:c_ + 1]], wr=[mo])
            osj = o_sb[j % 2]
            ts("dve", osj.ap, ps.ap[:, 0:64], rden_t.ap[:, c_:c_ + 1], None, ALU.mult, rd=[ps, rden_t[c_:c_ + 1]], wr=[osj])
            stt("dve", junk.ap[:, 0:64], osj.ap, 1.0, osj.ap, ALU.mult, ALU.mult, rd=[osj],
                wr=[junk[0:64], ssraw[c_:c_ + 1]], acc=ssraw.ap[:, c_:c_ + 1])

    LA = 1
    for n in range(len(its) + LA):
        if n < len(its):
            fox_S(n)
        if n >= LA:
            fox_rest(n - LA)
    P.nbanks = 8
    if stop >= 1.4:
        rstd_from_ss(ssraw, 64, 1, rstd_t)
        for qb in range(16):
            for h in range(8):
                c_ = qb * 8 + h
                mo = mixed[qb * 1024 + h * 64:qb * 1024 + (h + 1) * 64]
                stt("dve", mo.ap, mo.ap, rstd_t.ap[:, c_:c_ + 1], foxg.ap[:, h * 64:(h + 1) * 64], ALU.mult, ALU.mult,
                    rd=[mo, rstd_t, foxg], wr=[mo])
    P.release(m_phase1)
    if stop < 2:
        return finish()

    m1b = P.mark()
    wmq = P.alloc(8 * 256, BF16)
    wmk = P.alloc(8 * 256, BF16)
    wmv = P.alloc(8 * 512, BF16)
    wmo = P.alloc(8 * 512, BF16)
    dma("pool", wmk.re("p (c n) -> p c n", c=8).ap, wview(w_in, 1800, 2056), writes=[wmk])
    dma("pool", wmv.re("p (c n) -> p c n", c=8).ap, wview(w_in, 2056, 2568), writes=[wmv])
    dma("pool", wmq.re("p (c n) -> p c n", c=8).ap, wview(w_in, 1544, 1800), writes=[wmq])
    dma("pool", wmo.re("p (c n) -> p c n", c=8).ap, wview(w_in, 2576, 3088), writes=[wmo])
    convT = P.alloc(16)
    dma("sp", convT.ap, convT_d, writes=[convT])
    mlg = P.alloc(512)
    dma("sp", mlg.ap, mlg_d.partition_broadcast(128), writes=[mlg])
    xs_b = [P.alloc(1024) for _ in range(4)]
    xTb = [P.alloc(8 * 512, BF16) for _ in range(2)]
    kbuf = [P.alloc(515) for _ in range(2)]
    qbuf = [P.alloc(515) for _ in range(2)]
    for t_ in kbuf + qbuf:
        P.op("pool", lambda e, t_=t_: e.memset(t_.ap, 0.0), writes=[t_])
    cacc = P.alloc(512)
    ksil2 = [[P.alloc(512, BF16) for _ in range(2)] for _ in range(2)]
    qsil2 = [[P.alloc(512, BF16) for _ in range(2)] for _ in range(2)]
    ktok2 = [[P.alloc(256) for _ in range(4)] for _ in range(2)]
    vaug2 = [[P.alloc(4 * 129, BF16) for _ in range(4)] for _ in range(2)]
    for t_ in vaug2[0] + vaug2[1]:
        P.op("pool", lambda e, t_=t_: e.memset(t_.ap, 1.0), writes=[t_])
    osig2 = [[P.alloc(512) for _ in range(4)] for _ in range(2)]
    Cst = [P.alloc(129) for _ in range(4)]
    Cbf = [P.alloc(129, BF16) for _ in range(4)]
    for t_ in Cst + Cbf:
        P.op("pool", lambda e, t_=t_: e.memset(t_.ap, 0.0), writes=[t_])
    g4s = [P.alloc(32) for _ in range(4)]
    diagb4 = [P.alloc(128) for _ in range(4)]
    DT4 = [P.alloc(128) for _ in range(4)]
    PTm4 = [P.alloc(128, BF16) for _ in range(4)]
    kw0s = [P.alloc(64, BF16) for _ in range(2)]
    kw1s = [P.alloc(128, BF16) for _ in range(2)]
    for t_ in kw1s:
        P.op("pool", lambda e, t_=t_: e.memset(t_.ap, 0.0), writes=[t_])
    hun4 = [P.alloc(129) for _ in range(4)]
    hstore = P.alloc(16 * 128)
    ssm = P.alloc(16)
    rsm = P.alloc(16)
    dn4 = P.alloc(4)

    def conv_silu(buf, psrc, chunk, outv, scale):
        evac(buf[3:515], buf.ap[:, 3:515], psrc, psrc.ap)
        ts("dve", cacc.ap, buf.ap[:, 0:512], convT.ap[:, chunk * 4:chunk * 4 + 1], None, ALU.mult, rd=[buf, convT], wr=[cacc])
        for j in range(1, 4):
            stt("dve", cacc.ap, buf.ap[:, j:j + 512], convT.ap[:, chunk * 4 + j:chunk * 4 + j + 1], cacc.ap, ALU.mult, ALU.add,
                rd=[buf, convT, cacc], wr=[cacc])
        evac(buf[0:3], buf.ap[:, 0:3], buf[512:515], buf.ap[:, 512:515], "pool")
        if scale is None:
            act(outv.ap, cacc.ap, AF.Silu, rd=[cacc], wr=[outv])
        else:
            act(cacc.ap, cacc.ap, AF.Silu, rd=[cacc], wr=[cacc])
            ts("dve", outv.ap, cacc.ap, scale, None, ALU.mult, rd=[cacc], wr=[outv])

    def ml_proj(tb):
        own = tb >= 4
        xT = xTb[tb % 2]
        ksil, qsil, ktok, vaug, osig = ksil2[tb % 2], qsil2[tb % 2], ktok2[tb % 2], vaug2[tb % 2], osig2[tb % 2]
        load_x_block(tb, xs_b)
        transpose_block(xs_b, xT)
        for cc in range(2):
            ps = P.psum(P.nb())
            for c in range(8):
                mm(ps, wmk.ap[:, c * 256 + cc * 128:c * 256 + (cc + 1) * 128], xT.ap[:, c * 512:(c + 1) * 512],
                   c == 0, c == 7, [wmk, xT])
            conv_silu(kbuf[cc], ps, 2 + cc, ksil[cc], 0.125)
            if tb >= 3:
                ps = P.psum(P.nb())
                for c in range(8):
                    mm(ps, wmq.ap[:, c * 256 + cc * 128:c * 256 + (cc + 1) * 128], xT.ap[:, c * 512:(c + 1) * 512],
                       c == 0, c == 7, [wmq, xT])
                conv_silu(qbuf[cc], ps, cc, qsil[cc], None)
        for i in range(4):
            for cc in range(2):
                ps = P.psum(P.nb(), 0, 128, BF16)
                tr(ps, ksil[cc][i * 128:(i + 1) * 128], ksil[cc].ap[:, i * 128:(i + 1) * 128], identb, identb.ap)
                o = ktok[i][cc * 128:(cc + 1) * 128]
                evac(o, o.ap, ps, ps.ap)
            ps = P.psum(P.nb())
            for c in range(8):
                mm(ps, xT.ap[:, c * 512 + i * 128:c * 512 + (i + 1) * 128], wmv.ap[:, c * 512:(c + 1) * 512],
                   c == 0, c == 7, [xT, wmv])
            evac(vaug[i], vaug[i].ap.rearrange("p (h d) -> p h d", d=129)[:, :, 0:128], ps,
                 ps.ap.rearrange("p (h d) -> p h d", d=128))
            if own:
                ps = P.psum(P.nb())
                for c in range(8):
                    mm(ps, xT.ap[:, c * 512 + i * 128:c * 512 + (i + 1) * 128], wmo.ap[:, c * 512:(c + 1) * 512],
                       c == 0, c == 7, [xT, wmo])
                act(osig[i].ap, ps.ap, AF.Sigmoid, rd=[ps], wr=[osig[i]])
    def ml_chunks(tb):
        own = tb >= 4
        ksil, qsil, ktok, vaug, osig = ksil2[tb % 2], qsil2[tb % 2], ktok2[tb % 2], vaug2[tb % 2], osig2[tb % 2]
        for i in range(4):
            blk = tb * 4 + i
            qb = blk - 16
            fcol = blk * 16 + 12
            icol = blk * 16 + 8
            g4 = g4s[i]
            tt("dve", g4.ap[:, 16:20], tot.ap[:, fcol:fcol + 4], cs.ap[:, fcol:fcol + 4], ALU.subtract, rd=[tot, cs], wr=[g4[16:20]])
            tt("dve", g4.ap[:, 16:20], g4.ap[:, 16:20], gpre.ap[:, icol:icol + 4], ALU.add, rd=[g4[16:20], gpre], wr=[g4[16:20]])
            act(g4.ap[:, 4:8], g4.ap[:, 16:20], AF.Exp, rd=[g4[16:20]], wr=[g4[4:8]])
            act(g4.ap[:, 8:12], tot.ap[:, fcol:fcol + 4], AF.Exp, rd=[tot], wr=[g4[8:12]])
            if own:
                act(g4.ap[:, 0:4], cs.ap[:, fcol:fcol + 4], AF.Exp, rd=[cs], wr=[g4[0:4]])
                tt("dve", g4.ap[:, 12:16], gpre.ap[:, icol:icol + 4], cs.ap[:, fcol:fcol + 4], ALU.subtract, rd=[gpre, cs], wr=[g4[12:16]])
            H4 = range(4)
            vas = [vaug[i][hh * 129:(hh + 1) * 129] for hh in H4]
            ccs = [hh // 2 for hh in H4]
            pos = [(hh % 2) * 64 for hh in H4]
            if own:
                for hh in H4:
                    ts("dve", diagb4[hh].ap, ident.ap, cs.ap[:, fcol + hh:fcol + hh + 1], None, ALU.mult, rd=[ident, cs], wr=[diagb4[hh]])
                eps4 = []
                for hh in H4:
                    eps_ = P.psum(P.nb(), 0, 128)
                    mm(eps_, ones.ap, diagb4[hh].ap, True, False, [ones, diagb4[hh]])
                    mm(eps_, ident.ap, maskneg.ap, False, True, [ident, maskneg])
                    eps4.append(eps_)
                for hh in H4:
                    act(DT4[hh].ap, eps4[hh].ap, AF.Exp, rd=[eps4[hh], g4[12:16]], wr=[DT4[hh]], bias=g4.ap[:, 12 + hh:13 + hh])
                sps4 = []
                for hh in H4:
                    cc, po = ccs[hh], pos[hh]
                    sps = P.psum(P.nb(), 0, 128)
                    mm(sps, ksil[cc].ap[po:po + 64, i * 128:(i + 1) * 128], qsil[cc].ap[po:po + 64, i * 128:(i + 1) * 128],
                       True, True, [ksil[cc], qsil[cc]])
                    sps4.append(sps)
                for hh in H4:
                    tt("dve", PTm4[hh].ap, DT4[hh].ap, sps4[hh].ap, ALU.mult, rd=[DT4[hh], sps4[hh]], wr=[PTm4[hh]])
                n12 = []
                for hh in H4:
                    cc, po = ccs[hh], pos[hh]
                    n2 = P.psum(P.nb(), 0, 129)
                    mm(n2, qsil[cc].ap[po:po + 64, i * 128:(i + 1) * 128], Cbf[hh].ap[po:po + 64, :], True, True, [qsil[cc], Cbf[hh]])
                    n12.append(n2)
                for hh in H4:
                    ts("dve", hun4[hh].ap, n12[hh].ap, g4.ap[:, hh:hh + 1], None, ALU.mult, rd=[n12[hh], g4[0:4]], wr=[hun4[hh]])
                n11 = []
                for hh in H4:
                    n1 = P.psum(P.nb(), 0, 129)
                    mm(n1, PTm4[hh].ap, vas[hh].ap, True, True, [PTm4[hh], vas[hh]])
                    n11.append(n1)
                for hh in H4:
                    tt("dve", hun4[hh].ap, hun4[hh].ap, n11[hh].ap, ALU.add, rd=[hun4[hh], n11[hh]], wr=[hun4[hh]])
                for hh in H4:
                    u = i * 4 + hh
                    hs = hstore[u * 128:(u + 1) * 128]
                    d1 = dn4[hh:hh + 1]
                    stt("dve", d1.ap, hun4[hh].ap[:, 128:129], -1.0, hun4[hh].ap[:, 128:129], ALU.mult, ALU.max, rd=[hun4[hh]], wr=[d1])
                    ts("dve", d1.ap, d1.ap, 1.0, None, ALU.max, rd=[d1], wr=[d1])
                    P.op("dve", lambda e, d1=d1: e.reciprocal(out=d1.ap, in_=d1.ap), reads=[d1], writes=[d1])
                    ts("dve", hs.ap, hun4[hh].ap[:, 0:128], d1.ap, None, ALU.mult, rd=[hun4[hh], d1], wr=[hs])
                    stt("dve", junk.ap[:, 0:128], hs.ap, 1.0, hs.ap, ALU.mult, ALU.mult, rd=[hs], wr=[junk[0:128], ssm[u:u + 1]],
                        acc=ssm.ap[:, u:u + 1])
            dps4 = []
            for hh in H4:
                po = pos[hh]
                dps = P.psum(P.nb(), 0, 129)
                if po == 0:
                    kw0 = kw0s[hh // 2]
                    ts("dve", kw0.ap, ktok[i].ap[:, hh * 64:(hh + 1) * 64], g4.ap[:, 4 + hh:5 + hh], None, ALU.mult,
                       rd=[ktok[i], g4[4:8]], wr=[kw0])
                    P.op("pe", lambda e, dps=dps, va=vas[hh], kw0=kw0: e.matmul(dps.ap[0:64, :], kw0.ap, va.ap, start=True, stop=True),
                         reads=[kw0, vas[hh]], writes=[dps])
                else:
                    kw1 = kw1s[hh // 2]
                    ts("dve", kw1.ap[:, 64:128], ktok[i].ap[:, hh * 64:(hh + 1) * 64], g4.ap[:, 4 + hh:5 + hh], None, ALU.mult,
                       rd=[ktok[i], g4[4:8]], wr=[kw1])
                    P.op("pe", lambda e, dps=dps, va=vas[hh], kw1=kw1: e.matmul(dps.ap, kw1.ap, va.ap, start=True, stop=True),
                         reads=[kw1, vas[hh]], writes=[dps])
                dps4.append(dps)
            for hh in H4:
                po = pos[hh]
                stt("dve", Cst[hh].ap[po:po + 64, :], Cst[hh].ap[po:po + 64, :], g4.ap[po:po + 64, 8 + hh:9 + hh], dps4[hh].ap[po:po + 64, :],
                    ALU.mult, ALU.add, rd=[Cst[hh], g4[8:12], dps4[hh]], wr=[Cst[hh]])
            for hh in H4:
                po = pos[hh]
                evac(Cbf[hh], Cbf[hh].ap[po:po + 64, :], Cst[hh], Cst[hh].ap[po:po + 64, :], "pool")
        if own:
            rstd_from_ss(ssm, 128, 1, rsm)
            for i in range(4):
                qb = tb * 4 + i - 16
                for hh in range(4):
                    u = i * 4 + hh
                    hs = hstore[u * 128:(u + 1) * 128]
                    stt("dve", hs.ap, hs.ap, rsm.ap[:, u:u + 1], mlg.ap[:, hh * 128:(hh + 1) * 128], ALU.mult, ALU.mult,
                        rd=[hs, rsm, mlg], wr=[hs])
                    mo = mixed[qb * 1024 + 512 + hh * 128:qb * 1024 + 512 + (hh + 1) * 128]
                    tt("pool", mo.ap, hs.ap, osig[i].ap[:, hh * 128:(hh + 1) * 128], ALU.mult, rd=[hs, osig[i]], wr=[mo])
    ml_proj(0)
    for tb in range(8):
        if tb + 1 < 8:
            ml_proj(tb + 1)
        ml_chunks(tb)
    P.release(m1b)
    P.release(m_gates)
    if stop < 3:
        return finish()

    mdm = P.mark()
    dump_mixed()
    P.release(mdm)

    m2 = P.mark()
    lnp2 = P.alloc(4 * 1024)
    dma("sp", lnp2.ap, ln_d[:, 0:4096].partition_broadcast(128), writes=[lnp2])
    lnp_box[0], lnp_box[1] = lnp2, 0
    wmix = P.alloc(8 * 1024, BF16)
    wxq = P.alloc(8 * 1024, BF16)
    wxo = P.alloc(8 * 1024, BF16)
    dma("pool", wmix.re("p (c n) -> p c n", c=8).ap, wview(w_mix, 0, 1024), writes=[wmix])
    wr_sb = P.alloc(8 * 32)
    dma("sp", wr_sb.re("p (c n) -> p c n", c=8).ap, wview(w_r, 0, 32), writes=[wr_sb])
    br_bc = P.alloc(32)
    dma("sp", br_bc.ap, b_r.partition_broadcast(128), writes=[br_bc])
    KmT = P.alloc(8 * 256, BF16)
    Vm = P.alloc(2 * 4 * 257, BF16)
    P.op("pool", lambda e: e.memset(Vm.ap, 1.0), writes=[Vm])
    macc = P.alloc(32)
    P.op("pool", lambda e: e.memset(macc.ap, 0.0), writes=[macc])
    m2a = P.mark()
    wtmp = P.alloc(8 * 1024, BF16)
    mems = [P.alloc(1024) for _ in range(2)]
    memT = P.alloc(8 * 256, BF16)
    for mt in range(2):
        dma("sp", mems[mt].ap, memb[mt * 128:(mt + 1) * 128, :], writes=[mems[mt]])
    for c in range(8):
        b = P.nb()
        for mt in range(2):
            ps = P.psum(b, mt * 128, 128)
            tr(ps, mems[mt][c * 128:(c + 1) * 128], mems[mt].ap[:, c * 128:(c + 1) * 128], ident, ident.ap)
        psf = P.psum(b, 0, 256)
        evac(memT[c * 256:(c + 1) * 256], memT.ap[:, c * 256:(c + 1) * 256], psf, psf.ap)
    dma("pool", wtmp.re("p (c n) -> p c n", c=8).ap, wview(w_xk, 0, 1024), writes=[wtmp])
    for jc in range(8):
        ps = P.psum(P.nb(), 0, 256)
        for c in range(8):
            mm(ps, wtmp.ap[:, c * 1024 + jc * 128:c * 1024 + (jc + 1) * 128], memT.ap[:, c * 256:(c + 1) * 256],
               c == 0, c == 7, [wtmp, memT])
        o = KmT[jc * 256:(jc + 1) * 256]
        evac(o, o.ap, ps, ps.ap)
    dma("pool", wtmp.re("p (c n) -> p c n", c=8).ap, wview(w_xv, 0, 1024), reads=[], writes=[wtmp])
    for mt in range(2):
        for half in range(2):
            ps = P.psum(P.nb())
            for c in range(8):
                mm(ps, memT.ap[:, c * 256 + mt * 128:c * 256 + (mt + 1) * 128], wtmp.ap[:, c * 1024 + half * 512:c * 1024 + (half + 1) * 512],
                   c == 0, c == 7, [memT, wtmp])
            o = Vm[mt * 1028 + half * 514:mt * 1028 + (half + 1) * 514]
            evac(o, o.ap.rearrange("p (h d) -> p h d", d=257)[:, :, 0:256], ps, ps.ap.rearrange("p (h d) -> p h d", d=256))
    P.release(m2a)
    dma("pool", wxq.re("p (c n) -> p c n", c=8).ap, wview(w_xq, 0, 1024), writes=[wxq])
    dma("pool", wxo.re("p (c n) -> p c n", c=8).ap, wview(w_xo, 0, 1024), writes=[wxo])

    mT = P.alloc(8 * 128, BF16)
    x1s = [[P.alloc(1024) for _ in range(4)] for _ in range(2)]
    ytmpA = P.alloc(1024)
    ytmpC = P.alloc(1024)
    ltA = P.alloc(8)
    ltC = P.alloc(8)
    xres = P.alloc(1024)
    x1T = P.alloc(8 * 512, BF16)
    oT = P.alloc(8 * 512, BF16)
    qT = P.alloc(8 * 512, BF16)
    PTx = [P.alloc(512, BF16) for _ in range(2)]
    oatt = [P.alloc(1024, BF16) for _ in range(4)]
    x2 = P.alloc(1024)
    x2b = P.alloc(1024, BF16)
    x2T = ytmpC
    lg = P.alloc(32)
    m8 = P.alloc(8)
    msk = P.alloc(32)
    ex = P.alloc(32)
    rt = P.alloc(8)
    rtB = P.alloc(8)
    Dm = P.alloc(32)
    d8 = P.alloc(8)

    def stageA(grp):
        x1 = x1s[grp % 2]
        for i in range(4):
            qb = grp * 4 + i
            b = P.nb()
            for c in range(8):
                ps = P.psum(b, c * 128, 128, BF16)
                msl = mixed[qb * 1024 + c * 128:qb * 1024 + (c + 1) * 128]
                tr(ps, msl, msl.ap, identb, identb.ap)
            psf = P.psum(b, 0, 1024, BF16)
            evac(mT, mT.ap, psf, psf.ap)
            dma("sp", xres.ap, xcat[NTOK + qb * 128:NTOK + (qb + 1) * 128, :], writes=[xres])
            for half in range(2):
                ps = P.psum(P.nb())
                for c in range(8):
                    mm(ps, mT.ap[:, c * 128:(c + 1) * 128], wmix.ap[:, c * 1024 + half * 512:c * 1024 + (half + 1) * 512],
                       c == 0, c == 7, [mT, wmix])
                stt("dve", ytmpA.ap[:, half * 512:(half + 1) * 512], xres.ap[:, half * 512:(half + 1) * 512], ALPHA, ps.ap,
                    ALU.mult, ALU.add, rd=[xres, ps], wr=[ytmpA[half * 512:(half + 1) * 512]])
            layer_norm(ytmpA, 0, x1[i], ltA)
            if dbg:
                dma("sp", dbg_x1[qb * 128:(qb + 1) * 128, :], x1[i].ap, reads=[x1[i]])

    def stageB1(grp):
        x1 = x1s[grp % 2]
        for c in range(8):
            b = P.nb()
            for i in range(4):
                ps = P.psum(b, i * 128, 128)
                tr(ps, x1[i][c * 128:(c + 1) * 128], x1[i].ap[:, c * 128:(c + 1) * 128], ident, ident.ap)
            psf = P.psum(b)
            evac(x1T[c * 512:(c + 1) * 512], x1T.ap[:, c * 512:(c + 1) * 512], psf, psf.ap)

    def stageB2(grp):
        for jc in range(8):
            ps = P.psum(P.nb())
            for c in range(8):
                mm(ps, wxq.ap[:, c * 1024 + jc * 128:c * 1024 + (jc + 1) * 128], x1T.ap[:, c * 512:(c + 1) * 512],
                   c == 0, c == 7, [wxq, x1T])
            o = qT[jc * 512:(jc + 1) * 512]
            evac(o, o.ap, ps, ps.ap)

    def stageB3(grp):
        for hx in range(4):
            pts = []
            for mt in range(2):
                ps = P.psum(P.nb())
                for k2 in range(2):
                    jc = hx * 2 + k2
                    mm(ps, KmT.ap[:, jc * 256 + mt * 128:jc * 256 + (mt + 1) * 128], qT.ap[:, jc * 512:(jc + 1) * 512],
                       k2 == 0, k2 == 1, [KmT, qT])
                pt = PTx[mt]
                act(pt.ap, ps.ap, AF.Exp, rd=[ps], wr=[pt], scale=1.0 / 16.0)
                pts.append(pt)
            for i in range(4):
                ps = P.psum(P.nb(), 0, 257)
                for mt in range(2):
                    vsl = Vm[mt * 1028 + hx * 257:mt * 1028 + (hx + 1) * 257]
                    mm(ps, pts[mt].ap[:, i * 128:(i + 1) * 128], vsl.ap, mt == 0, mt == 1, [pts[mt], vsl])
                P.op("dve", lambda e, ps=ps: e.reciprocal(out=rtB.ap[:, 0:1], in_=ps.ap[:, 256:257]), reads=[ps], writes=[rtB[0:1]])
                o = oatt[i][hx * 256:(hx + 1) * 256]
                ts("dve", o.ap, ps.ap[:, 0:256], rtB.ap[:, 0:1], None, ALU.mult, rd=[ps, rtB[0:1]], wr=[o])

    def stageB4(grp):
        for c in range(8):
            b = P.nb()
            for i in range(4):
                ps = P.psum(b, i * 128, 128, BF16)
                tr(ps, oatt[i][c * 128:(c + 1) * 128], oatt[i].ap[:, c * 128:(c + 1) * 128], identb, identb.ap)
            psf = P.psum(b, 0, 512, BF16)
            evac(oT[c * 512:(c + 1) * 512], oT.ap[:, c * 512:(c + 1) * 512], psf, psf.ap)

    def stageC1(grp, i):
        x1 = x1s[grp % 2]
        qb = grp * 4 + i
        for half in range(2):
            ps = P.psum(P.nb())
            for c in range(8):
                mm(ps, oT.ap[:, c * 512 + i * 128:c * 512 + (i + 1) * 128], wxo.ap[:, c * 1024 + half * 512:c * 1024 + (half + 1) * 512],
                   c == 0, c == 7, [oT, wxo])
            stt("dve", ytmpC.ap[:, half * 512:(half + 1) * 512], x1[i].ap[:, half * 512:(half + 1) * 512], ALPHA, ps.ap,
                ALU.mult, ALU.add, rd=[x1[i], ps], wr=[ytmpC[half * 512:(half + 1) * 512]])
        layer_norm(ytmpC, 2, x2, ltC)
        dma("sp", X2[qb * 128:(qb + 1) * 128, :], x2.ap, reads=[x2], writes=[("X2", qb * 128, (qb + 1) * 128)])
        if dbg:
            dma("sp", dbg_x2[qb * 128:(qb + 1) * 128, :], x2.ap, reads=[x2])
        evac(x2b, x2b.ap, x2, x2.ap, "pool")

    def stageC2(grp, i):
        qb = grp * 4 + i
        for half in range(2):
            b = P.nb()
            for c4 in range(4):
                c = half * 4 + c4
                ps = P.psum(b, c4 * 128, 128)
                tr(ps, x2[c * 128:(c + 1) * 128], x2.ap[:, c * 128:(c + 1) * 128], ident, ident.ap)
            psf = P.psum(b)
            evac(x2T[half * 512:(half + 1) * 512], x2T.ap[:, half * 512:(half + 1) * 512], psf, psf.ap)
        ps = P.psum(P.nb(), 0, 32)
        for c in range(8):
            mm(ps, x2T.ap[:, c * 128:(c + 1) * 128], wr_sb.ap[:, c * 32:(c + 1) * 32], c == 0, c == 7, [x2T, wr_sb])
        tt("dve", lg.ap, ps.ap, br_bc.ap, ALU.add, rd=[ps, br_bc], wr=[lg])
        P.op("dve", lambda e: e.max(out=m8.ap, in_=lg.ap), reads=[lg], writes=[m8])
        ts("dve", msk.ap, lg.ap, m8.ap[:, 3:4], None, ALU.is_ge, rd=[lg, m8], wr=[msk])
        ts("dve", rt.ap[:, 1:2], m8.ap[:, 0:1], -1.0, None, ALU.mult, rd=[m8], wr=[rt[1:2]])
        act(ex.ap, lg.ap, AF.Exp, rd=[lg, rt[1:2]], wr=[ex], bias=rt.ap[:, 1:2])
        stt("dve", ex.ap, ex.ap, 1.0, msk.ap, ALU.mult, ALU.mult, rd=[ex, msk], wr=[ex, rt[2:3]], acc=rt.ap[:, 2:3])
        P.op("dve", lambda e: e.reciprocal(out=rt.ap[:, 2:3], in_=rt.ap[:, 2:3]), reads=[rt[2:3]], writes=[rt[2:3]])
        gf = gfull[qb * 32:(qb + 1) * 32]
        ts("dve", gf.ap, ex.ap, rt.ap[:, 2:3], None, ALU.mult, rd=[ex, rt[2:3]], wr=[gf])
        ps = P.psum(P.nb(), 0, 32)
        mm(ps, Ult.ap, msk.ap, True, False, [Ult, msk])
        mm(ps, ones.ap, macc.ap, False, True, [ones, macc])
        ts("dve", Dm.ap, ps.ap, float(CAP - 1), None, ALU.min, rd=[ps], wr=[Dm])
        tt("dve", Dm.ap, Dm.ap, ebase.ap, ALU.add, rd=[Dm, ebase], wr=[Dm])
        tt("dve", Dm.ap, Dm.ap, msk.ap, ALU.mult, rd=[Dm, msk], wr=[Dm])
        tt("pool", macc.ap, macc.ap, msk.ap, ALU.add, rd=[macc, msk], wr=[macc])
        P.op("dve", lambda e: e.max(out=d8.ap, in_=Dm.ap), reads=[Dm], writes=[d8])
        de = dest_all[qb * 4:(qb + 1) * 4]
        ts("dve", de.ap, d8.ap[:, 0:4], -1.0, None, ALU.add, rd=[d8], wr=[de])
        for k in range(4):
            ga = gate_all[qb * 4 + k:qb * 4 + k + 1]
            stt("dve", junk.ap[:, 0:32], Dm.ap, d8.ap[:, k:k + 1], gf.ap, ALU.is_equal, ALU.mult, rd=[Dm, d8, gf],
                wr=[junk[0:32], ga], acc=ga.ap)
            P.op("pool", lambda e, k=k, de=de: e.indirect_dma_start(
                out=Xg, out_offset=bass.IndirectOffsetOnAxis(ap=de.ap[:, k:k + 1], axis=0), in_=x2b.ap, in_offset=None),
                reads=[x2b, de], writes=[("Xg", 0, NE * CAP)], dma=True)

    stageA(0)
    stageB1(0)
    stageB2(0)
    stageB3(0)
    stageB4(0)
    for grp in range(4):
        nxt = grp + 1 < 4
        if nxt:
            stageA(grp + 1)
        Bs = [stageB1, stageB2, stageB3, stageB4]
        for i in range(4):
            stageC1(grp, i)
            if nxt:
                Bs[i](grp + 1)
            stageC2(grp, i)
    P.release(m2)
    if stop < 4:
        return finish()

    m3 = P.mark()
    bguT = P.alloc(NE * 16)
    dma("sp", bguT.ap, bguT_d, writes=[bguT])
    bguS = P.alloc(NE * 16)
    ts("dve", bguS.ap, bguT.ap, 1.0 / 1.702, None, ALU.mult, rd=[bguT], wr=[bguS])
    wgu2 = [P.alloc(8 * 2048, BF16) for _ in range(2)]
    wd2 = [P.alloc(8 * 1024, BF16) for _ in range(2)]
    Xe2 = [[P.alloc(1024, BF16) for _ in range(3)] for _ in range(2)]
    XeT2 = [P.alloc(8 * CAP, BF16) for _ in range(2)]
    hidT = P.alloc(8 * CAP, BF16)
    gt2 = [P.alloc(CAP) for _ in range(2)]
    sg2 = [P.alloc(CAP) for _ in range(2)]
    ln2 = [P.alloc(CAP) for _ in range(2)]
    Ysb = [P.alloc(1024, BF16) for _ in range(2)]

    def load_wgu(en):
        if en >= NE:
            return
        wgu_ = wgu2[en % 2]
        for c4 in range(4):
            sl_ = wgu_[c4 * 4096:(c4 + 1) * 4096]
            dma("pool", sl_.ap.rearrange("p (c n) -> p c n", c=2),
                w_gu[en, c4 * 256:(c4 + 1) * 256, :].rearrange("(c p) n -> p c n", p=128), writes=[sl_])

    def load_wd(en):
        if en >= NE:
            return
        wd_ = wd2[en % 2]
        for c4 in range(2):
            sl_ = wd_[c4 * 4096:(c4 + 1) * 4096]
            dma("pool", sl_.ap.rearrange("p (c n) -> p c n", c=4),
                w_dn[en, c4 * 512:(c4 + 1) * 512, :].rearrange("(c p) n -> p c n", p=128), writes=[sl_])

    def load_xe(en):
        if en >= NE:
            return
        for s in range(3):
            r0 = en * CAP + s * 128
            dma("sp", Xe2[en % 2][s].ap, Xg[r0:r0 + 128, :], reads=[("Xg", r0, r0 + 128)], writes=[Xe2[en % 2][s]])

    def xe_transposes(en):
        if en >= NE:
            return
        Xe, XeT = Xe2[en % 2], XeT2[en % 2]
        for c in range(8):
            b = P.nb()
            for s in range(3):
                ps = P.psum(b, s * 128, 128, BF16)
                tr(ps, Xe[s][c * 128:(c + 1) * 128], Xe[s].ap[:, c * 128:(c + 1) * 128], identb, identb.ap)
            psf = P.psum(b, 0, CAP, BF16)
            evac(XeT[c * CAP:(c + 1) * CAP], XeT.ap[:, c * CAP:(c + 1) * CAP], psf, psf.ap)

    load_wgu(0)
    load_wd(0)
    load_wgu(1)
    load_wd(1)
    load_xe(0)
    xe_transposes(0)
    yi = 0
    for ex_ in range(NE):
        wgu = wgu2[ex_ % 2]
        wd = wd2[ex_ % 2]
        XeT = XeT2[ex_ % 2]
        load_xe(ex_ + 1)
        for g in range(8):
            psg = P.psum(P.nb(), 0, CAP)
            for c in range(8):
                mm(psg, wgu.ap[:, c * 2048 + g * 128:c * 2048 + (g + 1) * 128], XeT.ap[:, c * CAP:(c + 1) * CAP],
                   c == 0, c == 7, [wgu[c * 2048:(c + 1) * 2048], XeT])
            psl = P.psum(P.nb(), 0, CAP)
            for c in range(8):
                mm(psl, wgu.ap[:, c * 2048 + 1024 + g * 128:c * 2048 + 1024 + (g + 1) * 128], XeT.ap[:, c * CAP:(c + 1) * CAP],
                   c == 0, c == 7, [wgu[c * 2048:(c + 1) * 2048], XeT])
            gt, sg, ln = gt2[g % 2], sg2[g % 2], ln2[g % 2]
            bg = bguT[ex_ * 16 + g:ex_ * 16 + g + 1]
            bl = bguT[ex_ * 16 + 8 + g:ex_ * 16 + 8 + g + 1]
            ts("dve", gt.ap, psg.ap, bg.ap, 7.0, ALU.add, ALU.min, rd=[psg, bg], wr=[gt])
            act(sg.ap, gt.ap, AF.Silu, rd=[gt], wr=[sg], scale=1.702)
            bls = bguS[ex_ * 16 + 8 + g:ex_ * 16 + 8 + g + 1]
            act(ln.ap, psl.ap, AF.Identity, rd=[psl, bls], wr=[ln], bias=bls.ap, scale=1.0 / 1.702)
            ts("dve", ln.ap, ln.ap, 7.0 / 1.702, -7.0 / 1.702, ALU.min, ALU.max, rd=[ln], wr=[ln])
            o = hidT[g * CAP:(g + 1) * CAP]
            stt("dve", o.ap, ln.ap, 1.0 / 1.702, sg.ap, ALU.add, ALU.mult, rd=[ln, sg], wr=[o])
        load_wgu(ex_ + 2)
        xe_transposes(ex_ + 1)
        for s in range(3):
            ysb = Ysb[yi % 2]
            yi += 1
            for half in range(2):
                ps = P.psum(P.nb())
                for g in range(8):
                    mm(ps, hidT.ap[:, g * CAP + s * 128:g * CAP + (s + 1) * 128], wd.ap[:, g * 1024 + half * 512:g * 1024 + (half + 1) * 512],
                       g == 0, g == 7, [hidT, wd[g * 1024:(g + 1) * 1024]])
                evac(ysb[half * 512:(half + 1) * 512], ysb.ap[:, half * 512:(half + 1) * 512], ps, ps.ap)
            r0 = ex_ * CAP + s * 128
            dma("sp", Yg[r0:r0 + 128, :], ysb.ap, reads=[ysb], writes=[("Yg", r0, r0 + 128)])
        load_wd(ex_ + 2)
    P.release(m3)
    if stop < 5:
        return finish()

    lnp4 = P.alloc(2 * 1024)
    dma("sp", lnp4.ap, ln_d[:, 4096:6144].partition_broadcast(128), writes=[lnp4])
    lnp_box[0], lnp_box[1] = lnp4, 4
    bdn = P.alloc(1024)
    dma("sp", bdn.ap[0:32, :], b_dn, writes=[bdn])
    Gk2 = [[P.alloc(1024, BF16) for _ in range(4)] for _ in range(2)]
    acc2 = [P.alloc(1024) for _ in range(2)]
    x2r2 = [P.alloc(1024) for _ in range(2)]
    gT = P.alloc(128)
    outt2 = [P.alloc(1024) for _ in range(2)]
    lt4 = P.alloc(8)

    def comb_loads(qb):
        de = dest_all[qb * 4:(qb + 1) * 4]
        Gk = Gk2[qb % 2]
        for k in range(4):
            P.op("pool", lambda e, k=k, de=de, Gk=Gk: e.indirect_dma_start(
                out=Gk[k].ap, out_offset=None, in_=Yg, in_offset=bass.IndirectOffsetOnAxis(ap=de.ap[:, k:k + 1], axis=0)),
                reads=[("Yg", 0, NE * CAP), de], writes=[Gk[k]], dma=True)
        dma("sp", x2r2[qb % 2].ap, X2[qb * 128:(qb + 1) * 128, :], reads=[("X2", qb * 128, (qb + 1) * 128)], writes=[x2r2[qb % 2]])

    comb_loads(0)
    for qb in range(16):
        if qb + 1 < 16:
            comb_loads(qb + 1)
        Gk, acc, x2r, outt = Gk2[qb % 2], acc2[qb % 2], x2r2[qb % 2], outt2[qb % 2]
        ga = gate_all[qb * 4:(qb + 1) * 4]
        ts("dve", acc.ap, Gk[0].ap, ga.ap[:, 0:1], None, ALU.mult, rd=[Gk[0], ga], wr=[acc])
        for k in range(1, 4):
            stt("dve", acc.ap, Gk[k].ap, ga.ap[:, k:k + 1], acc.ap, ALU.mult, ALU.add, rd=[Gk[k], ga, acc], wr=[acc])
        gf = gfull[qb * 32:(qb + 1) * 32]
        ps = P.psum(P.nb(), 0, 128)
        P.op("pe", lambda e, ps=ps, gf=gf: e.transpose(ps.ap[0:32, :], gf.ap, ident.ap), reads=[gf, ident], writes=[ps])
        evac(gT, gT.ap[0:32, :], ps, ps.ap[0:32, :], "dve")
        for half in range(2):
            ps = P.psum(P.nb())
            mm(ps, gT.ap[0:32, :], bdn.ap[0:32, half * 512:(half + 1) * 512], True, True, [gT, bdn])
            tt("dve", acc.ap[:, half * 512:(half + 1) * 512], acc.ap[:, half * 512:(half + 1) * 512], ps.ap, ALU.add,
               rd=[acc[half * 512:(half + 1) * 512], ps], wr=[acc[half * 512:(half + 1) * 512]])
        stt("dve", acc.ap, x2r.ap, ALPHA, acc.ap, ALU.mult, ALU.add, rd=[x2r, acc], wr=[acc])
        layer_norm(acc, 4, outt, lt4)
        dma("sp", out_d[qb * 128:(qb + 1) * 128, :], outt.ap, reads=[outt])

    return finish()


_CACHE = {}


def _consts():
    c = np.zeros((128, 800), np.float32)
    idx = np.arange(128)
    c[:, 0:128] = np.eye(128)
    c[:, 128:256] = (idx[:, None] <= idx[None, :])
    c[:, 256:384] = (idx[:, None] > idx[None, :])
    c[:, 384:512] = 1.0
    c[:, 512:640] = np.where(idx[:, None] <= idx[None, :], 0.0, NEG)
    c[:, 640:768] = (idx[:, None] < idx[None, :])
    c[:, 768:800] = (np.arange(NE) * CAP + 1)[None, :]
    return c


def make_in_maps(x, mem, w_in, fox_f_bias, mlstm_conv_w, mlstm_i_bias, mlstm_f_bias, fox_norm_g, mlstm_norm_g,
                 w_mix_out, ln1_g, ln1_b, w_xq, w_xk, w_xv, w_xo, ln2_g, ln2_b, w_router, b_router, w_gate_up,
                 b_gate_up, w_down, b_down, ln3_g, ln3_b):
    f = lambda a: np.ascontiguousarray(np.asarray(a, dtype=np.float32))
    x = f(x)
    mem = f(mem)
    gb = np.concatenate([f(fox_f_bias)[0], f(mlstm_i_bias)[0], f(mlstm_f_bias)[0]])
    shared = {
        "consts": _consts(),
        "w_in": f(w_in)[0],
        "gbias": np.ascontiguousarray(np.tile(gb, 32)[None, :]),
        "convT": np.ascontiguousarray(f(mlstm_conv_w)[0].reshape(4, 4, 128).transpose(2, 1, 0).reshape(128, 16)),
        "foxg": f(fox_norm_g),
        "mlg": f(mlstm_norm_g),
        "w_mix": f(w_mix_out)[0],
        "lnp": np.ascontiguousarray(np.concatenate([f(ln1_g)[0], f(ln1_b)[0], f(ln2_g)[0], f(ln2_b)[0], f(ln3_g)[0], f(ln3_b)[0]])[None, :]),
        "w_xq": f(w_xq)[0], "w_xk": f(w_xk)[0], "w_xv": f(w_xv)[0], "w_xo": f(w_xo)[0],
        "w_r": f(w_router)[0], "b_r": f(b_router),
        "w_gu": f(w_gate_up)[0],
        "bguT": np.ascontiguousarray(f(b_gate_up)[0].reshape(NE, 16, 128).transpose(2, 0, 1).reshape(128, NE * 16)),
        "w_dn": f(w_down)[0], "b_dn": f(b_down)[0],
    }
    maps = []
    for c in range(8):
        b, h = c // 2, c % 2
        if h == 0:
            xc = np.concatenate([np.zeros((NTOK, D), np.float32), x[b, :NTOK]], axis=0)
            km = np.full((128, 1), NEG, np.float32)
        else:
            xc = x[b]
            km = np.zeros((128, 1), np.float32)
        m = dict(shared)
        m["xcat"] = np.ascontiguousarray(xc)
        m["memb"] = mem[b]
        m["kmask"] = km
        maps.append(m)
    return maps


def kernel(**inputs):
    if "nc" not in _CACHE:
        _CACHE["nc"] = build_program(False)
    nc = _CACHE["nc"]
    maps = make_in_maps(**inputs)
    res = run_bass_kernel_spmd(nc, maps, core_ids=list(range(8)))
    out = np.zeros((4, 4096, D), np.float32)
    for c in range(8):
        b, h = c // 2, c % 2
        out[b, h * NTOK:(h + 1) * NTOK] = res.results[c]["out"]
    return out
```

```python
import numpy as np
from contextlib import ExitStack
import concourse.bass as bass
import concourse.mybir as mybir
from concourse.bass_utils import run_bass_kernel_spmd

F32 = mybir.dt.float32
BF16 = mybir.dt.bfloat16
I32 = mybir.dt.int32
AF = mybir.ActivationFunctionType
ALU = mybir.AluOpType
AX = mybir.AxisListType
ESZ = {F32: 4, BF16: 2, I32: 4}

D = 1024
NTOK = 2048
NALL = 4096
NE = 32
CAP = 384
ALPHA = 2.0 ** 0.25
NEG = -30000.0
PAGE = 2048
_DBG = {}


class V:
    def __init__(self, ap, key, lo, hi, es):
        self.ap, self.key, self.lo, self.hi, self.es = ap, key, lo, hi, es

    def __getitem__(self, sl):
        a, b = sl.start or 0, sl.stop
        return V(self.ap[:, a:b], self.key, self.lo + a * self.es, self.lo + b * self.es, self.es)

    def p(self, p0, p1):
        return V(self.ap[p0:p1], self.key, self.lo, self.hi, self.es)

    def re(self, pat, **kw):
        return V(self.ap.rearrange(pat, **kw), self.key, self.lo, self.hi, self.es)

    def w(self, ap):
        return V(ap, self.key, self.lo, self.hi, self.es)


class Op:
    __slots__ = ("eng", "fn", "deps", "dma", "inc", "val", "sem", "idx", "prewait")


class Prog:
    ENGS = ["pe", "dve", "act", "pool", "sp"]

    def __init__(self, nc, sbuf_bytes, dma_pool=20):
        self.nc = nc
        self.ops = []
        self.recs = {}
        self.sb_off = 0
        self.sbuf_bytes = sbuf_bytes
        self.dma_pool = dma_pool
        self.bank = 0

    def setup(self, stack):
        nc = self.nc
        self.sb = stack.enter_context(nc.sbuf_tensor("sb_all", [128, self.sbuf_bytes // 4], F32))
        self.ps = stack.enter_context(nc.psum_tensor("ps_all", [128, 4096], F32))

    def alloc(self, ncols, dt=F32):
        nbytes = ((ncols * ESZ[dt] + 63) // 64) * 64
        off = self.sb_off
        self.sb_off += nbytes
        assert self.sb_off <= self.sbuf_bytes, f"SBUF overflow {self.sb_off}"
        ap = self.sb[:, off // 4:(off + nbytes) // 4]
        if dt != F32:
            ap = ap.bitcast(dt)
        ap = ap[:, 0:ncols]
        return V(ap, "S", off, off + ncols * ESZ[dt], ESZ[dt])

    def mark(self):
        return self.sb_off

    def release(self, m):
        self.sb_off = m

    def psum(self, bank, col0=0, ncols=512, dt=F32):
        off = bank * 2048 + col0 * ESZ[dt]
        nb = ncols * ESZ[dt]
        assert col0 * ESZ[dt] + nb <= 2048
        ap = self.ps[:, bank * 512:(bank + 1) * 512]
        if dt != F32:
            ap = ap.bitcast(dt)
        ap = ap[:, col0:col0 + ncols]
        return V(ap, "P", off, off + nb, ESZ[dt])

    nbanks = 8

    def nb(self):
        b = self.bank % self.nbanks
        self.bank = (b + 1) % self.nbanks
        return b

    def _deps(self, idx, eng, isdma, reads, writes):
        deps = set()
        ops = self.ops
        for (key, lo, hi) in reads:
            for pg in range(lo // PAGE, (hi - 1) // PAGE + 1):
                a, b = max(lo, pg * PAGE), min(hi, (pg + 1) * PAGE)
                lst = self.recs.setdefault((key, pg), [])
                found = False
                for r in lst:
                    if r[2] == "w":
                        if r[0] < b and a < r[1]:
                            deps.add(r[3])
                    elif (not found) and r[0] == a and r[1] == b and not isdma:
                        od = ops[r[3]]
                        if od.eng == eng and not od.dma:
                            r[3] = idx
                            found = True
                if not found:
                    lst.append([a, b, "r", idx])
        for (key, lo, hi) in writes:
            for pg in range(lo // PAGE, (hi - 1) // PAGE + 1):
                a, b = max(lo, pg * PAGE), min(hi, (pg + 1) * PAGE)
                lst = self.recs.get((key, pg), [])
                keep = []
                for r in lst:
                    if r[3] != idx and r[0] < b and a < r[1]:
                        deps.add(r[3])
                        if r[0] >= a and r[1] <= b:
                            continue
                    keep.append(r)
                keep.append([a, b, "w", idx])
                self.recs[(key, pg)] = keep
        return deps

    @staticmethod
    def _reg(x):
        if isinstance(x, V):
            if x.key == "P":
                return (x.key, (x.lo // PAGE) * PAGE, ((x.hi - 1) // PAGE + 1) * PAGE)
            return (x.key, x.lo, x.hi)
        return x

    def op(self, eng, fn, reads=(), writes=(), dma=False):
        o = Op()
        o.eng, o.fn, o.dma, o.inc, o.val, o.sem, o.prewait = eng, fn, dma, False, None, None, None
        o.idx = len(self.ops)
        self.ops.append(o)
        deps = self._deps(o.idx, eng, dma, [self._reg(r) for r in reads], [self._reg(w) for w in writes])
        deps.discard(o.idx)
        fin = set()
        for d in deps:
            od = self.ops[d]
            if od.eng == eng and eng == "pe" and not od.dma and not dma:
                continue
            fin.add(d)
        o.deps = fin
        return o

    def emit(self, stack):
        nc = self.nc
        ops = self.ops
        for o in ops:
            for d in o.deps:
                ops[d].inc = True
        esem = {e: stack.enter_context(nc.semaphore("sem_" + e)) for e in self.ENGS}
        dq = ("sp", "pool", "act")
        dsem = {e: [stack.enter_context(nc.semaphore(f"dsem_{e}_{i}")) for i in range(self.dma_pool)] for e in dq}
        cnt = {e: 0 for e in self.ENGS}
        dcnt = {e: 0 for e in dq}
        for o in ops:
            if o.dma:
                k = dcnt[o.eng]
                dcnt[o.eng] += 1
                s = k % self.dma_pool
                o.sem = dsem[o.eng][s]
                o.val = 16 * (k // self.dma_pool + 1)
                o.prewait = (o.sem, o.val - 16) if o.val > 16 else None
            elif o.inc:
                cnt[o.eng] += 1
                o.sem = esem[o.eng]
                o.val = cnt[o.eng]
        block = stack.enter_context(nc.Block())
        engobj = {"pe": "tensor", "dve": "vector", "act": "scalar", "pool": "gpsimd", "sp": "sync"}

        def make(ename):
            mine = [o for o in ops if o.eng == ename]

            def body(e):
                waited = {}

                def wait(sem, val):
                    k = id(sem)
                    if waited.get(k, 0) >= val:
                        return
                    waited[k] = val
                    e.wait_ge(sem, val)

                for o in mine:
                    if o.prewait is not None:
                        wait(*o.prewait)
                    need = {}
                    for d in o.deps:
                        od = ops[d]
                        k = id(od.sem)
                        if k not in need or need[k][1] < od.val:
                            need[k] = (od.sem, od.val)
                    for sem, val in need.values():
                        wait(sem, val)
                    ins = o.fn(e)
                    if o.dma:
                        ins.then_inc(o.sem, 16)
                    elif o.inc:
                        ins.then_inc(o.sem, 1)
                if ename in dsem:
                    last = {}
                    for o in mine:
                        if o.dma:
                            last[id(o.sem)] = (o.sem, o.val)
                    for sem, val in last.values():
                        wait(sem, val)
            return body

        for ename in self.ENGS:
            getattr(block, engobj[ename])(make(ename))


def build_program(dbg=False, stop=99):
    nc = bass.Bass("TRN2", target_bir_lowering=False)

    state = {"dumped": False}

    def dump_mixed():
        if not dbg or state["dumped"] or "mixed" not in state:
            return
        state["dumped"] = True
        mixed_ = state["mixed"]
        mtmp = P.alloc(1024)
        for qb in range(16):
            P.op("dve", lambda e, qb=qb: e.tensor_copy(out=mtmp.ap, in_=mixed_.ap[:, qb * 1024:(qb + 1) * 1024]),
                 reads=[mixed_[qb * 1024:(qb + 1) * 1024]], writes=[mtmp])
            P.op("sp", lambda e, qb=qb: e.dma_start(out=dbg_mixed[qb * 128:(qb + 1) * 128, :], in_=mtmp.ap), reads=[mtmp], dma=True)

    def finish():
        dump_mixed()
        P.emit(st)
        st.close()
        return nc

    def din(name, shape, dt=F32):
        return nc.dram_tensor(name, list(shape), dt, kind="ExternalInput").ap()

    xcat = din("xcat", [NALL, D])
    memb = din("memb", [256, D])
    kmask_d = din("kmask", [128, 1])
    consts_d = din("consts", [128, 800])
    w_in = din("w_in", [D, 3088])
    gbias_d = din("gbias", [1, 512])
    convT_d = din("convT", [128, 16])
    foxg_d = din("foxg", [1, 512])
    mlg_d = din("mlg", [1, 512])
    w_mix = din("w_mix", [D, D])
    ln_d = din("lnp", [1, 6 * D])
    w_xq = din("w_xq", [D, D])
    w_xk = din("w_xk", [D, D])
    w_xv = din("w_xv", [D, D])
    w_xo = din("w_xo", [D, D])
    w_r = din("w_r", [D, NE])
    b_r = din("b_r", [1, NE])
    NEd = NE if stop >= 5 else 1
    w_gu = din("w_gu", [NEd, D, 2 * D])
    bguT_d = din("bguT", [128, NE * 16])
    w_dn = din("w_dn", [NEd, D, D])
    b_dn = din("b_dn", [NE, D])
    out_d = nc.dram_tensor("out", [NTOK, D], F32, kind="ExternalOutput").ap()
    if dbg:
        dbg_mixed = nc.dram_tensor("dbg_mixed", [NTOK, D], F32, kind="ExternalOutput").ap()
        dbg_x1 = nc.dram_tensor("dbg_x1", [NTOK, D], F32, kind="ExternalOutput").ap()
        dbg_x2 = nc.dram_tensor("dbg_x2", [NTOK, D], F32, kind="ExternalOutput").ap()

    st = ExitStack()
    P = Prog(nc, 200 * 1024)
    P.setup(st)
    Xg = nc.dram_tensor("Xg", [NE * CAP, D], BF16).ap()
    Yg = nc.dram_tensor("Yg", [NE * CAP, D], BF16).ap()
    X2 = nc.dram_tensor("X2s", [NTOK, D], F32).ap()

    def wview(w, c0, c1):
        return w.rearrange("(c p) n -> p c n", p=128)[:, :, c0:c1]

    def mm(ps, lhsT, rhs, start, stop, rd):
        P.op("pe", lambda e: e.matmul(ps.ap, lhsT, rhs, start=start, stop=stop), reads=rd, writes=[ps])

    def tr(ps, in_v, in_ap, ident_v, ident_ap):
        P.op("pe", lambda e: e.transpose(ps.ap, in_ap, ident_ap), reads=[in_v, ident_v], writes=[ps])

    ev_rr = [0]

    def evac(out_v, out_ap, in_v, in_ap, eng=None):
        if eng is None:
            eng = ("dve", "act")[ev_rr[0] % 2]
            ev_rr[0] += 1
        if eng == "act":
            P.op("act", lambda e: e.copy(out=out_ap, in_=in_ap), reads=[in_v], writes=[out_v])
        else:
            P.op(eng, lambda e: e.tensor_copy(out=out_ap, in_=in_ap), reads=[in_v], writes=[out_v])

    def dma(eng, out_ap, in_ap, reads=(), writes=()):
        P.op(eng, lambda e: e.dma_start(out=out_ap, in_=in_ap), reads=reads, writes=writes, dma=True)

    def ts(eng, out, in0, s1, s2, op0, op1=None, rd=(), wr=()):
        if s2 is None:
            P.op(eng, lambda e: e.tensor_scalar(out=out, in0=in0, scalar1=s1, scalar2=None, op0=op0), reads=rd, writes=wr)
        else:
            P.op(eng, lambda e: e.tensor_scalar(out=out, in0=in0, scalar1=s1, scalar2=s2, op0=op0, op1=op1), reads=rd, writes=wr)

    def tt(eng, out, in0, in1, op, rd=(), wr=()):
        P.op(eng, lambda e: e.tensor_tensor(out=out, in0=in0, in1=in1, op=op), reads=rd, writes=wr)

    def stt(eng, out, in0, sc, in1, op0, op1, rd=(), wr=(), acc=None):
        if acc is None:
            P.op(eng, lambda e: e.scalar_tensor_tensor(out=out, in0=in0, scalar=sc, in1=in1, op0=op0, op1=op1), reads=rd, writes=wr)
        else:
            P.op(eng, lambda e: e.scalar_tensor_tensor(out=out, in0=in0, scalar=sc, in1=in1, op0=op0, op1=op1, accum_out=acc),
                 reads=rd, writes=wr)

    def act(out, in_, func, rd=(), wr=(), bias=None, scale=None):
        kw = {}
        if bias is not None:
            kw["bias"] = bias
        if scale is not None:
            kw["scale"] = scale
        P.op("act", lambda e: e.activation(out=out, in_=in_, func=func, **kw), reads=rd, writes=wr)

    cst = P.alloc(800)
    dma("sp", cst.ap, consts_d, writes=[cst])
    ident = cst[0:128]
    Uincl = cst[128:256]
    ones = cst[384:512]
    maskneg = cst[512:640]
    Ult = cst[640:768]
    ebase = cst[768:800]
    identb = P.alloc(128, BF16)
    evac(identb, identb.ap, ident, ident.ap, "dve")
    causb = P.alloc(128, BF16)
    evac(causb, causb.ap, Uincl, Uincl.ap, "dve")
    kmask = P.alloc(1)
    dma("sp", kmask.ap, kmask_d, writes=[kmask])
    mixed = P.alloc(16 * 1024, BF16)
    state["mixed"] = mixed
    P.op("pool", lambda e: e.memset(mixed.ap, 0.0), writes=[mixed])
    junk = P.alloc(128)
    dest_all = P.alloc(64, I32)
    gate_all = P.alloc(64)
    gfull = P.alloc(16 * 32)
    lnp_box = [None, 0]
    m_gates = P.mark()
    gbias = P.alloc(512)
    dma("sp", gbias.ap, gbias_d.partition_broadcast(128), writes=[gbias])
    gpre = P.alloc(512)
    glog = P.alloc(512)
    cs = P.alloc(512)
    tot = P.alloc(512)

    def load_x_block(tb, xs):
        for i in range(4):
            r0 = tb * 512 + i * 128
            dma("sp", xs[i].ap, xcat[r0:r0 + 128, :], writes=[xs[i]])

    def transpose_block(xs, xT):
        for c in range(8):
            b = P.nb()
            for i in range(4):
                ps = P.psum(b, i * 128, 128)
                tr(ps, xs[i][c * 128:(c + 1) * 128], xs[i].ap[:, c * 128:(c + 1) * 128], ident, ident.ap)
            psf = P.psum(b)
            evac(xT[c * 512:(c + 1) * 512], xT.ap[:, c * 512:(c + 1) * 512], psf, psf.ap)

    def rstd_from_ss(ss, n, eps_col, outv):
        ts("dve", outv.ap, ss.ap, 1.0 / n, (1e-5, 1e-6)[eps_col], ALU.mult, ALU.add, rd=[ss], wr=[outv])
        act(outv.ap, outv.ap, AF.Sqrt, rd=[outv], wr=[outv])
        P.op("dve", lambda e: e.reciprocal(out=outv.ap, in_=outv.ap), reads=[outv], writes=[outv])

    def layer_norm(y, gi, outv, tmp):
        lnp, base = lnp_box
        g = lnp[(gi - base) * 1024:(gi - base + 1) * 1024]
        b = lnp[(gi - base + 1) * 1024:(gi - base + 2) * 1024]
        s1 = tmp[0:1]
        nm = tmp[1:2]
        ss = tmp[2:3]
        rs = tmp[3:4]
        P.op("dve", lambda e: e.tensor_reduce(out=s1.ap, in_=y.ap, axis=AX.X, op=ALU.add), reads=[y], writes=[s1])
        ts("dve", nm.ap, s1.ap, -1.0 / D, None, ALU.mult, rd=[s1], wr=[nm])
        act(y.ap, y.ap, AF.Identity, rd=[y, nm], wr=[y], bias=nm.ap)
        P.op("act", lambda e: e.activation(out=outv.ap, in_=y.ap, func=AF.Square, accum_out=ss.ap), reads=[y], writes=[outv, ss])
        rstd_from_ss(ss, D, 0, rs)
        stt("dve", outv.ap, y.ap, rs.ap, g.ap, ALU.mult, ALU.mult, rd=[y, rs, g], wr=[outv])
        tt("dve" if gi == 4 else "pool", outv.ap, outv.ap, b.ap, ALU.add, rd=[outv, b], wr=[outv])

    m_phase1 = P.mark()
    zt = P.alloc(2048, BF16)
    P.op("pool", lambda e: e.memset(zt.ap, 0.0), writes=[zt])
    for r in range(0, NE * CAP, 256):
        P.op("act", lambda e, r=r: e.dma_start(out=Xg[r:r + 256, :].rearrange("(p a) n -> p (a n)", a=2), in_=zt.ap),
             reads=[zt], writes=[("Xg", r, r + 256)], dma=True)

    KT = P.alloc(4 * NALL, BF16)
    QT = P.alloc(4 * NTOK, BF16)
    Vaug = P.alloc(32 * 8 * 65, BF16)
    P.op("pool", lambda e: e.memset(Vaug.ap, 1.0), writes=[Vaug])
    m_proj = P.mark()
    wq = P.alloc(8 * 512, BF16)
    wk = P.alloc(8 * 512, BF16)
    wv = P.alloc(8 * 512, BF16)
    wg = P.alloc(8 * 16, BF16)
    dma("pool", wk.re("p (c n) -> p c n", c=8).ap, wview(w_in, 512, 1024), writes=[wk])
    dma("pool", wv.re("p (c n) -> p c n", c=8).ap, wview(w_in, 1024, 1536), writes=[wv])
    dma("pool", wg.re("p (c n) -> p c n", c=8).ap[:, :, 0:8], wview(w_in, 1536, 1544), writes=[wg])
    dma("pool", wg.re("p (c n) -> p c n", c=8).ap[:, :, 8:16], wview(w_in, 2568, 2576), writes=[wg])
    dma("pool", wq.re("p (c n) -> p c n", c=8).ap, wview(w_in, 0, 512), writes=[wq])
    xs_a = [P.alloc(1024) for _ in range(4)]
    xT2 = [P.alloc(8 * 512, BF16) for _ in range(2)]
    for tb in range(8):
        xT = xT2[tb % 2]
        load_x_block(tb, xs_a)
        transpose_block(xs_a, xT)
        for hp in range(4):
            ps = P.psum(P.nb())
            for c in range(8):
                mm(ps, wk.ap[:, c * 512 + hp * 128:c * 512 + (hp + 1) * 128], xT.ap[:, c * 512:(c + 1) * 512],
                   c == 0, c == 7, [wk, xT])
            o = KT[hp * NALL + tb * 512:hp * NALL + (tb + 1) * 512]
            evac(o, o.ap, ps, ps.ap)
        if tb >= 4:
            for hp in range(4):
                ps = P.psum(P.nb())
                for c in range(8):
                    mm(ps, wq.ap[:, c * 512 + hp * 128:c * 512 + (hp + 1) * 128], xT.ap[:, c * 512:(c + 1) * 512],
                       c == 0, c == 7, [wq, xT])
                o = QT[hp * NTOK + (tb - 4) * 512:hp * NTOK + (tb - 3) * 512]
                P.op("act", lambda e, o=o, ps=ps: e.activation(out=o.ap, in_=ps.ap, func=AF.Copy, scale=0.125),
                     reads=[ps], writes=[o])
        for i in range(4):
            blk = tb * 4 + i
            ps = P.psum(P.nb())
            for c in range(8):
                mm(ps, xT.ap[:, c * 512 + i * 128:c * 512 + (i + 1) * 128], wv.ap[:, c * 512:(c + 1) * 512],
                   c == 0, c == 7, [xT, wv])
            o = Vaug[blk * 520:(blk + 1) * 520]
            evac(o, o.ap.rearrange("p (h d) -> p h d", d=65)[:, :, 0:64], ps, ps.ap.rearrange("p (h d) -> p h d", d=64))
            ps2 = P.psum(P.nb(), 0, 16)
            for c in range(8):
                mm(ps2, xT.ap[:, c * 512 + i * 128:c * 512 + (i + 1) * 128], wg.ap[:, c * 16:(c + 1) * 16],
                   c == 0, c == 7, [xT, wg])
            o2 = gpre[blk * 16:(blk + 1) * 16]
            evac(o2, o2.ap, ps2, ps2.ap, "dve")
    P.release(m_proj)
    if stop < 1:
        return finish()

    tt("dve", gpre.ap, gpre.ap, gbias.ap, ALU.add, rd=[gpre, gbias], wr=[gpre])
    act(glog.ap, gpre.ap, AF.Sigmoid, rd=[gpre], wr=[glog])
    act(glog.ap, glog.ap, AF.Ln, rd=[glog], wr=[glog])
    ps = P.psum(P.nb())
    mm(ps, Uincl.ap, glog.ap, True, True, [Uincl, glog])
    evac(cs, cs.ap, ps, ps.ap, "dve")
    ps = P.psum(P.nb())
    mm(ps, ones.ap, glog.ap, True, True, [ones, glog])
    evac(tot, tot.ap, ps, ps.ap, "dve")
    m_fox = P.mark()
    pa = P.alloc(512)
    pb = P.alloc(512)
    evac(pa, pa.ap, tot, tot.ap, "dve")
    cur, oth = pa, pb
    for k in range(5):
        s = 16 * (2 ** k)
        evac(oth[0:s], oth.ap[:, 0:s], cur[0:s], cur.ap[:, 0:s], "dve")
        tt("dve", oth.ap[:, s:512], cur.ap[:, s:512], cur.ap[:, 0:512 - s], ALU.add, rd=[cur], wr=[oth[s:512]])
        cur, oth = oth, cur
    pincl = cur
    pexcl = oth
    tt("dve", pexcl.ap, pincl.ap, tot.ap, ALU.subtract, rd=[pincl, tot], wr=[pexcl])
    negc = P.alloc(512)
    stt("dve", negc.ap, cs.ap, -1.0, pexcl.ap, ALU.mult, ALU.subtract, rd=[cs, pexcl], wr=[negc])
    biasT = P.alloc(4 * 32 * 8)
    for G in range(4):
        for h in range(8):
            col = (16 + 4 * G) * 16 + h
            o = biasT[G * 256:(G + 1) * 256]
            ts("dve", o.ap.rearrange("p (b h) -> p b h", h=8)[:, :, h], negc.ap.rearrange("p (b g) -> p b g", g=16)[:, :, h],
               pexcl.ap[:, col:col + 1], None, ALU.add, rd=[negc, pexcl], wr=[o])
        o = biasT[G * 256:G * 256 + 128]
        ts("dve", o.ap, o.ap, kmask.ap, None, ALU.add, rd=[o, kmask], wr=[o])
    foxg = P.alloc(512)
    dma("sp", foxg.ap, foxg_d.partition_broadcast(128), writes=[foxg])
    if stop < 1.2:
        return finish()

    P.nbanks = 6
    PT3 = [P.alloc(1024, BF16) for _ in range(3)]
    Vs = [P.alloc(128, BF16) for _ in range(8)]
    for t_ in Vs:
        P.op("pool", lambda e, t_=t_: e.memset(t_.ap, 0.0), writes=[t_])
    qz = [[P.alloc(512, BF16) for _ in range(2)] for _ in range(2)]
    for t_ in qz[0] + qz[1]:
        P.op("pool", lambda e, t_=t_: e.memset(t_.ap, 0.0), writes=[t_])
    qz_of = {}
    wexp = P.alloc(4 * 32 * 8)
    ts("dve", wexp.ap, biasT.ap, 80.0, None, ALU.min, rd=[biasT], wr=[wexp])
    act(wexp.ap, wexp.ap, AF.Exp, rd=[wexp], wr=[wexp])
    ot_sb = [P.alloc(512) for _ in range(2)]
    for t_ in ot_sb:
        P.op("pool", lambda e, t_=t_: e.memset(t_.ap, 0.0), writes=[t_])
    rden_t = P.alloc(128)
    ssraw = P.alloc(128)
    rstd_t = P.alloc(128)
    o_sb = [P.alloc(64) for _ in range(2)]
    its = []
    for h in range(8):
        for G in range(4):
            if stop < 1.4 and (h, G) != (0, 0):
                continue
            nk = 20 + 4 * G
            for kp in range(nk // 2):
                its.append((h, G, kp, nk))
    S_of = {}
    vi = [0]

    def fox_S(n):
        h, G, kp, nk = its[n]
        hp, po = h // 2, (h % 2) * 64
        b = (0, 2, 4)[n % 3]
        if (h, G) not in qz_of:
            qt_ = qz[h % 2][len(qz_of) // 2 % 2] if False else qz[h % 2][(h // 2 * 4 + G) % 2]
            qsrc = QT[hp * NTOK + G * 512:hp * NTOK + (G + 1) * 512]
            P.op("dve", lambda e: e.tensor_copy(out=qt_.ap[po:po + 64, :], in_=qsrc.ap[po:po + 64, :]), reads=[qsrc], writes=[qt_])
            qz_of[(h, G)] = qt_
        qt_ = qz_of[(h, G)]
        for t in range(2):
            kb = 2 * kp + t
            sps = P.psum(b + t)
            mm(sps, KT.ap[:, hp * NALL + kb * 128:hp * NALL + (kb + 1) * 128], qt_.ap, True, True, [KT, qt_])
        S_of[n] = b

    def fox_rest(n):
        h, G, kp, nk = its[n]
        b = S_of.pop(n)
        ob = 6
        pt = PT3[n % 3]
        spair = V(P.ps[:, b * 512:(b + 2) * 512], "P", b * 2048, (b + 2) * 2048, 4)
        act(pt.ap, spair.ap, AF.Exp, rd=[spair], wr=[pt])
        for t in range(2):
            kb = 2 * kp + t
            j0 = max(0, kb - (16 + 4 * G))
            col0 = 128 * j0
            ncols = 512 - col0
            ptk = pt[t * 512 + col0:(t + 1) * 512]
            if kb >= 16 + 4 * G:
                pd = pt[t * 512 + col0:t * 512 + col0 + 128]
                tt("pool", pd.ap, pd.ap, causb.ap, ALU.mult, rd=[pd, causb], wr=[pd])
            vs = Vs[vi[0] % 8]
            vi[0] += 1
            bcol = G * 256 + kb * 8 + h
            vsl = Vaug[kb * 520 + h * 65:kb * 520 + (h + 1) * 65]
            ts("dve", vs.ap[:, 0:65], vsl.ap, wexp.ap[:, bcol:bcol + 1], None, ALU.mult, rd=[vsl, wexp], wr=[vs])
            ops_ = P.psum(ob, col0, ncols)
            P.op("pe", lambda e, ops_=ops_, vs=vs, ptk=ptk, kb=kb: e.matmul(ops_.ap, vs.ap, ptk.ap, start=(kb == 0), stop=(kb == nk - 1)),
                 reads=[vs, ptk], writes=[ops_])
        if 2 * kp + 1 != nk - 1 or stop < 1.3:
            return
        opsf = P.psum(ob)
        osb = ot_sb[(h * 4 + G) % 2]
        evac(osb, osb.ap[0:65, :], opsf, opsf.ap[0:65, :], "dve")
        tb_ = 7
        for j in range(4):
            ps = P.psum(tb_, j * 128, 128)
            tr(ps, osb, osb.ap[:, j * 128:(j + 1) * 128], ident, ident.ap)
        for j in range(4):
            qb = G * 4 + j
            c_ = qb * 8 + h
            ps = P.psum(tb_, j * 128, 128)
            P.op("dve", lambda e, ps=ps, c_=c_: e.reciprocal(out=rden_t.ap[:, c_:c_ + 1], in_=ps.ap[:, 64:65]),
                 reads=[ps], writes=[rden_t[c_:c_ + 1]])
            mo = mixed[qb * 1024 + h * 64:qb * 1024 + (h + 1) * 64]
            ts("dve", mo.ap, ps.ap[:, 0:64], rden_t.ap[:, c_:c_ + 1], None, ALU.mult, rd=[ps, rden_t[c_:c_ + 1]], wr=[mo])
            osj = o_sb[j % 2]
            ts("dve", osj.ap, ps.ap[:, 0:64], rden_t.ap[:, c_:c_ + 1], None, ALU.mult, rd=[ps, rden_t[c_:c_ + 1]], wr=[osj])
            stt("dve", junk.ap[:, 0:64], osj.ap, 1.0, osj.ap, ALU.mult, ALU.mult, rd=[osj],
                wr=[junk[0:64], ssraw[c_:c_ + 1]], acc=ssraw.ap[:, c_:c_ + 1])

    LA = 2
    for n in range(len(its) + LA):
        if n < len(its):
            fox_S(n)
        if n >= LA:
            fox_rest(n - LA)
    P.nbanks = 8
    if stop >= 1.4:
        rstd_from_ss(ssraw, 64, 1, rstd_t)
        for qb in range(16):
            for h in range(8):
                c_ = qb * 8 + h
                mo = mixed[qb * 1024 + h * 64:qb * 1024 + (h + 1) * 64]
                stt("dve", mo.ap, mo.ap, rstd_t.ap[:, c_:c_ + 1], foxg.ap[:, h * 64:(h + 1) * 64], ALU.mult, ALU.mult,
                    rd=[mo, rstd_t, foxg], wr=[mo])
    P.release(m_phase1)
    if stop < 2:
        return finish()

    m1b = P.mark()
    wmq = P.alloc(8 * 256, BF16)
    wmk = P.alloc(8 * 256, BF16)
    wmv = P.alloc(8 * 512, BF16)
    wmo = P.alloc(8 * 512, BF16)
    dma("pool", wmk.re("p (c n) -> p c n", c=8).ap, wview(w_in, 1800, 2056), writes=[wmk])
    dma("pool", wmv.re("p (c n) -> p c n", c=8).ap, wview(w_in, 2056, 2568), writes=[wmv])
    dma("pool", wmq.re("p (c n) -> p c n", c=8).ap, wview(w_in, 1544, 1800), writes=[wmq])
    dma("pool", wmo.re("p (c n) -> p c n", c=8).ap, wview(w_in, 2576, 3088), writes=[wmo])
    convT = P.alloc(16)
    dma("sp", convT.ap, convT_d, writes=[convT])
    mlg = P.alloc(512)
    dma("sp", mlg.ap, mlg_d.partition_broadcast(128), writes=[mlg])
    xs_b = [P.alloc(1024) for _ in range(4)]
    xTb = [P.alloc(8 * 512, BF16) for _ in range(2)]
    kbuf = [P.alloc(515) for _ in range(2)]
    qbuf = [P.alloc(515) for _ in range(2)]
    for t_ in kbuf + qbuf:
        P.op("pool", lambda e, t_=t_: e.memset(t_.ap, 0.0), writes=[t_])
    cacc = P.alloc(512)
    ksil2 = [[P.alloc(512, BF16) for _ in range(2)] for _ in range(2)]
    qsil2 = [[P.alloc(512, BF16) for _ in range(2)] for _ in range(2)]
    ktok2 = [[P.alloc(256) for _ in range(4)] for _ in range(2)]
    vaug2 = [[P.alloc(4 * 129, BF16) for _ in range(4)] for _ in range(2)]
    for t_ in vaug2[0] + vaug2[1]:
        P.op("pool", lambda e, t_=t_: e.memset(t_.ap, 1.0), writes=[t_])
    osig2 = [[P.alloc(512) for _ in range(4)] for _ in range(2)]
    Cst = [P.alloc(129) for _ in range(4)]
    Cbf = [P.alloc(129, BF16) for _ in range(4)]
    for t_ in Cst + Cbf:
        P.op("pool", lambda e, t_=t_: e.memset(t_.ap, 0.0), writes=[t_])
    g4s = [P.alloc(32) for _ in range(4)]
    diagb4 = [P.alloc(128) for _ in range(4)]
    DT4 = [P.alloc(128) for _ in range(4)]
    PTm4 = [P.alloc(128, BF16) for _ in range(4)]
    kw0s = [P.alloc(64, BF16) for _ in range(2)]
    kw1s = [P.alloc(128, BF16) for _ in range(2)]
    for t_ in kw1s:
        P.op("pool", lambda e, t_=t_: e.memset(t_.ap, 0.0), writes=[t_])
    hun4 = [P.alloc(129) for _ in range(4)]
    hstore = P.alloc(16 * 128)
    ssm = P.alloc(16)
    rsm = P.alloc(16)
    dn4 = P.alloc(4)

    def conv_silu(buf, psrc, chunk, outv, scale):
        evac(buf[3:515], buf.ap[:, 3:515], psrc, psrc.ap)
        ts("dve", cacc.ap, buf.ap[:, 0:512], convT.ap[:, chunk * 4:chunk * 4 + 1], None, ALU.mult, rd=[buf, convT], wr=[cacc])
        for j in range(1, 4):
            stt("dve", cacc.ap, buf.ap[:, j:j + 512], convT.ap[:, chunk * 4 + j:chunk * 4 + j + 1], cacc.ap, ALU.mult, ALU.add,
                rd=[buf, convT, cacc], wr=[cacc])
        evac(buf[0:3], buf.ap[:, 0:3], buf[512:515], buf.ap[:, 512:515], "pool")
        if scale is None:
            act(outv.ap, cacc.ap, AF.Silu, rd=[cacc], wr=[outv])
        else:
            act(cacc.ap, cacc.ap, AF.Silu, rd=[cacc], wr=[cacc])
            ts("dve", outv.ap, cacc.ap, scale, None, ALU.mult, rd=[cacc], wr=[outv])

    def ml_proj(tb):
        own = tb >= 4
        xT = xTb[tb % 2]
        ksil, qsil, ktok, vaug, osig = ksil2[tb % 2], qsil2[tb % 2], ktok2[tb % 2], vaug2[tb % 2], osig2[tb % 2]
        load_x_block(tb, xs_b)
        transpose_block(xs_b, xT)
        for cc in range(2):
            ps = P.psum(P.nb())
            for c in range(8):
                mm(ps, wmk.ap[:, c * 256 + cc * 128:c * 256 + (cc + 1) * 128], xT.ap[:, c * 512:(c + 1) * 512],
                   c == 0, c == 7, [wmk, xT])
            conv_silu(kbuf[cc], ps, 2 + cc, ksil[cc], 0.125)
            if tb >= 3:
                ps = P.psum(P.nb())
                for c in range(8):
                    mm(ps, wmq.ap[:, c * 256 + cc * 128:c * 256 + (cc + 1) * 128], xT.ap[:, c * 512:(c + 1) * 512],
                       c == 0, c == 7, [wmq, xT])
                conv_silu(qbuf[cc], ps, cc, qsil[cc], None)
        for i in range(4):
            for cc in range(2):
                ps = P.psum(P.nb(), 0, 128, BF16)
                tr(ps, ksil[cc][i * 128:(i + 1) * 128], ksil[cc].ap[:, i * 128:(i + 1) * 128], identb, identb.ap)
                o = ktok[i][cc * 128:(cc + 1) * 128]
                evac(o, o.ap, ps, ps.ap)
            ps = P.psum(P.nb())
            for c in range(8):
                mm(ps, xT.ap[:, c * 512 + i * 128:c * 512 + (i + 1) * 128], wmv.ap[:, c * 512:(c + 1) * 512],
                   c == 0, c == 7, [xT, wmv])
            evac(vaug[i], vaug[i].ap.rearrange("p (h d) -> p h d", d=129)[:, :, 0:128], ps,
                 ps.ap.rearrange("p (h d) -> p h d", d=128))
            if own:
                ps = P.psum(P.nb())
                for c in range(8):
                    mm(ps, xT.ap[:, c * 512 + i * 128:c * 512 + (i + 1) * 128], wmo.ap[:, c * 512:(c + 1) * 512],
                       c == 0, c == 7, [xT, wmo])
                act(osig[i].ap, ps.ap, AF.Sigmoid, rd=[ps], wr=[osig[i]])
    def ml_chunks(tb):
        own = tb >= 4
        ksil, qsil, ktok, vaug, osig = ksil2[tb % 2], qsil2[tb % 2], ktok2[tb % 2], vaug2[tb % 2], osig2[tb % 2]
        for i in range(4):
            blk = tb * 4 + i
            qb = blk - 16
            fcol = blk * 16 + 12
            icol = blk * 16 + 8
            g4 = g4s[i]
            tt("dve", g4.ap[:, 16:20], tot.ap[:, fcol:fcol + 4], cs.ap[:, fcol:fcol + 4], ALU.subtract, rd=[tot, cs], wr=[g4[16:20]])
            tt("dve", g4.ap[:, 16:20], g4.ap[:, 16:20], gpre.ap[:, icol:icol + 4], ALU.add, rd=[g4[16:20], gpre], wr=[g4[16:20]])
            act(g4.ap[:, 4:8], g4.ap[:, 16:20], AF.Exp, rd=[g4[16:20]], wr=[g4[4:8]])
            act(g4.ap[:, 8:12], tot.ap[:, fcol:fcol + 4], AF.Exp, rd=[tot], wr=[g4[8:12]])
            if own:
                act(g4.ap[:, 0:4], cs.ap[:, fcol:fcol + 4], AF.Exp, rd=[cs], wr=[g4[0:4]])
                tt("dve", g4.ap[:, 12:16], gpre.ap[:, icol:icol + 4], cs.ap[:, fcol:fcol + 4], ALU.subtract, rd=[gpre, cs], wr=[g4[12:16]])
            H4 = range(4)
            vas = [vaug[i][hh * 129:(hh + 1) * 129] for hh in H4]
            ccs = [hh // 2 for hh in H4]
            pos = [(hh % 2) * 64 for hh in H4]
            if own:
                for hh in H4:
                    ts("dve", diagb4[hh].ap, ident.ap, cs.ap[:, fcol + hh:fcol + hh + 1], None, ALU.mult, rd=[ident, cs], wr=[diagb4[hh]])
                eps4 = []
                for hh in H4:
                    eps_ = P.psum(P.nb(), 0, 128)
                    mm(eps_, ones.ap, diagb4[hh].ap, True, False, [ones, diagb4[hh]])
                    mm(eps_, ident.ap, maskneg.ap, False, True, [ident, maskneg])
                    eps4.append(eps_)
                for hh in H4:
                    act(DT4[hh].ap, eps4[hh].ap, AF.Exp, rd=[eps4[hh], g4[12:16]], wr=[DT4[hh]], bias=g4.ap[:, 12 + hh:13 + hh])
                sps4 = []
                for hh in H4:
                    cc, po = ccs[hh], pos[hh]
                    sps = P.psum(P.nb(), 0, 128)
                    mm(sps, ksil[cc].ap[po:po + 64, i * 128:(i + 1) * 128], qsil[cc].ap[po:po + 64, i * 128:(i + 1) * 128],
                       True, True, [ksil[cc], qsil[cc]])
                    sps4.append(sps)
                for hh in H4:
                    tt("dve", PTm4[hh].ap, DT4[hh].ap, sps4[hh].ap, ALU.mult, rd=[DT4[hh], sps4[hh]], wr=[PTm4[hh]])
                n12 = []
                for hh in H4:
                    cc, po = ccs[hh], pos[hh]
                    n2 = P.psum(P.nb(), 0, 129)
                    mm(n2, qsil[cc].ap[po:po + 64, i * 128:(i + 1) * 128], Cbf[hh].ap[po:po + 64, :], True, True, [qsil[cc], Cbf[hh]])
                    n12.append(n2)
                for hh in H4:
                    ts("dve", hun4[hh].ap, n12[hh].ap, g4.ap[:, hh:hh + 1], None, ALU.mult, rd=[n12[hh], g4[0:4]], wr=[hun4[hh]])
                n11 = []
                for hh in H4:
                    n1 = P.psum(P.nb(), 0, 129)
                    mm(n1, PTm4[hh].ap, vas[hh].ap, True, True, [PTm4[hh], vas[hh]])
                    n11.append(n1)
                for hh in H4:
                    tt("dve", hun4[hh].ap, hun4[hh].ap, n11[hh].ap, ALU.add, rd=[hun4[hh], n11[hh]], wr=[hun4[hh]])
                for hh in H4:
                    u = i * 4 + hh
                    hs = hstore[u * 128:(u + 1) * 128]
                    d1 = dn4[hh:hh + 1]
                    stt("dve", d1.ap, hun4[hh].ap[:, 128:129], -1.0, hun4[hh].ap[:, 128:129], ALU.mult, ALU.max, rd=[hun4[hh]], wr=[d1])
                    ts("dve", d1.ap, d1.ap, 1.0, None, ALU.max, rd=[d1], wr=[d1])
                    P.op("dve", lambda e, d1=d1: e.reciprocal(out=d1.ap, in_=d1.ap), reads=[d1], writes=[d1])
                    ts("dve", hs.ap, hun4[hh].ap[:, 0:128], d1.ap, None, ALU.mult, rd=[hun4[hh], d1], wr=[hs])
                    stt("dve", junk.ap[:, 0:128], hs.ap, 1.0, hs.ap, ALU.mult, ALU.mult, rd=[hs], wr=[junk[0:128], ssm[u:u + 1]],
                        acc=ssm.ap[:, u:u + 1])
            dps4 = []
            for hh in H4:
                po = pos[hh]
                dps = P.psum(P.nb(), 0, 129)
                if po == 0:
                    kw0 = kw0s[hh // 2]
                    ts("dve", kw0.ap, ktok[i].ap[:, hh * 64:(hh + 1) * 64], g4.ap[:, 4 + hh:5 + hh], None, ALU.mult,
                       rd=[ktok[i], g4[4:8]], wr=[kw0])
                    P.op("pe", lambda e, dps=dps, va=vas[hh], kw0=kw0: e.matmul(dps.ap[0:64, :], kw0.ap, va.ap, start=True, stop=True),
                         reads=[kw0, vas[hh]], writes=[dps])
                else:
                    kw1 = kw1s[hh // 2]
                    ts("dve", kw1.ap[:, 64:128], ktok[i].ap[:, hh * 64:(hh + 1) * 64], g4.ap[:, 4 + hh:5 + hh], None, ALU.mult,
                       rd=[ktok[i], g4[4:8]], wr=[kw1])
                    P.op("pe", lambda e, dps=dps, va=vas[hh], kw1=kw1: e.matmul(dps.ap, kw1.ap, va.ap, start=True, stop=True),
                         reads=[kw1, vas[hh]], writes=[dps])
                dps4.append(dps)
            for hh in H4:
                po = pos[hh]
                stt("dve", Cst[hh].ap[po:po + 64, :], Cst[hh].ap[po:po + 64, :], g4.ap[po:po + 64, 8 + hh:9 + hh], dps4[hh].ap[po:po + 64, :],
                    ALU.mult, ALU.add, rd=[Cst[hh], g4[8:12], dps4[hh]], wr=[Cst[hh]])
            for hh in H4:
                po = pos[hh]
                evac(Cbf[hh], Cbf[hh].ap[po:po + 64, :], Cst[hh], Cst[hh].ap[po:po + 64, :], "pool")
        if own:
            rstd_from_ss(ssm, 128, 1, rsm)
            for i in range(4):
                qb = tb * 4 + i - 16
                for hh in range(4):
                    u = i * 4 + hh
                    hs = hstore[u * 128:(u + 1) * 128]
                    stt("dve", hs.ap, hs.ap, rsm.ap[:, u:u + 1], mlg.ap[:, hh * 128:(hh + 1) * 128], ALU.mult, ALU.mult,
                        rd=[hs, rsm, mlg], wr=[hs])
                    mo = mixed[qb * 1024 + 512 + hh * 128:qb * 1024 + 512 + (hh + 1) * 128]
                    tt("pool", mo.ap, hs.ap, osig[i].ap[:, hh * 128:(hh + 1) * 128], ALU.mult, rd=[hs, osig[i]], wr=[mo])
    ml_proj(0)
    for tb in range(8):
        if tb + 1 < 8:
            ml_proj(tb + 1)
        ml_chunks(tb)
    P.release(m1b)
    P.release(m_gates)
    if stop < 3:
        return finish()

    mdm = P.mark()
    dump_mixed()
    P.release(mdm)

    m2 = P.mark()
    lnp2 = P.alloc(4 * 1024)
    dma("sp", lnp2.ap, ln_d[:, 0:4096].partition_broadcast(128), writes=[lnp2])
    lnp_box[0], lnp_box[1] = lnp2, 0
    wmix = P.alloc(8 * 1024, BF16)
    wxq = P.alloc(8 * 1024, BF16)
    wxo = P.alloc(8 * 1024, BF16)
    dma("pool", wmix.re("p (c n) -> p c n", c=8).ap, wview(w_mix, 0, 1024), writes=[wmix])
    wr_sb = P.alloc(8 * 32)
    dma("sp", wr_sb.re("p (c n) -> p c n", c=8).ap, wview(w_r, 0, 32), writes=[wr_sb])
    br_bc = P.alloc(32)
    dma("sp", br_bc.ap, b_r.partition_broadcast(128), writes=[br_bc])
    KmT = P.alloc(8 * 256, BF16)
    Vm = P.alloc(2 * 4 * 257, BF16)
    P.op("pool", lambda e: e.memset(Vm.ap, 1.0), writes=[Vm])
    macc = P.alloc(32)
    P.op("pool", lambda e: e.memset(macc.ap, 0.0), writes=[macc])
    m2a = P.mark()
    wtmp = P.alloc(8 * 1024, BF16)
    mems = [P.alloc(1024) for _ in range(2)]
    memT = P.alloc(8 * 256, BF16)
    for mt in range(2):
        dma("sp", mems[mt].ap, memb[mt * 128:(mt + 1) * 128, :], writes=[mems[mt]])
    for c in range(8):
        b = P.nb()
        for mt in range(2):
            ps = P.psum(b, mt * 128, 128)
            tr(ps, mems[mt][c * 128:(c + 1) * 128], mems[mt].ap[:, c * 128:(c + 1) * 128], ident, ident.ap)
        psf = P.psum(b, 0, 256)
        evac(memT[c * 256:(c + 1) * 256], memT.ap[:, c * 256:(c + 1) * 256], psf, psf.ap)
    dma("pool", wtmp.re("p (c n) -> p c n", c=8).ap, wview(w_xk, 0, 1024), writes=[wtmp])
    for jc in range(8):
        ps = P.psum(P.nb(), 0, 256)
        for c in range(8):
            mm(ps, wtmp.ap[:, c * 1024 + jc * 128:c * 1024 + (jc + 1) * 128], memT.ap[:, c * 256:(c + 1) * 256],
               c == 0, c == 7, [wtmp, memT])
        o = KmT[jc * 256:(jc + 1) * 256]
        evac(o, o.ap, ps, ps.ap)
    dma("pool", wtmp.re("p (c n) -> p c n", c=8).ap, wview(w_xv, 0, 1024), reads=[], writes=[wtmp])
    for mt in range(2):
        for half in range(2):
            ps = P.psum(P.nb())
            for c in range(8):
                mm(ps, memT.ap[:, c * 256 + mt * 128:c * 256 + (mt + 1) * 128], wtmp.ap[:, c * 1024 + half * 512:c * 1024 + (half + 1) * 512],
                   c == 0, c == 7, [memT, wtmp])
            o = Vm[mt * 1028 + half * 514:mt * 1028 + (half + 1) * 514]
            evac(o, o.ap.rearrange("p (h d) -> p h d", d=257)[:, :, 0:256], ps, ps.ap.rearrange("p (h d) -> p h d", d=256))
    P.release(m2a)
    dma("pool", wxq.re("p (c n) -> p c n", c=8).ap, wview(w_xq, 0, 1024), writes=[wxq])
    dma("pool", wxo.re("p (c n) -> p c n", c=8).ap, wview(w_xo, 0, 1024), writes=[wxo])

    mT = P.alloc(8 * 128, BF16)
    x1s = [[P.alloc(1024) for _ in range(4)] for _ in range(2)]
    ytmpA = P.alloc(1024)
    ytmpC = P.alloc(1024)
    ltA = P.alloc(8)
    ltC = P.alloc(8)
    xres = P.alloc(1024)
    x1T = P.alloc(8 * 512, BF16)
    oT = P.alloc(8 * 512, BF16)
    qT = P.alloc(8 * 512, BF16)
    PTx = [P.alloc(512, BF16) for _ in range(2)]
    oatt = [P.alloc(1024, BF16) for _ in range(4)]
    x2 = P.alloc(1024)
    x2b = P.alloc(1024, BF16)
    x2T = ytmpC
    lg = P.alloc(32)
    m8 = P.alloc(8)
    msk = P.alloc(32)
    ex = P.alloc(32)
    rt = P.alloc(8)
    rtB = P.alloc(8)
    Dm = P.alloc(32)
    d8 = P.alloc(8)

    def stageA(grp):
        x1 = x1s[grp % 2]
        for i in range(4):
            qb = grp * 4 + i
            b = P.nb()
            for c in range(8):
                ps = P.psum(b, c * 128, 128, BF16)
                msl = mixed[qb * 1024 + c * 128:qb * 1024 + (c + 1) * 128]
                tr(ps, msl, msl.ap, identb, identb.ap)
            psf = P.psum(b, 0, 1024, BF16)
            evac(mT, mT.ap, psf, psf.ap)
            dma("sp", xres.ap, xcat[NTOK + qb * 128:NTOK + (qb + 1) * 128, :], writes=[xres])
            for half in range(2):
                ps = P.psum(P.nb())
                for c in range(8):
                    mm(ps, mT.ap[:, c * 128:(c + 1) * 128], wmix.ap[:, c * 1024 + half * 512:c * 1024 + (half + 1) * 512],
                       c == 0, c == 7, [mT, wmix])
                stt("dve", ytmpA.ap[:, half * 512:(half + 1) * 512], xres.ap[:, half * 512:(half + 1) * 512], ALPHA, ps.ap,
                    ALU.mult, ALU.add, rd=[xres, ps], wr=[ytmpA[half * 512:(half + 1) * 512]])
            layer_norm(ytmpA, 0, x1[i], ltA)
            if dbg:
                dma("sp", dbg_x1[qb * 128:(qb + 1) * 128, :], x1[i].ap, reads=[x1[i]])

    def stageB1(grp):
        x1 = x1s[grp % 2]
        for c in range(8):
            b = P.nb()
            for i in range(4):
                ps = P.psum(b, i * 128, 128)
                tr(ps, x1[i][c * 128:(c + 1) * 128], x1[i].ap[:, c * 128:(c + 1) * 128], ident, ident.ap)
            psf = P.psum(b)
            evac(x1T[c * 512:(c + 1) * 512], x1T.ap[:, c * 512:(c + 1) * 512], psf, psf.ap)

    def stageB2(grp):
        for jc in range(8):
            ps = P.psum(P.nb())
            for c in range(8):
                mm(ps, wxq.ap[:, c * 1024 + jc * 128:c * 1024 + (jc + 1) * 128], x1T.ap[:, c * 512:(c + 1) * 512],
                   c == 0, c == 7, [wxq, x1T])
            o = qT[jc * 512:(jc + 1) * 512]
            evac(o, o.ap, ps, ps.ap)

    def stageB3(grp):
        for hx in range(4):
            pts = []
            for mt in range(2):
                ps = P.psum(P.nb())
                for k2 in range(2):
                    jc = hx * 2 + k2
                    mm(ps, KmT.ap[:, jc * 256 + mt * 128:jc * 256 + (mt + 1) * 128], qT.ap[:, jc * 512:(jc + 1) * 512],
                       k2 == 0, k2 == 1, [KmT, qT])
                pt = PTx[mt]
                act(pt.ap, ps.ap, AF.Exp, rd=[ps], wr=[pt], scale=1.0 / 16.0)
                pts.append(pt)
            for i in range(4):
                ps = P.psum(P.nb(), 0, 257)
                for mt in range(2):
                    vsl = Vm[mt * 1028 + hx * 257:mt * 1028 + (hx + 1) * 257]
                    mm(ps, pts[mt].ap[:, i * 128:(i + 1) * 128], vsl.ap, mt == 0, mt == 1, [pts[mt], vsl])
                P.op("dve", lambda e, ps=ps: e.reciprocal(out=rtB.ap[:, 0:1], in_=ps.ap[:, 256:257]), reads=[ps], writes=[rtB[0:1]])
                o = oatt[i][hx * 256:(hx + 1) * 256]
                ts("dve", o.ap, ps.ap[:, 0:256], rtB.ap[:, 0:1], None, ALU.mult, rd=[ps, rtB[0:1]], wr=[o])

    def stageB4(grp):
        for c in range(8):
            b = P.nb()
            for i in range(4):
                ps = P.psum(b, i * 128, 128, BF16)
                tr(ps, oatt[i][c * 128:(c + 1) * 128], oatt[i].ap[:, c * 128:(c + 1) * 128], identb, identb.ap)
            psf = P.psum(b, 0, 512, BF16)
            evac(oT[c * 512:(c + 1) * 512], oT.ap[:, c * 512:(c + 1) * 512], psf, psf.ap)

    def stageC1(grp, i):
        x1 = x1s[grp % 2]
        qb = grp * 4 + i
        for half in range(2):
            ps = P.psum(P.nb())
            for c in range(8):
                mm(ps, oT.ap[:, c * 512 + i * 128:c * 512 + (i + 1) * 128], wxo.ap[:, c * 1024 + half * 512:c * 1024 + (half + 1) * 512],
                   c == 0, c == 7, [oT, wxo])
            stt("dve", ytmpC.ap[:, half * 512:(half + 1) * 512], x1[i].ap[:, half * 512:(half + 1) * 512], ALPHA, ps.ap,
                ALU.mult, ALU.add, rd=[x1[i], ps], wr=[ytmpC[half * 512:(half + 1) * 512]])
        layer_norm(ytmpC, 2, x2, ltC)
        dma("sp", X2[qb * 128:(qb + 1) * 128, :], x2.ap, reads=[x2], writes=[("X2", qb * 128, (qb + 1) * 128)])
        if dbg:
            dma("sp", dbg_x2[qb * 128:(qb + 1) * 128, :], x2.ap, reads=[x2])
        evac(x2b, x2b.ap, x2, x2.ap, "pool")

    def stageC2(grp, i):
        qb = grp * 4 + i
        for half in range(2):
            b = P.nb()
            for c4 in range(4):
                c = half * 4 + c4
                ps = P.psum(b, c4 * 128, 128)
                tr(ps, x2[c * 128:(c + 1) * 128], x2.ap[:, c * 128:(c + 1) * 128], ident, ident.ap)
            psf = P.psum(b)
            evac(x2T[half * 512:(half + 1) * 512], x2T.ap[:, half * 512:(half + 1) * 512], psf, psf.ap)
        ps = P.psum(P.nb(), 0, 32)
        for c in range(8):
            mm(ps, x2T.ap[:, c * 128:(c + 1) * 128], wr_sb.ap[:, c * 32:(c + 1) * 32], c == 0, c == 7, [x2T, wr_sb])
        tt("dve", lg.ap, ps.ap, br_bc.ap, ALU.add, rd=[ps, br_bc], wr=[lg])
        P.op("dve", lambda e: e.max(out=m8.ap, in_=lg.ap), reads=[lg], writes=[m8])
        ts("dve", msk.ap, lg.ap, m8.ap[:, 3:4], None, ALU.is_ge, rd=[lg, m8], wr=[msk])
        ts("dve", rt.ap[:, 1:2], m8.ap[:, 0:1], -1.0, None, ALU.mult, rd=[m8], wr=[rt[1:2]])
        act(ex.ap, lg.ap, AF.Exp, rd=[lg, rt[1:2]], wr=[ex], bias=rt.ap[:, 1:2])
        stt("dve", ex.ap, ex.ap, 1.0, msk.ap, ALU.mult, ALU.mult, rd=[ex, msk], wr=[ex, rt[2:3]], acc=rt.ap[:, 2:3])
        P.op("dve", lambda e: e.reciprocal(out=rt.ap[:, 2:3], in_=rt.ap[:, 2:3]), reads=[rt[2:3]], writes=[rt[2:3]])
        gf = gfull[qb * 32:(qb + 1) * 32]
        ts("dve", gf.ap, ex.ap, rt.ap[:, 2:3], None, ALU.mult, rd=[ex, rt[2:3]], wr=[gf])
        ps = P.psum(P.nb(), 0, 32)
        mm(ps, Ult.ap, msk.ap, True, False, [Ult, msk])
        mm(ps, ones.ap, macc.ap, False, True, [ones, macc])
        ts("dve", Dm.ap, ps.ap, float(CAP - 1), None, ALU.min, rd=[ps], wr=[Dm])
        tt("dve", Dm.ap, Dm.ap, ebase.ap, ALU.add, rd=[Dm, ebase], wr=[Dm])
        tt("dve", Dm.ap, Dm.ap, msk.ap, ALU.mult, rd=[Dm, msk], wr=[Dm])
        tt("pool", macc.ap, macc.ap, msk.ap, ALU.add, rd=[macc, msk], wr=[macc])
        P.op("dve", lambda e: e.max(out=d8.ap, in_=Dm.ap), reads=[Dm], writes=[d8])
        de = dest_all[qb * 4:(qb + 1) * 4]
        ts("dve", de.ap, d8.ap[:, 0:4], -1.0, None, ALU.add, rd=[d8], wr=[de])
        for k in range(4):
            ga = gate_all[qb * 4 + k:qb * 4 + k + 1]
            stt("dve", junk.ap[:, 0:32], Dm.ap, d8.ap[:, k:k + 1], gf.ap, ALU.is_equal, ALU.mult, rd=[Dm, d8, gf],
                wr=[junk[0:32], ga], acc=ga.ap)
            P.op("pool", lambda e, k=k, de=de: e.indirect_dma_start(
                out=Xg, out_offset=bass.IndirectOffsetOnAxis(ap=de.ap[:, k:k + 1], axis=0), in_=x2b.ap, in_offset=None),
                reads=[x2b, de], writes=[("Xg", 0, NE * CAP)], dma=True)

    stageA(0)
    stageB1(0)
    stageB2(0)
    stageB3(0)
    stageB4(0)
    for grp in range(4):
        nxt = grp + 1 < 4
        if nxt:
            stageA(grp + 1)
        Bs = [stageB1, stageB2, stageB3, stageB4]
        for i in range(4):
            stageC1(grp, i)
            if nxt:
                Bs[i](grp + 1)
            stageC2(grp, i)
    P.release(m2)
    if stop < 4:
        return finish()

    m3 = P.mark()
    bguT = P.alloc(NE * 16)
    dma("sp", bguT.ap, bguT_d, writes=[bguT])
    bguS = P.alloc(NE * 16)
    ts("dve", bguS.ap, bguT.ap, 1.0 / 1.702, None, ALU.mult, rd=[bguT], wr=[bguS])
    wgu2 = [P.alloc(8 * 2048, BF16) for _ in range(2)]
    wd2 = [P.alloc(8 * 1024, BF16) for _ in range(2)]
    Xe2 = [[P.alloc(1024, BF16) for _ in range(3)] for _ in range(2)]
    XeT2 = [P.alloc(8 * CAP, BF16) for _ in range(2)]
    hidT = P.alloc(8 * CAP, BF16)
    gt2 = [P.alloc(CAP) for _ in range(2)]
    sg2 = [P.alloc(CAP) for _ in range(2)]
    ln2 = [P.alloc(CAP) for _ in range(2)]
    Ysb = [P.alloc(1024, BF16) for _ in range(2)]

    def load_wgu(en):
        if en >= NE:
            return
        wgu_ = wgu2[en % 2]
        for c4 in range(4):
            sl_ = wgu_[c4 * 4096:(c4 + 1) * 4096]
            dma("pool", sl_.ap.rearrange("p (c n) -> p c n", c=2),
                w_gu[en, c4 * 256:(c4 + 1) * 256, :].rearrange("(c p) n -> p c n", p=128), writes=[sl_])

    def load_wd(en):
        if en >= NE:
            return
        wd_ = wd2[en % 2]
        for c4 in range(2):
            sl_ = wd_[c4 * 4096:(c4 + 1) * 4096]
            dma("pool", sl_.ap.rearrange("p (c n) -> p c n", c=4),
                w_dn[en, c4 * 512:(c4 + 1) * 512, :].rearrange("(c p) n -> p c n", p=128), writes=[sl_])

    def load_xe(en):
        if en >= NE:
            return
        for s in range(3):
            r0 = en * CAP + s * 128
            dma("sp", Xe2[en % 2][s].ap, Xg[r0:r0 + 128, :], reads=[("Xg", r0, r0 + 128)], writes=[Xe2[en % 2][s]])

    def xe_transposes(en):
        if en >= NE:
            return
        Xe, XeT = Xe2[en % 2], XeT2[en % 2]
        for c in range(8):
            b = P.nb()
            for s in range(3):
                ps = P.psum(b, s * 128, 128, BF16)
                tr(ps, Xe[s][c * 128:(c + 1) * 128], Xe[s].ap[:, c * 128:(c + 1) * 128], identb, identb.ap)
            psf = P.psum(b, 0, CAP, BF16)
            evac(XeT[c * CAP:(c + 1) * CAP], XeT.ap[:, c * CAP:(c + 1) * CAP], psf, psf.ap)

    load_wgu(0)
    load_wd(0)
    load_wgu(1)
    load_wd(1)
    load_xe(0)
    xe_transposes(0)
    yi = 0
    for ex_ in range(NE):
        wgu = wgu2[ex_ % 2]
        wd = wd2[ex_ % 2]
        XeT = XeT2[ex_ % 2]
        load_xe(ex_ + 1)
        for g in range(8):
            psg = P.psum(P.nb(), 0, CAP)
            for c in range(8):
                mm(psg, wgu.ap[:, c * 2048 + g * 128:c * 2048 + (g + 1) * 128], XeT.ap[:, c * CAP:(c + 1) * CAP],
                   c == 0, c == 7, [wgu[c * 2048:(c + 1) * 2048], XeT])
            psl = P.psum(P.nb(), 0, CAP)
            for c in range(8):
                mm(psl, wgu.ap[:, c * 2048 + 1024 + g * 128:c * 2048 + 1024 + (g + 1) * 128], XeT.ap[:, c * CAP:(c + 1) * CAP],
                   c == 0, c == 7, [wgu[c * 2048:(c + 1) * 2048], XeT])
            gt, sg, ln = gt2[g % 2], sg2[g % 2], ln2[g % 2]
            bg = bguT[ex_ * 16 + g:ex_ * 16 + g + 1]
            bl = bguT[ex_ * 16 + 8 + g:ex_ * 16 + 8 + g + 1]
            ts("dve", gt.ap, psg.ap, bg.ap, 7.0, ALU.add, ALU.min, rd=[psg, bg], wr=[gt])
            act(sg.ap, gt.ap, AF.Silu, rd=[gt], wr=[sg], scale=1.702)
            bls = bguS[ex_ * 16 + 8 + g:ex_ * 16 + 8 + g + 1]
            act(ln.ap, psl.ap, AF.Identity, rd=[psl, bls], wr=[ln], bias=bls.ap, scale=1.0 / 1.702)
            ts("dve", ln.ap, ln.ap, 7.0 / 1.702, -7.0 / 1.702, ALU.min, ALU.max, rd=[ln], wr=[ln])
            o = hidT[g * CAP:(g + 1) * CAP]
            stt("dve", o.ap, ln.ap, 1.0 / 1.702, sg.ap, ALU.add, ALU.mult, rd=[ln, sg], wr=[o])
        load_wgu(ex_ + 2)
        xe_transposes(ex_ + 1)
        for s in range(3):
            ysb = Ysb[yi % 2]
            yi += 1
            for half in range(2):
                ps = P.psum(P.nb())
                for g in range(8):
                    mm(ps, hidT.ap[:, g * CAP + s * 128:g * CAP + (s + 1) * 128], wd.ap[:, g * 1024 + half * 512:g * 1024 + (half + 1) * 512],
                       g == 0, g == 7, [hidT, wd[g * 1024:(g + 1) * 1024]])
                evac(ysb[half * 512:(half + 1) * 512], ysb.ap[:, half * 512:(half + 1) * 512], ps, ps.ap)
            r0 = ex_ * CAP + s * 128
            dma("sp", Yg[r0:r0 + 128, :], ysb.ap, reads=[ysb], writes=[("Yg", r0, r0 + 128)])
        load_wd(ex_ + 2)
    P.release(m3)
    if stop < 5:
        return finish()

    lnp4 = P.alloc(2 * 1024)
    dma("sp", lnp4.ap, ln_d[:, 4096:6144].partition_broadcast(128), writes=[lnp4])
    lnp_box[0], lnp_box[1] = lnp4, 4
    bdn = P.alloc(1024)
    dma("sp", bdn.ap[0:32, :], b_dn, writes=[bdn])
    Gk2 = [[P.alloc(1024, BF16) for _ in range(4)] for _ in range(2)]
    acc2 = [P.alloc(1024) for _ in range(2)]
    x2r2 = [P.alloc(1024) for _ in range(2)]
    gT = P.alloc(128)
    outt2 = [P.alloc(1024) for _ in range(2)]
    lt4 = P.alloc(8)

    def comb_loads(qb):
        de = dest_all[qb * 4:(qb + 1) * 4]
        Gk = Gk2[qb % 2]
        for k in range(4):
            P.op("pool", lambda e, k=k, de=de, Gk=Gk: e.indirect_dma_start(
                out=Gk[k].ap, out_offset=None, in_=Yg, in_offset=bass.IndirectOffsetOnAxis(ap=de.ap[:, k:k + 1], axis=0)),
                reads=[("Yg", 0, NE * CAP), de], writes=[Gk[k]], dma=True)
        dma("sp", x2r2[qb % 2].ap, X2[qb * 128:(qb + 1) * 128, :], reads=[("X2", qb * 128, (qb + 1) * 128)], writes=[x2r2[qb % 2]])

    comb_loads(0)
    for qb in range(16):
        if qb + 1 < 16:
            comb_loads(qb + 1)
        Gk, acc, x2r, outt = Gk2[qb % 2], acc2[qb % 2], x2r2[qb % 2], outt2[qb % 2]
        ga = gate_all[qb * 4:(qb + 1) * 4]
        ts("dve", acc.ap, Gk[0].ap, ga.ap[:, 0:1], None, ALU.mult, rd=[Gk[0], ga], wr=[acc])
        for k in range(1, 4):
            stt("dve", acc.ap, Gk[k].ap, ga.ap[:, k:k + 1], acc.ap, ALU.mult, ALU.add, rd=[Gk[k], ga, acc], wr=[acc])
        gf = gfull[qb * 32:(qb + 1) * 32]
        ps = P.psum(P.nb(), 0, 128)
        P.op("pe", lambda e, ps=ps, gf=gf: e.transpose(ps.ap[0:32, :], gf.ap, ident.ap), reads=[gf, ident], writes=[ps])
        evac(gT, gT.ap[0:32, :], ps, ps.ap[0:32, :], "dve")
        for half in range(2):
            ps = P.psum(P.nb())
            mm(ps, gT.ap[0:32, :], bdn.ap[0:32, half * 512:(half + 1) * 512], True, True, [gT, bdn])
            tt("dve", acc.ap[:, half * 512:(half + 1) * 512], acc.ap[:, half * 512:(half + 1) * 512], ps.ap, ALU.add,
               rd=[acc[half * 512:(half + 1) * 512], ps], wr=[acc[half * 512:(half + 1) * 512]])
        stt("dve", acc.ap, x2r.ap, ALPHA, acc.ap, ALU.mult, ALU.add, rd=[x2r, acc], wr=[acc])
        layer_norm(acc, 4, outt, lt4)
        dma("sp", out_d[qb * 128:(qb + 1) * 128, :], outt.ap, reads=[outt])

    return finish()


_CACHE = {}


def _consts():
    c = np.zeros((128, 800), np.float32)
    idx = np.arange(128)
    c[:, 0:128] = np.eye(128)
    c[:, 128:256] = (idx[:, None] <= idx[None, :])
    c[:, 256:384] = (idx[:, None] > idx[None, :])
    c[:, 384:512] = 1.0
    c[:, 512:640] = np.where(idx[:, None] <= idx[None, :], 0.0, NEG)
    c[:, 640:768] = (idx[:, None] < idx[None, :])
    c[:, 768:800] = (np.arange(NE) * CAP + 1)[None, :]
    return c


def make_in_maps(x, mem, w_in, fox_f_bias, mlstm_conv_w, mlstm_i_bias, mlstm_f_bias, fox_norm_g, mlstm_norm_g,
                 w_mix_out, ln1_g, ln1_b, w_xq, w_xk, w_xv, w_xo, ln2_g, ln2_b, w_router, b_router, w_gate_up,
                 b_gate_up, w_down, b_down, ln3_g, ln3_b):
    f = lambda a: np.ascontiguousarray(np.asarray(a, dtype=np.float32))
    x = f(x)
    mem = f(mem)
    gb = np.concatenate([f(fox_f_bias)[0], f(mlstm_i_bias)[0], f(mlstm_f_bias)[0]])
    shared = {
        "consts": _consts(),
        "w_in": f(w_in)[0],
        "gbias": np.ascontiguousarray(np.tile(gb, 32)[None, :]),
        "convT": np.ascontiguousarray(f(mlstm_conv_w)[0].reshape(4, 4, 128).transpose(2, 1, 0).reshape(128, 16)),
        "foxg": f(fox_norm_g),
        "mlg": f(mlstm_norm_g),
        "w_mix": f(w_mix_out)[0],
        "lnp": np.ascontiguousarray(np.concatenate([f(ln1_g)[0], f(ln1_b)[0], f(ln2_g)[0], f(ln2_b)[0], f(ln3_g)[0], f(ln3_b)[0]])[None, :]),
        "w_xq": f(w_xq)[0], "w_xk": f(w_xk)[0], "w_xv": f(w_xv)[0], "w_xo": f(w_xo)[0],
        "w_r": f(w_router)[0], "b_r": f(b_router),
        "w_gu": f(w_gate_up)[0],
        "bguT": np.ascontiguousarray(f(b_gate_up)[0].reshape(NE, 16, 128).transpose(2, 0, 1).reshape(128, NE * 16)),
        "w_dn": f(w_down)[0], "b_dn": f(b_down)[0],
    }
    maps = []
    for c in range(8):
        b, h = c // 2, c % 2
        if h == 0:
            xc = np.concatenate([np.zeros((NTOK, D), np.float32), x[b, :NTOK]], axis=0)
            km = np.full((128, 1), NEG, np.float32)
        else:
            xc = x[b]
            km = np.zeros((128, 1), np.float32)
        m = dict(shared)
        m["xcat"] = np.ascontiguousarray(xc)
        m["memb"] = mem[b]
        m["kmask"] = km
        maps.append(m)
    return maps


def kernel(**inputs):
    if "nc" not in _CACHE:
        _CACHE["nc"] = build_program(False)
    nc = _CACHE["nc"]
    maps = make_in_maps(**inputs)
    res = run_bass_kernel_spmd(nc, maps, core_ids=list(range(8)))
    out = np.zeros((4, 4096, D), np.float32)
    for c in range(8):
        b, h = c // 2, c % 2
        out[b, h * NTOK:(h + 1) * NTOK] = res.results[c]["out"]
    return out
```

```python
import numpy as np
from contextlib import ExitStack
import concourse.bass as bass
import concourse.mybir as mybir
from concourse.bass_utils import run_bass_kernel_spmd

F32 = mybir.dt.float32
BF16 = mybir.dt.bfloat16
I32 = mybir.dt.int32
AF = mybir.ActivationFunctionType
ALU = mybir.AluOpType
AX = mybir.AxisListType
ESZ = {F32: 4, BF16: 2, I32: 4}

D = 1024
NTOK = 2048
NALL = 4096
NE = 32
CAP = 384
ALPHA = 2.0 ** 0.25
NEG = -30000.0
PAGE = 2048
_DBG = {}


class V:
    def __init__(self, ap, key, lo, hi, es):
        self.ap, self.key, self.lo, self.hi, self.es = ap, key, lo, hi, es

    def __getitem__(self, sl):
        a, b = sl.start or 0, sl.stop
        return V(self.ap[:, a:b], self.key, self.lo + a * self.es, self.lo + b * self.es, self.es)

    def p(self, p0, p1):
        return V(self.ap[p0:p1], self.key, self.lo, self.hi, self.es)

    def re(self, pat, **kw):
        return V(self.ap.rearrange(pat, **kw), self.key, self.lo, self.hi, self.es)

    def w(self, ap):
        return V(ap, self.key, self.lo, self.hi, self.es)


class Op:
    __slots__ = ("eng", "fn", "deps", "dma", "inc", "val", "sem", "idx", "prewait")


class Prog:
    ENGS = ["pe", "dve", "act", "pool", "sp"]

    def __init__(self, nc, sbuf_bytes, dma_pool=20):
        self.nc = nc
        self.ops = []
        self.recs = {}
        self.sb_off = 0
        self.sbuf_bytes = sbuf_bytes
        self.dma_pool = dma_pool
        self.bank = 0

    def setup(self, stack):
        nc = self.nc
        self.sb = stack.enter_context(nc.sbuf_tensor("sb_all", [128, self.sbuf_bytes // 4], F32))
        self.ps = stack.enter_context(nc.psum_tensor("ps_all", [128, 4096], F32))

    def alloc(self, ncols, dt=F32):
        nbytes = ((ncols * ESZ[dt] + 63) // 64) * 64
        off = self.sb_off
        self.sb_off += nbytes
        assert self.sb_off <= self.sbuf_bytes, f"SBUF overflow {self.sb_off}"
        ap = self.sb[:, off // 4:(off + nbytes) // 4]
        if dt != F32:
            ap = ap.bitcast(dt)
        ap = ap[:, 0:ncols]
        return V(ap, "S", off, off + ncols * ESZ[dt], ESZ[dt])

    def mark(self):
        return self.sb_off

    def release(self, m):
        self.sb_off = m

    def psum(self, bank, col0=0, ncols=512, dt=F32):
        off = bank * 2048 + col0 * ESZ[dt]
        nb = ncols * ESZ[dt]
        assert col0 * ESZ[dt] + nb <= 2048
        ap = self.ps[:, bank * 512:(bank + 1) * 512]
        if dt != F32:
            ap = ap.bitcast(dt)
        ap = ap[:, col0:col0 + ncols]
        return V(ap, "P", off, off + nb, ESZ[dt])

    nbanks = 8

    def nb(self):
        b = self.bank % self.nbanks
        self.bank = (b + 1) % self.nbanks
        return b

    def _deps(self, idx, eng, isdma, reads, writes):
        deps = set()
        ops = self.ops
        for (key, lo, hi) in reads:
            for pg in range(lo // PAGE, (hi - 1) // PAGE + 1):
                a, b = max(lo, pg * PAGE), min(hi, (pg + 1) * PAGE)
                lst = self.recs.setdefault((key, pg), [])
                found = False
                for r in lst:
                    if r[2] == "w":
                        if r[0] < b and a < r[1]:
                            deps.add(r[3])
                    elif (not found) and r[0] == a and r[1] == b and not isdma:
                        od = ops[r[3]]
                        if od.eng == eng and not od.dma:
                            r[3] = idx
                            found = True
                if not found:
                    lst.append([a, b, "r", idx])
        for (key, lo, hi) in writes:
            for pg in range(lo // PAGE, (hi - 1) // PAGE + 1):
                a, b = max(lo, pg * PAGE), min(hi, (pg + 1) * PAGE)
                lst = self.recs.get((key, pg), [])
                keep = []
                for r in lst:
                    if r[3] != idx and r[0] < b and a < r[1]:
                        deps.add(r[3])
                        if r[0] >= a and r[1] <= b:
                            continue
                    keep.append(r)
                keep.append([a, b, "w", idx])
                self.recs[(key, pg)] = keep
        return deps

    @staticmethod
    def _reg(x):
        if isinstance(x, V):
            if x.key == "P":
                return (x.key, (x.lo // PAGE) * PAGE, ((x.hi - 1) // PAGE + 1) * PAGE)
            return (x.key, x.lo, x.hi)
        return x

    def op(self, eng, fn, reads=(), writes=(), dma=False):
        o = Op()
        o.eng, o.fn, o.dma, o.inc, o.val, o.sem, o.prewait = eng, fn, dma, False, None, None, None
        o.idx = len(self.ops)
        self.ops.append(o)
        deps = self._deps(o.idx, eng, dma, [self._reg(r) for r in reads], [self._reg(w) for w in writes])
        deps.discard(o.idx)
        fin = set()
        for d in deps:
            od = self.ops[d]
            if od.eng == eng and eng == "pe" and not od.dma and not dma:
                continue
            fin.add(d)
        o.deps = fin
        return o

    def emit(self, stack):
        nc = self.nc
        ops = self.ops
        for o in ops:
            for d in o.deps:
                ops[d].inc = True
        esem = {e: stack.enter_context(nc.semaphore("sem_" + e)) for e in self.ENGS}
        dq = ("sp", "pool", "act")
        dsem = {e: [stack.enter_context(nc.semaphore(f"dsem_{e}_{i}")) for i in range(self.dma_pool)] for e in dq}
        cnt = {e: 0 for e in self.ENGS}
        dcnt = {e: 0 for e in dq}
        for o in ops:
            if o.dma:
                k = dcnt[o.eng]
                dcnt[o.eng] += 1
                s = k % self.dma_pool
                o.sem = dsem[o.eng][s]
                o.val = 16 * (k // self.dma_pool + 1)
                o.prewait = (o.sem, o.val - 16) if o.val > 16 else None
            elif o.inc:
                cnt[o.eng] += 1
                o.sem = esem[o.eng]
                o.val = cnt[o.eng]
        block = stack.enter_context(nc.Block())
        engobj = {"pe": "tensor", "dve": "vector", "act": "scalar", "pool": "gpsimd", "sp": "sync"}

        def make(ename):
            mine = [o for o in ops if o.eng == ename]

            def body(e):
                waited = {}

                def wait(sem, val):
                    k = id(sem)
                    if waited.get(k, 0) >= val:
                        return
                    waited[k] = val
                    e.wait_ge(sem, val)

                for o in mine:
                    if o.prewait is not None:
                        wait(*o.prewait)
                    need = {}
                    for d in o.deps:
                        od = ops[d]
                        k = id(od.sem)
                        if k not in need or need[k][1] < od.val:
                            need[k] = (od.sem, od.val)
                    for sem, val in need.values():
                        wait(sem, val)
                    ins = o.fn(e)
                    if o.dma:
                        ins.then_inc(o.sem, 16)
                    elif o.inc:
                        ins.then_inc(o.sem, 1)
                if ename in dsem:
                    last = {}
                    for o in mine:
                        if o.dma:
                            last[id(o.sem)] = (o.sem, o.val)
                    for sem, val in last.values():
                        wait(sem, val)
            return body

        for ename in self.ENGS:
            getattr(block, engobj[ename])(make(ename))


def build_program(dbg=False, stop=99):
    nc = bass.Bass("TRN2", target_bir_lowering=False)

    state = {"dumped": False}

    def dump_mixed():
        if not dbg or state["dumped"] or "mixed" not in state:
            return
        state["dumped"] = True
        mixed_ = state["mixed"]
        mtmp = P.alloc(1024)
        for qb in range(16):
            P.op("dve", lambda e, qb=qb: e.tensor_copy(out=mtmp.ap, in_=mixed_.ap[:, qb * 1024:(qb + 1) * 1024]),
                 reads=[mixed_[qb * 1024:(qb + 1) * 1024]], writes=[mtmp])
            P.op("sp", lambda e, qb=qb: e.dma_start(out=dbg_mixed[qb * 128:(qb + 1) * 128, :], in_=mtmp.ap), reads=[mtmp], dma=True)

    def finish():
        dump_mixed()
        P.emit(st)
        st.close()
        return nc

    def din(name, shape, dt=F32):
        return nc.dram_tensor(name, list(shape), dt, kind="ExternalInput").ap()

    xcat = din("xcat", [NALL, D])
    memb = din("memb", [256, D])
    kmask_d = din("kmask", [128, 1])
    consts_d = din("consts", [128, 800])
    w_in = din("w_in", [D, 3088])
    gbias_d = din("gbias", [1, 512])
    convT_d = din("convT", [128, 16])
    foxg_d = din("foxg", [1, 512])
    mlg_d = din("mlg", [1, 512])
    w_mix = din("w_mix", [D, D])
    ln_d = din("lnp", [1, 6 * D])
    w_xq = din("w_xq", [D, D])
    w_xk = din("w_xk", [D, D])
    w_xv = din("w_xv", [D, D])
    w_xo = din("w_xo", [D, D])
    w_r = din("w_r", [D, NE])
    b_r = din("b_r", [1, NE])
    NEd = NE if stop >= 5 else 1
    w_gu = din("w_gu", [NEd, D, 2 * D])
    bguT_d = din("bguT", [128, NE * 16])
    w_dn = din("w_dn", [NEd, D, D])
    b_dn = din("b_dn", [NE, D])
    out_d = nc.dram_tensor("out", [NTOK, D], F32, kind="ExternalOutput").ap()
    if dbg:
        dbg_mixed = nc.dram_tensor("dbg_mixed", [NTOK, D], F32, kind="ExternalOutput").ap()
        dbg_x1 = nc.dram_tensor("dbg_x1", [NTOK, D], F32, kind="ExternalOutput").ap()
        dbg_x2 = nc.dram_tensor("dbg_x2", [NTOK, D], F32, kind="ExternalOutput").ap()

    st = ExitStack()
    P = Prog(nc, 200 * 1024)
    P.setup(st)
    Xg = nc.dram_tensor("Xg", [NE * CAP, D], BF16).ap()
    Yg = nc.dram_tensor("Yg", [NE * CAP, D], BF16).ap()
    X2 = nc.dram_tensor("X2s", [NTOK, D], F32).ap()

    def wview(w, c0, c1):
        return w.rearrange("(c p) n -> p c n", p=128)[:, :, c0:c1]

    def mm(ps, lhsT, rhs, start, stop, rd):
        P.op("pe", lambda e: e.matmul(ps.ap, lhsT, rhs, start=start, stop=stop), reads=rd, writes=[ps])

    def tr(ps, in_v, in_ap, ident_v, ident_ap):
        P.op("pe", lambda e: e.transpose(ps.ap, in_ap, ident_ap), reads=[in_v, ident_v], writes=[ps])

    ev_rr = [0]

    def evac(out_v, out_ap, in_v, in_ap, eng=None):
        if eng is None:
            eng = ("dve", "act")[ev_rr[0] % 2]
            ev_rr[0] += 1
        if eng == "act":
            P.op("act", lambda e: e.copy(out=out_ap, in_=in_ap), reads=[in_v], writes=[out_v])
        else:
            P.op(eng, lambda e: e.tensor_copy(out=out_ap, in_=in_ap), reads=[in_v], writes=[out_v])

    def dma(eng, out_ap, in_ap, reads=(), writes=()):
        P.op(eng, lambda e: e.dma_start(out=out_ap, in_=in_ap), reads=reads, writes=writes, dma=True)

    def ts(eng, out, in0, s1, s2, op0, op1=None, rd=(), wr=()):
        if s2 is None:
            P.op(eng, lambda e: e.tensor_scalar(out=out, in0=in0, scalar1=s1, scalar2=None, op0=op0), reads=rd, writes=wr)
        else:
            P.op(eng, lambda e: e.tensor_scalar(out=out, in0=in0, scalar1=s1, scalar2=s2, op0=op0, op1=op1), reads=rd, writes=wr)

    def tt(eng, out, in0, in1, op, rd=(), wr=()):
        P.op(eng, lambda e: e.tensor_tensor(out=out, in0=in0, in1=in1, op=op), reads=rd, writes=wr)

    def stt(eng, out, in0, sc, in1, op0, op1, rd=(), wr=(), acc=None):
        if acc is None:
            P.op(eng, lambda e: e.scalar_tensor_tensor(out=out, in0=in0, scalar=sc, in1=in1, op0=op0, op1=op1), reads=rd, writes=wr)
        else:
            P.op(eng, lambda e: e.scalar_tensor_tensor(out=out, in0=in0, scalar=sc, in1=in1, op0=op0, op1=op1, accum_out=acc),
                 reads=rd, writes=wr)

    def act(out, in_, func, rd=(), wr=(), bias=None, scale=None):
        kw = {}
        if bias is not None:
            kw["bias"] = bias
        if scale is not None:
            kw["scale"] = scale
        P.op("act", lambda e: e.activation(out=out, in_=in_, func=func, **kw), reads=rd, writes=wr)

    cst = P.alloc(800)
    dma("sp", cst.ap, consts_d, writes=[cst])
    ident = cst[0:128]
    Uincl = cst[128:256]
    ones = cst[384:512]
    maskneg = cst[512:640]
    Ult = cst[640:768]
    ebase = cst[768:800]
    identb = P.alloc(128, BF16)
    evac(identb, identb.ap, ident, ident.ap, "dve")
    causb = P.alloc(128, BF16)
    evac(causb, causb.ap, Uincl, Uincl.ap, "dve")
    kmask = P.alloc(1)
    dma("sp", kmask.ap, kmask_d, writes=[kmask])
    mixed = P.alloc(16 * 1024, BF16)
    state["mixed"] = mixed
    P.op("pool", lambda e: e.memset(mixed.ap, 0.0), writes=[mixed])
    junk = P.alloc(128)
    dest_all = P.alloc(64, I32)
    gate_all = P.alloc(64)
    gfull = P.alloc(16 * 32)
    lnp_box = [None, 0]
    m_gates = P.mark()
    gbias = P.alloc(512)
    dma("sp", gbias.ap, gbias_d.partition_broadcast(128), writes=[gbias])
    gpre = P.alloc(512)
    glog = P.alloc(512)
    cs = P.alloc(512)
    tot = P.alloc(512)

    def load_x_block(tb, xs):
        for i in range(4):
            r0 = tb * 512 + i * 128
            dma("sp", xs[i].ap, xcat[r0:r0 + 128, :], writes=[xs[i]])

    def transpose_block(xs, xT):
        for c in range(8):
            b = P.nb()
            for i in range(4):
                ps = P.psum(b, i * 128, 128)
                tr(ps, xs[i][c * 128:(c + 1) * 128], xs[i].ap[:, c * 128:(c + 1) * 128], ident, ident.ap)
            psf = P.psum(b)
            evac(xT[c * 512:(c + 1) * 512], xT.ap[:, c * 512:(c + 1) * 512], psf, psf.ap)

    def rstd_from_ss(ss, n, eps_col, outv):
        ts("dve", outv.ap, ss.ap, 1.0 / n, (1e-5, 1e-6)[eps_col], ALU.mult, ALU.add, rd=[ss], wr=[outv])
        act(outv.ap, outv.ap, AF.Sqrt, rd=[outv], wr=[outv])
        P.op("dve", lambda e: e.reciprocal(out=outv.ap, in_=outv.ap), reads=[outv], writes=[outv])

    def layer_norm(y, gi, outv, tmp):
        lnp, base = lnp_box
        g = lnp[(gi - base) * 1024:(gi - base + 1) * 1024]
        b = lnp[(gi - base + 1) * 1024:(gi - base + 2) * 1024]
        s1 = tmp[0:1]
        nm = tmp[1:2]
        ss = tmp[2:3]
        rs = tmp[3:4]
        P.op("dve", lambda e: e.tensor_reduce(out=s1.ap, in_=y.ap, axis=AX.X, op=ALU.add), reads=[y], writes=[s1])
        ts("dve", nm.ap, s1.ap, -1.0 / D, None, ALU.mult, rd=[s1], wr=[nm])
        act(y.ap, y.ap, AF.Identity, rd=[y, nm], wr=[y], bias=nm.ap)
        P.op("act", lambda e: e.activation(out=outv.ap, in_=y.ap, func=AF.Square, accum_out=ss.ap), reads=[y], writes=[outv, ss])
        rstd_from_ss(ss, D, 0, rs)
        stt("dve", outv.ap, y.ap, rs.ap, g.ap, ALU.mult, ALU.mult, rd=[y, rs, g], wr=[outv])
        tt("dve", outv.ap, outv.ap, b.ap, ALU.add, rd=[outv, b], wr=[outv])

    m_phase1 = P.mark()
    zt = P.alloc(2048, BF16)
    P.op("pool", lambda e: e.memset(zt.ap, 0.0), writes=[zt])
    for r in range(0, NE * CAP, 256):
        P.op("act", lambda e, r=r: e.dma_start(out=Xg[r:r + 256, :].rearrange("(p a) n -> p (a n)", a=2), in_=zt.ap),
             reads=[zt], writes=[("Xg", r, r + 256)], dma=True)

    KT = P.alloc(4 * NALL, BF16)
    QT = P.alloc(4 * NTOK, BF16)
    Vaug = P.alloc(32 * 8 * 65, BF16)
    P.op("pool", lambda e: e.memset(Vaug.ap, 1.0), writes=[Vaug])
    m_proj = P.mark()
    wq = P.alloc(8 * 512, BF16)
    wk = P.alloc(8 * 512, BF16)
    wv = P.alloc(8 * 512, BF16)
    wg = P.alloc(8 * 16, BF16)
    dma("pool", wk.re("p (c n) -> p c n", c=8).ap, wview(w_in, 512, 1024), writes=[wk])
    dma("pool", wv.re("p (c n) -> p c n", c=8).ap, wview(w_in, 1024, 1536), writes=[wv])
    dma("pool", wg.re("p (c n) -> p c n", c=8).ap[:, :, 0:8], wview(w_in, 1536, 1544), writes=[wg])
    dma("pool", wg.re("p (c n) -> p c n", c=8).ap[:, :, 8:16], wview(w_in, 2568, 2576), writes=[wg])
    dma("pool", wq.re("p (c n) -> p c n", c=8).ap, wview(w_in, 0, 512), writes=[wq])
    xs_a = [P.alloc(1024) for _ in range(4)]
    xT2 = [P.alloc(8 * 512, BF16) for _ in range(2)]
    for tb in range(8):
        xT = xT2[tb % 2]
        load_x_block(tb, xs_a)
        transpose_block(xs_a, xT)
        for hp in range(4):
            ps = P.psum(P.nb())
            for c in range(8):
                mm(ps, wk.ap[:, c * 512 + hp * 128:c * 512 + (hp + 1) * 128], xT.ap[:, c * 512:(c + 1) * 512],
                   c == 0, c == 7, [wk, xT])
            o = KT[hp * NALL + tb * 512:hp * NALL + (tb + 1) * 512]
            evac(o, o.ap, ps, ps.ap)
        if tb >= 4:
            for hp in range(4):
                ps = P.psum(P.nb())
                for c in range(8):
                    mm(ps, wq.ap[:, c * 512 + hp * 128:c * 512 + (hp + 1) * 128], xT.ap[:, c * 512:(c + 1) * 512],
                       c == 0, c == 7, [wq, xT])
                o = QT[hp * NTOK + (tb - 4) * 512:hp * NTOK + (tb - 3) * 512]
                P.op("act", lambda e, o=o, ps=ps: e.activation(out=o.ap, in_=ps.ap, func=AF.Copy, scale=0.125),
                     reads=[ps], writes=[o])
        for i in range(4):
            blk = tb * 4 + i
            ps = P.psum(P.nb())
            for c in range(8):
                mm(ps, xT.ap[:, c * 512 + i * 128:c * 512 + (i + 1) * 128], wv.ap[:, c * 512:(c + 1) * 512],
                   c == 0, c == 7, [xT, wv])
            o = Vaug[blk * 520:(blk + 1) * 520]
            evac(o, o.ap.rearrange("p (h d) -> p h d", d=65)[:, :, 0:64], ps, ps.ap.rearrange("p (h d) -> p h d", d=64))
            ps2 = P.psum(P.nb(), 0, 16)
            for c in range(8):
                mm(ps2, xT.ap[:, c * 512 + i * 128:c * 512 + (i + 1) * 128], wg.ap[:, c * 16:(c + 1) * 16],
                   c == 0, c == 7, [xT, wg])
            o2 = gpre[blk * 16:(blk + 1) * 16]
            evac(o2, o2.ap, ps2, ps2.ap, "dve")
    P.release(m_proj)
    if stop < 1:
        return finish()

    tt("dve", gpre.ap, gpre.ap, gbias.ap, ALU.add, rd=[gpre, gbias], wr=[gpre])
    act(glog.ap, gpre.ap, AF.Sigmoid, rd=[gpre], wr=[glog])
    act(glog.ap, glog.ap, AF.Ln, rd=[glog], wr=[glog])
    ps = P.psum(P.nb())
    mm(ps, Uincl.ap, glog.ap, True, True, [Uincl, glog])
    evac(cs, cs.ap, ps, ps.ap, "dve")
    ps = P.psum(P.nb())
    mm(ps, ones.ap, glog.ap, True, True, [ones, glog])
    evac(tot, tot.ap, ps, ps.ap, "dve")
    m_fox = P.mark()
    pa = P.alloc(512)
    pb = P.alloc(512)
    evac(pa, pa.ap, tot, tot.ap, "dve")
    cur, oth = pa, pb
    for k in range(5):
        s = 16 * (2 ** k)
        evac(oth[0:s], oth.ap[:, 0:s], cur[0:s], cur.ap[:, 0:s], "dve")
        tt("dve", oth.ap[:, s:512], cur.ap[:, s:512], cur.ap[:, 0:512 - s], ALU.add, rd=[cur], wr=[oth[s:512]])
        cur, oth = oth, cur
    pincl = cur
    pexcl = oth
    tt("dve", pexcl.ap, pincl.ap, tot.ap, ALU.subtract, rd=[pincl, tot], wr=[pexcl])
    negc = P.alloc(512)
    stt("dve", negc.ap, cs.ap, -1.0, pexcl.ap, ALU.mult, ALU.subtract, rd=[cs, pexcl], wr=[negc])
    biasT = P.alloc(4 * 32 * 8)
    for G in range(4):
        for h in range(8):
            col = (16 + 4 * G) * 16 + h
            o = biasT[G * 256:(G + 1) * 256]
            ts("dve", o.ap.rearrange("p (b h) -> p b h", h=8)[:, :, h], negc.ap.rearrange("p (b g) -> p b g", g=16)[:, :, h],
               pexcl.ap[:, col:col + 1], None, ALU.add, rd=[negc, pexcl], wr=[o])
        o = biasT[G * 256:G * 256 + 128]
        ts("dve", o.ap, o.ap, kmask.ap, None, ALU.add, rd=[o, kmask], wr=[o])
    foxg = P.alloc(512)
    dma("sp", foxg.ap, foxg_d.partition_broadcast(128), writes=[foxg])
    if stop < 1.2:
        return finish()

    P.nbanks = 6
    PT3 = [P.alloc(1024, BF16) for _ in range(3)]
    Vs = [P.alloc(128, BF16) for _ in range(8)]
    for t_ in Vs:
        P.op("pool", lambda e, t_=t_: e.memset(t_.ap, 0.0), writes=[t_])
    qz = [[P.alloc(512, BF16) for _ in range(2)] for _ in range(2)]
    for t_ in qz[0] + qz[1]:
        P.op("pool", lambda e, t_=t_: e.memset(t_.ap, 0.0), writes=[t_])
    qz_of = {}
    wexp = P.alloc(4 * 32 * 8)
    ts("dve", wexp.ap, biasT.ap, 80.0, None, ALU.min, rd=[biasT], wr=[wexp])
    act(wexp.ap, wexp.ap, AF.Exp, rd=[wexp], wr=[wexp])
    ot_sb = [P.alloc(512) for _ in range(2)]
    for t_ in ot_sb:
        P.op("pool", lambda e, t_=t_: e.memset(t_.ap, 0.0), writes=[t_])
    rden_t = P.alloc(128)
    ssraw = P.alloc(128)
    rstd_t = P.alloc(128)
    o_sb = [P.alloc(64) for _ in range(2)]
    its = []
    for h in range(8):
        for G in range(4):
            if stop < 1.4 and (h, G) != (0, 0):
                continue
            nk = 20 + 4 * G
            for kp in range(nk // 2):
                its.append((h, G, kp, nk))
    S_of = {}
    vi = [0]

    def fox_S(n):
        h, G, kp, nk = its[n]
        hp, po = h // 2, (h % 2) * 64
        b = (0, 2, 4)[n % 3]
        if (h, G) not in qz_of:
            qt_ = qz[h % 2][len(qz_of) // 2 % 2] if False else qz[h % 2][(h // 2 * 4 + G) % 2]
            qsrc = QT[hp * NTOK + G * 512:hp * NTOK + (G + 1) * 512]
            P.op("dve", lambda e: e.tensor_copy(out=qt_.ap[po:po + 64, :], in_=qsrc.ap[po:po + 64, :]), reads=[qsrc], writes=[qt_])
            qz_of[(h, G)] = qt_
        qt_ = qz_of[(h, G)]
        for t in range(2):
            kb = 2 * kp + t
            sps = P.psum(b + t)
            mm(sps, KT.ap[:, hp * NALL + kb * 128:hp * NALL + (kb + 1) * 128], qt_.ap, True, True, [KT, qt_])
        S_of[n] = b

    def fox_rest(n):
        h, G, kp, nk = its[n]
        b = S_of.pop(n)
        ob = 6
        pt = PT3[n % 3]
        spair = V(P.ps[:, b * 512:(b + 2) * 512], "P", b * 2048, (b + 2) * 2048, 4)
        act(pt.ap, spair.ap, AF.Exp, rd=[spair], wr=[pt])
        for t in range(2):
            kb = 2 * kp + t
            j0 = max(0, kb - (16 + 4 * G))
            col0 = 128 * j0
            ncols = 512 - col0
            ptk = pt[t * 512 + col0:(t + 1) * 512]
            if kb >= 16 + 4 * G:
                pd = pt[t * 512 + col0:t * 512 + col0 + 128]
                tt("pool", pd.ap, pd.ap, causb.ap, ALU.mult, rd=[pd, causb], wr=[pd])
            vs = Vs[vi[0] % 8]
            vi[0] += 1
            bcol = G * 256 + kb * 8 + h
            vsl = Vaug[kb * 520 + h * 65:kb * 520 + (h + 1) * 65]
            ts("dve", vs.ap[:, 0:65], vsl.ap, wexp.ap[:, bcol:bcol + 1], None, ALU.mult, rd=[vsl, wexp], wr=[vs])
            ops_ = P.psum(ob, col0, ncols)
            P.op("pe", lambda e, ops_=ops_, vs=vs, ptk=ptk, kb=kb: e.matmul(ops_.ap, vs.ap, ptk.ap, start=(kb == 0), stop=(kb == nk - 1)),
                 reads=[vs, ptk], writes=[ops_])
        if 2 * kp + 1 != nk - 1 or stop < 1.3:
            return
        opsf = P.psum(ob)
        osb = ot_sb[(h * 4 + G) % 2]
        evac(osb, osb.ap[0:65, :], opsf, opsf.ap[0:65, :], "dve")
        tb_ = 7
        for j in range(4):
            ps = P.psum(tb_, j * 128, 128)
            tr(ps, osb, osb.ap[:, j * 128:(j + 1) * 128], ident, ident.ap)
        for j in range(4):
            qb = G * 4 + j
            c_ = qb * 8 + h
            ps = P.psum(tb_, j * 128, 128)
            P.op("dve", lambda e, ps=ps, c_=c_: e.reciprocal(out=rden_t.ap[:, c_:c_ + 1], in_=ps.ap[:, 64:65]),
                 reads=[ps], writes=[rden_t[c_:c_ + 1]])
            mo = mixed[qb * 1024 + h * 64:qb * 1024 + (h + 1) * 64]
            ts("dve", mo.ap, ps.ap[:, 0:64], rden_t.ap[:, c_:c_ + 1], None, ALU.mult, rd=[ps, rden_t[c_:c_ + 1]], wr=[mo])
            osj = o_sb[j % 2]
            ts("dve", osj.ap, ps.ap[:, 0:64], rden_t.ap[:, c_:c_ + 1], None, ALU.mult, rd=[ps, rden_t[c_:c_ + 1]], wr=[osj])
            stt("dve", junk.ap[:, 0:64], osj.ap, 1.0, osj.ap, ALU.mult, ALU.mult, rd=[osj],
                wr=[junk[0:64], ssraw[c_:c_ + 1]], acc=ssraw.ap[:, c_:c_ + 1])

    LA = 2
    for n in range(len(its) + LA):
        if n < len(its):
            fox_S(n)
        if n >= LA:
            fox_rest(n - LA)
    P.nbanks = 8
    if stop >= 1.4:
        rstd_from_ss(ssraw, 64, 1, rstd_t)
        for qb in range(16):
            for h in range(8):
                c_ = qb * 8 + h
                mo = mixed[qb * 1024 + h * 64:qb * 1024 + (h + 1) * 64]
                stt("dve", mo.ap, mo.ap, rstd_t.ap[:, c_:c_ + 1], foxg.ap[:, h * 64:(h + 1) * 64], ALU.mult, ALU.mult,
                    rd=[mo, rstd_t, foxg], wr=[mo])
    P.release(m_phase1)
    if stop < 2:
        return finish()

    m1b = P.mark()
    wmq = P.alloc(8 * 256, BF16)
    wmk = P.alloc(8 * 256, BF16)
    wmv = P.alloc(8 * 512, BF16)
    wmo = P.alloc(8 * 512, BF16)
    dma("pool", wmk.re("p (c n) -> p c n", c=8).ap, wview(w_in, 1800, 2056), writes=[wmk])
    dma("pool", wmv.re("p (c n) -> p c n", c=8).ap, wview(w_in, 2056, 2568), writes=[wmv])
    dma("pool", wmq.re("p (c n) -> p c n", c=8).ap, wview(w_in, 1544, 1800), writes=[wmq])
    dma("pool", wmo.re("p (c n) -> p c n", c=8).ap, wview(w_in, 2576, 3088), writes=[wmo])
    convT = P.alloc(16)
    dma("sp", convT.ap, convT_d, writes=[convT])
    mlg = P.alloc(512)
    dma("sp", mlg.ap, mlg_d.partition_broadcast(128), writes=[mlg])
    xs_b = [P.alloc(1024) for _ in range(4)]
    xTb = [P.alloc(8 * 512, BF16) for _ in range(2)]
    kbuf = [P.alloc(515) for _ in range(2)]
    qbuf = [P.alloc(515) for _ in range(2)]
    for t_ in kbuf + qbuf:
        P.op("pool", lambda e, t_=t_: e.memset(t_.ap, 0.0), writes=[t_])
    cacc = P.alloc(512)
    ksil2 = [[P.alloc(512, BF16) for _ in range(2)] for _ in range(2)]
    qsil2 = [[P.alloc(512, BF16) for _ in range(2)] for _ in range(2)]
    ktok2 = [[P.alloc(256) for _ in range(4)] for _ in range(2)]
    vaug2 = [[P.alloc(4 * 129, BF16) for _ in range(4)] for _ in range(2)]
    for t_ in vaug2[0] + vaug2[1]:
        P.op("pool", lambda e, t_=t_: e.memset(t_.ap, 1.0), writes=[t_])
    osig2 = [[P.alloc(512) for _ in range(4)] for _ in range(2)]
    Cst = [P.alloc(129) for _ in range(4)]
    Cbf = [P.alloc(129, BF16) for _ in range(4)]
    for t_ in Cst + Cbf:
        P.op("pool", lambda e, t_=t_: e.memset(t_.ap, 0.0), writes=[t_])
    g4s = [P.alloc(32) for _ in range(4)]
    diagb4 = [P.alloc(128) for _ in range(4)]
    DT4 = [P.alloc(128) for _ in range(4)]
    PTm4 = [P.alloc(128, BF16) for _ in range(4)]
    kw0s = [P.alloc(64, BF16) for _ in range(2)]
    kw1s = [P.alloc(128, BF16) for _ in range(2)]
    for t_ in kw1s:
        P.op("pool", lambda e, t_=t_: e.memset(t_.ap, 0.0), writes=[t_])
    hun4 = [P.alloc(129) for _ in range(4)]
    hstore = P.alloc(16 * 128)
    ssm = P.alloc(16)
    rsm = P.alloc(16)
    dn4 = P.alloc(4)

    def conv_silu(buf, psrc, chunk, outv, scale):
        evac(buf[3:515], buf.ap[:, 3:515], psrc, psrc.ap)
        ts("dve", cacc.ap, buf.ap[:, 0:512], convT.ap[:, chunk * 4:chunk * 4 + 1], None, ALU.mult, rd=[buf, convT], wr=[cacc])
        for j in range(1, 4):
            stt("dve", cacc.ap, buf.ap[:, j:j + 512], convT.ap[:, chunk * 4 + j:chunk * 4 + j + 1], cacc.ap, ALU.mult, ALU.add,
                rd=[buf, convT, cacc], wr=[cacc])
        evac(buf[0:3], buf.ap[:, 0:3], buf[512:515], buf.ap[:, 512:515], "pool")
        if scale is None:
            act(outv.ap, cacc.ap, AF.Silu, rd=[cacc], wr=[outv])
        else:
            act(cacc.ap, cacc.ap, AF.Silu, rd=[cacc], wr=[cacc])
            ts("dve", outv.ap, cacc.ap, scale, None, ALU.mult, rd=[cacc], wr=[outv])

    def ml_proj(tb):
        own = tb >= 4
        xT = xTb[tb % 2]
        ksil, qsil, ktok, vaug, osig = ksil2[tb % 2], qsil2[tb % 2], ktok2[tb % 2], vaug2[tb % 2], osig2[tb % 2]
        load_x_block(tb, xs_b)
        transpose_block(xs_b, xT)
        for cc in range(2):
            ps = P.psum(P.nb())
            for c in range(8):
                mm(ps, wmk.ap[:, c * 256 + cc * 128:c * 256 + (cc + 1) * 128], xT.ap[:, c * 512:(c + 1) * 512],
                   c == 0, c == 7, [wmk, xT])
            conv_silu(kbuf[cc], ps, 2 + cc, ksil[cc], 0.125)
            if tb >= 3:
                ps = P.psum(P.nb())
                for c in range(8):
                    mm(ps, wmq.ap[:, c * 256 + cc * 128:c * 256 + (cc + 1) * 128], xT.ap[:, c * 512:(c + 1) * 512],
                       c == 0, c == 7, [wmq, xT])
                conv_silu(qbuf[cc], ps, cc, qsil[cc], None)
        for i in range(4):
            for cc in range(2):
                ps = P.psum(P.nb(), 0, 128, BF16)
                tr(ps, ksil[cc][i * 128:(i + 1) * 128], ksil[cc].ap[:, i * 128:(i + 1) * 128], identb, identb.ap)
                o = ktok[i][cc * 128:(cc + 1) * 128]
                evac(o, o.ap, ps, ps.ap)
            ps = P.psum(P.nb())
            for c in range(8):
                mm(ps, xT.ap[:, c * 512 + i * 128:c * 512 + (i + 1) * 128], wmv.ap[:, c * 512:(c + 1) * 512],
                   c == 0, c == 7, [xT, wmv])
            evac(vaug[i], vaug[i].ap.rearrange("p (h d) -> p h d", d=129)[:, :, 0:128], ps,
                 ps.ap.rearrange("p (h d) -> p h d", d=128))
            if own:
                ps = P.psum(P.nb())
                for c in range(8):
                    mm(ps, xT.ap[:, c * 512 + i * 128:c * 512 + (i + 1) * 128], wmo.ap[:, c * 512:(c + 1) * 512],
                       c == 0, c == 7, [xT, wmo])
                act(osig[i].ap, ps.ap, AF.Sigmoid, rd=[ps], wr=[osig[i]])
    def ml_chunks(tb):
        own = tb >= 4
        ksil, qsil, ktok, vaug, osig = ksil2[tb % 2], qsil2[tb % 2], ktok2[tb % 2], vaug2[tb % 2], osig2[tb % 2]
        for i in range(4):
            blk = tb * 4 + i
            qb = blk - 16
            fcol = blk * 16 + 12
            icol = blk * 16 + 8
            g4 = g4s[i]
            tt("dve", g4.ap[:, 16:20], tot.ap[:, fcol:fcol + 4], cs.ap[:, fcol:fcol + 4], ALU.subtract, rd=[tot, cs], wr=[g4[16:20]])
            tt("dve", g4.ap[:, 16:20], g4.ap[:, 16:20], gpre.ap[:, icol:icol + 4], ALU.add, rd=[g4[16:20], gpre], wr=[g4[16:20]])
            act(g4.ap[:, 4:8], g4.ap[:, 16:20], AF.Exp, rd=[g4[16:20]], wr=[g4[4:8]])
            act(g4.ap[:, 8:12], tot.ap[:, fcol:fcol + 4], AF.Exp, rd=[tot], wr=[g4[8:12]])
            if own:
                act(g4.ap[:, 0:4], cs.ap[:, fcol:fcol + 4], AF.Exp, rd=[cs], wr=[g4[0:4]])
                tt("dve", g4.ap[:, 12:16], gpre.ap[:, icol:icol + 4], cs.ap[:, fcol:fcol + 4], ALU.subtract, rd=[gpre, cs], wr=[g4[12:16]])
            H4 = range(4)
            vas = [vaug[i][hh * 129:(hh + 1) * 129] for hh in H4]
            ccs = [hh // 2 for hh in H4]
            pos = [(hh % 2) * 64 for hh in H4]
            if own:
                for hh in H4:
                    ts("dve", diagb4[hh].ap, ident.ap, cs.ap[:, fcol + hh:fcol + hh + 1], None, ALU.mult, rd=[ident, cs], wr=[diagb4[hh]])
                eps4 = []
                for hh in H4:
                    eps_ = P.psum(P.nb(), 0, 128)
                    mm(eps_, ones.ap, diagb4[hh].ap, True, False, [ones, diagb4[hh]])
                    mm(eps_, ident.ap, maskneg.ap, False, True, [ident, maskneg])
                    eps4.append(eps_)
                for hh in H4:
                    act(DT4[hh].ap, eps4[hh].ap, AF.Exp, rd=[eps4[hh], g4[12:16]], wr=[DT4[hh]], bias=g4.ap[:, 12 + hh:13 + hh])
                sps4 = []
                for hh in H4:
                    cc, po = ccs[hh], pos[hh]
                    sps = P.psum(P.nb(), 0, 128)
                    mm(sps, ksil[cc].ap[po:po + 64, i * 128:(i + 1) * 128], qsil[cc].ap[po:po + 64, i * 128:(i + 1) * 128],
                       True, True, [ksil[cc], qsil[cc]])
                    sps4.append(sps)
                for hh in H4:
                    tt("dve", PTm4[hh].ap, DT4[hh].ap, sps4[hh].ap, ALU.mult, rd=[DT4[hh], sps4[hh]], wr=[PTm4[hh]])
                n12 = []
                for hh in H4:
                    cc, po = ccs[hh], pos[hh]
                    n2 = P.psum(P.nb(), 0, 129)
                    mm(n2, qsil[cc].ap[po:po + 64, i * 128:(i + 1) * 128], Cbf[hh].ap[po:po + 64, :], True, True, [qsil[cc], Cbf[hh]])
                    n12.append(n2)
                for hh in H4:
                    ts("dve", hun4[hh].ap, n12[hh].ap, g4.ap[:, hh:hh + 1], None, ALU.mult, rd=[n12[hh], g4[0:4]], wr=[hun4[hh]])
                n11 = []
                for hh in H4:
                    n1 = P.psum(P.nb(), 0, 129)
                    mm(n1, PTm4[hh].ap, vas[hh].ap, True, True, [PTm4[hh], vas[hh]])
                    n11.append(n1)
                for hh in H4:
                    tt("dve", hun4[hh].ap, hun4[hh].ap, n11[hh].ap, ALU.add, rd=[hun4[hh], n11[hh]], wr=[hun4[hh]])
                for hh in H4:
                    u = i * 4 + hh
                    hs = hstore[u * 128:(u + 1) * 128]
                    d1 = dn4[hh:hh + 1]
                    stt("dve", d1.ap, hun4[hh].ap[:, 128:129], -1.0, hun4[hh].ap[:, 128:129], ALU.mult, ALU.max, rd=[hun4[hh]], wr=[d1])
                    ts("dve", d1.ap, d1.ap, 1.0, None, ALU.max, rd=[d1], wr=[d1])
                    P.op("dve", lambda e, d1=d1: e.reciprocal(out=d1.ap, in_=d1.ap), reads=[d1], writes=[d1])
                    ts("dve", hs.ap, hun4[hh].ap[:, 0:128], d1.ap, None, ALU.mult, rd=[hun4[hh], d1], wr=[hs])
                    stt("dve", junk.ap[:, 0:128], hs.ap, 1.0, hs.ap, ALU.mult, ALU.mult, rd=[hs], wr=[junk[0:128], ssm[u:u + 1]],
                        acc=ssm.ap[:, u:u + 1])
            dps4 = []
            for hh in H4:
                po = pos[hh]
                dps = P.psum(P.nb(), 0, 129)
                if po == 0:
                    kw0 = kw0s[hh // 2]
                    ts("dve", kw0.ap, ktok[i].ap[:, hh * 64:(hh + 1) * 64], g4.ap[:, 4 + hh:5 + hh], None, ALU.mult,
                       rd=[ktok[i], g4[4:8]], wr=[kw0])
                    P.op("pe", lambda e, dps=dps, va=vas[hh], kw0=kw0: e.matmul(dps.ap[0:64, :], kw0.ap, va.ap, start=True, stop=True),
                         reads=[kw0, vas[hh]], writes=[dps])
                else:
                    kw1 = kw1s[hh // 2]
                    ts("dve", kw1.ap[:, 64:128], ktok[i].ap[:, hh * 64:(hh + 1) * 64], g4.ap[:, 4 + hh:5 + hh], None, ALU.mult,
                       rd=[ktok[i], g4[4:8]], wr=[kw1])
                    P.op("pe", lambda e, dps=dps, va=vas[hh], kw1=kw1: e.matmul(dps.ap, kw1.ap, va.ap, start=True, stop=True),
                         reads=[kw1, vas[hh]], writes=[dps])
                dps4.append(dps)
            for hh in H4:
                po = pos[hh]
                stt("dve", Cst[hh].ap[po:po + 64, :], Cst[hh].ap[po:po + 64, :], g4.ap[po:po + 64, 8 + hh:9 + hh], dps4[hh].ap[po:po + 64, :],
                    ALU.mult, ALU.add, rd=[Cst[hh], g4[8:12], dps4[hh]], wr=[Cst[hh]])
            for hh in H4:
                po = pos[hh]
                evac(Cbf[hh], Cbf[hh].ap[po:po + 64, :], Cst[hh], Cst[hh].ap[po:po + 64, :], "pool")
        if own:
            rstd_from_ss(ssm, 128, 1, rsm)
            for i in range(4):
                qb = tb * 4 + i - 16
                for hh in range(4):
                    u = i * 4 + hh
                    hs = hstore[u * 128:(u + 1) * 128]
                    stt("dve", hs.ap, hs.ap, rsm.ap[:, u:u + 1], mlg.ap[:, hh * 128:(hh + 1) * 128], ALU.mult, ALU.mult,
                        rd=[hs, rsm, mlg], wr=[hs])
                    mo = mixed[qb * 1024 + 512 + hh * 128:qb * 1024 + 512 + (hh + 1) * 128]
                    tt("pool", mo.ap, hs.ap, osig[i].ap[:, hh * 128:(hh + 1) * 128], ALU.mult, rd=[hs, osig[i]], wr=[mo])
    ml_proj(0)
    for tb in range(8):
        if tb + 1 < 8:
            ml_proj(tb + 1)
        ml_chunks(tb)
    P.release(m1b)
    P.release(m_gates)
    if stop < 3:
        return finish()

    mdm = P.mark()
    dump_mixed()
    P.release(mdm)

    m2 = P.mark()
    lnp2 = P.alloc(4 * 1024)
    dma("sp", lnp2.ap, ln_d[:, 0:4096].partition_broadcast(128), writes=[lnp2])
    lnp_box[0], lnp_box[1] = lnp2, 0
    wmix = P.alloc(8 * 1024, BF16)
    wxq = P.alloc(8 * 1024, BF16)
    wxo = P.alloc(8 * 1024, BF16)
    dma("pool", wmix.re("p (c n) -> p c n", c=8).ap, wview(w_mix, 0, 1024), writes=[wmix])
    wr_sb = P.alloc(8 * 32)
    dma("sp", wr_sb.re("p (c n) -> p c n", c=8).ap, wview(w_r, 0, 32), writes=[wr_sb])
    br_bc = P.alloc(32)
    dma("sp", br_bc.ap, b_r.partition_broadcast(128), writes=[br_bc])
    KmT = P.alloc(8 * 256, BF16)
    Vm = P.alloc(2 * 4 * 257, BF16)
    P.op("pool", lambda e: e.memset(Vm.ap, 1.0), writes=[Vm])
    macc = P.alloc(32)
    P.op("pool", lambda e: e.memset(macc.ap, 0.0), writes=[macc])
    m2a = P.mark()
    wtmp = P.alloc(8 * 1024, BF16)
    mems = [P.alloc(1024) for _ in range(2)]
    memT = P.alloc(8 * 256, BF16)
    for mt in range(2):
        dma("sp", mems[mt].ap, memb[mt * 128:(mt + 1) * 128, :], writes=[mems[mt]])
    for c in range(8):
        b = P.nb()
        for mt in range(2):
            ps = P.psum(b, mt * 128, 128)
            tr(ps, mems[mt][c * 128:(c + 1) * 128], mems[mt].ap[:, c * 128:(c + 1) * 128], ident, ident.ap)
        psf = P.psum(b, 0, 256)
        evac(memT[c * 256:(c + 1) * 256], memT.ap[:, c * 256:(c + 1) * 256], psf, psf.ap)
    dma("pool", wtmp.re("p (c n) -> p c n", c=8).ap, wview(w_xk, 0, 1024), writes=[wtmp])
    for jc in range(8):
        ps = P.psum(P.nb(), 0, 256)
        for c in range(8):
            mm(ps, wtmp.ap[:, c * 1024 + jc * 128:c * 1024 + (jc + 1) * 128], memT.ap[:, c * 256:(c + 1) * 256],
               c == 0, c == 7, [wtmp, memT])
        o = KmT[jc * 256:(jc + 1) * 256]
        evac(o, o.ap, ps, ps.ap)
    dma("pool", wtmp.re("p (c n) -> p c n", c=8).ap, wview(w_xv, 0, 1024), reads=[], writes=[wtmp])
    for mt in range(2):
        for half in range(2):
            ps = P.psum(P.nb())
            for c in range(8):
                mm(ps, memT.ap[:, c * 256 + mt * 128:c * 256 + (mt + 1) * 128], wtmp.ap[:, c * 1024 + half * 512:c * 1024 + (half + 1) * 512],
                   c == 0, c == 7, [memT, wtmp])
            o = Vm[mt * 1028 + half * 514:mt * 1028 + (half + 1) * 514]
            evac(o, o.ap.rearrange("p (h d) -> p h d", d=257)[:, :, 0:256], ps, ps.ap.rearrange("p (h d) -> p h d", d=256))
    P.release(m2a)
    dma("pool", wxq.re("p (c n) -> p c n", c=8).ap, wview(w_xq, 0, 1024), writes=[wxq])
    dma("pool", wxo.re("p (c n) -> p c n", c=8).ap, wview(w_xo, 0, 1024), writes=[wxo])

    mT = P.alloc(8 * 128, BF16)
    x1s = [[P.alloc(1024) for _ in range(4)] for _ in range(2)]
    ytmpA = P.alloc(1024)
    ytmpC = P.alloc(1024)
    ltA = P.alloc(8)
    ltC = P.alloc(8)
    xres = P.alloc(1024)
    x1T = P.alloc(8 * 512, BF16)
    oT = P.alloc(8 * 512, BF16)
    qT = P.alloc(8 * 512, BF16)
    PTx = [P.alloc(512, BF16) for _ in range(2)]
    oatt = [P.alloc(1024, BF16) for _ in range(4)]
    x2 = P.alloc(1024)
    x2b = P.alloc(1024, BF16)
    x2T = ytmpC
    lg = P.alloc(32)
    m8 = P.alloc(8)
    msk = P.alloc(32)
    ex = P.alloc(32)
    rt = P.alloc(8)
    rtB = P.alloc(8)
    Dm = P.alloc(32)
    d8 = P.alloc(8)

    def stageA(grp):
        x1 = x1s[grp % 2]
        for i in range(4):
            qb = grp * 4 + i
            b = P.nb()
            for c in range(8):
                ps = P.psum(b, c * 128, 128, BF16)
                msl = mixed[qb * 1024 + c * 128:qb * 1024 + (c + 1) * 128]
                tr(ps, msl, msl.ap, identb, identb.ap)
            psf = P.psum(b, 0, 1024, BF16)
            evac(mT, mT.ap, psf, psf.ap)
            dma("sp", xres.ap, xcat[NTOK + qb * 128:NTOK + (qb + 1) * 128, :], writes=[xres])
            for half in range(2):
                ps = P.psum(P.nb())
                for c in range(8):
                    mm(ps, mT.ap[:, c * 128:(c + 1) * 128], wmix.ap[:, c * 1024 + half * 512:c * 1024 + (half + 1) * 512],
                       c == 0, c == 7, [mT, wmix])
                stt("dve", ytmpA.ap[:, half * 512:(half + 1) * 512], xres.ap[:, half * 512:(half + 1) * 512], ALPHA, ps.ap,
                    ALU.mult, ALU.add, rd=[xres, ps], wr=[ytmpA[half * 512:(half + 1) * 512]])
            layer_norm(ytmpA, 0, x1[i], ltA)
            if dbg:
                dma("sp", dbg_x1[qb * 128:(qb + 1) * 128, :], x1[i].ap, reads=[x1[i]])

    def stageB1(grp):
        x1 = x1s[grp % 2]
        for c in range(8):
            b = P.nb()
            for i in range(4):
                ps = P.psum(b, i * 128, 128)
                tr(ps, x1[i][c * 128:(c + 1) * 128], x1[i].ap[:, c * 128:(c + 1) * 128], ident, ident.ap)
            psf = P.psum(b)
            evac(x1T[c * 512:(c + 1) * 512], x1T.ap[:, c * 512:(c + 1) * 512], psf, psf.ap)

    def stageB2(grp):
        for jc in range(8):
            ps = P.psum(P.nb())
            for c in range(8):
                mm(ps, wxq.ap[:, c * 1024 + jc * 128:c * 1024 + (jc + 1) * 128], x1T.ap[:, c * 512:(c + 1) * 512],
                   c == 0, c == 7, [wxq, x1T])
            o = qT[jc * 512:(jc + 1) * 512]
            evac(o, o.ap, ps, ps.ap)

    def stageB3(grp):
        for hx in range(4):
            pts = []
            for mt in range(2):
                ps = P.psum(P.nb())
                for k2 in range(2):
                    jc = hx * 2 + k2
                    mm(ps, KmT.ap[:, jc * 256 + mt * 128:jc * 256 + (mt + 1) * 128], qT.ap[:, jc * 512:(jc + 1) * 512],
                       k2 == 0, k2 == 1, [KmT, qT])
                pt = PTx[mt]
                act(pt.ap, ps.ap, AF.Exp, rd=[ps], wr=[pt], scale=1.0 / 16.0)
                pts.append(pt)
            for i in range(4):
                ps = P.psum(P.nb(), 0, 257)
                for mt in range(2):
                    vsl = Vm[mt * 1028 + hx * 257:mt * 1028 + (hx + 1) * 257]
                    mm(ps, pts[mt].ap[:, i * 128:(i + 1) * 128], vsl.ap, mt == 0, mt == 1, [pts[mt], vsl])
                P.op("dve", lambda e, ps=ps: e.reciprocal(out=rtB.ap[:, 0:1], in_=ps.ap[:, 256:257]), reads=[ps], writes=[rtB[0:1]])
                o = oatt[i][hx * 256:(hx + 1) * 256]
                ts("dve", o.ap, ps.ap[:, 0:256], rtB.ap[:, 0:1], None, ALU.mult, rd=[ps, rtB[0:1]], wr=[o])

    def stageB4(grp):
        for c in range(8):
            b = P.nb()
            for i in range(4):
                ps = P.psum(b, i * 128, 128, BF16)
                tr(ps, oatt[i][c * 128:(c + 1) * 128], oatt[i].ap[:, c * 128:(c + 1) * 128], identb, identb.ap)
            psf = P.psum(b, 0, 512, BF16)
            evac(oT[c * 512:(c + 1) * 512], oT.ap[:, c * 512:(c + 1) * 512], psf, psf.ap)

    def stageC1(grp, i):
        x1 = x1s[grp % 2]
        qb = grp * 4 + i
        for half in range(2):
            ps = P.psum(P.nb())
            for c in range(8):
                mm(ps, oT.ap[:, c * 512 + i * 128:c * 512 + (i + 1) * 128], wxo.ap[:, c * 1024 + half * 512:c * 1024 + (half + 1) * 512],
                   c == 0, c == 7, [oT, wxo])
            stt("dve", ytmpC.ap[:, half * 512:(half + 1) * 512], x1[i].ap[:, half * 512:(half + 1) * 512], ALPHA, ps.ap,
                ALU.mult, ALU.add, rd=[x1[i], ps], wr=[ytmpC[half * 512:(half + 1) * 512]])
        layer_norm(ytmpC, 2, x2, ltC)
        dma("sp", X2[qb * 128:(qb + 1) * 128, :], x2.ap, reads=[x2], writes=[("X2", qb * 128, (qb + 1) * 128)])
        if dbg:
            dma("sp", dbg_x2[qb * 128:(qb + 1) * 128, :], x2.ap, reads=[x2])
        evac(x2b, x2b.ap, x2, x2.ap, "pool")

    def stageC2(grp, i):
        qb = grp * 4 + i
        for half in range(2):
            b = P.nb()
            for c4 in range(4):
                c = half * 4 + c4
                ps = P.psum(b, c4 * 128, 128)
                tr(ps, x2[c * 128:(c + 1) * 128], x2.ap[:, c * 128:(c + 1) * 128], ident, ident.ap)
            psf = P.psum(b)
            evac(x2T[half * 512:(half + 1) * 512], x2T.ap[:, half * 512:(half + 1) * 512], psf, psf.ap)
        ps = P.psum(P.nb(), 0, 32)
        for c in range(8):
            mm(ps, x2T.ap[:, c * 128:(c + 1) * 128], wr_sb.ap[:, c * 32:(c + 1) * 32], c == 0, c == 7, [x2T, wr_sb])
        tt("dve", lg.ap, ps.ap, br_bc.ap, ALU.add, rd=[ps, br_bc], wr=[lg])
        P.op("dve", lambda e: e.max(out=m8.ap, in_=lg.ap), reads=[lg], writes=[m8])
        ts("dve", msk.ap, lg.ap, m8.ap[:, 3:4], None, ALU.is_ge, rd=[lg, m8], wr=[msk])
        ts("dve", rt.ap[:, 1:2], m8.ap[:, 0:1], -1.0, None, ALU.mult, rd=[m8], wr=[rt[1:2]])
        act(ex.ap, lg.ap, AF.Exp, rd=[lg, rt[1:2]], wr=[ex], bias=rt.ap[:, 1:2])
        stt("dve", ex.ap, ex.ap, 1.0, msk.ap, ALU.mult, ALU.mult, rd=[ex, msk], wr=[ex, rt[2:3]], acc=rt.ap[:, 2:3])
        P.op("dve", lambda e: e.reciprocal(out=rt.ap[:, 2:3], in_=rt.ap[:, 2:3]), reads=[rt[2:3]], writes=[rt[2:3]])
        gf = gfull[qb * 32:(qb + 1) * 32]
        ts("dve", gf.ap, ex.ap, rt.ap[:, 2:3], None, ALU.mult, rd=[ex, rt[2:3]], wr=[gf])
        ps = P.psum(P.nb(), 0, 32)
        mm(ps, Ult.ap, msk.ap, True, False, [Ult, msk])
        mm(ps, ones.ap, macc.ap, False, True, [ones, macc])
        ts("dve", Dm.ap, ps.ap, float(CAP - 1), None, ALU.min, rd=[ps], wr=[Dm])
        tt("dve", Dm.ap, Dm.ap, ebase.ap, ALU.add, rd=[Dm, ebase], wr=[Dm])
        tt("dve", Dm.ap, Dm.ap, msk.ap, ALU.mult, rd=[Dm, msk], wr=[Dm])
        tt("pool", macc.ap, macc.ap, msk.ap, ALU.add, rd=[macc, msk], wr=[macc])
        P.op("dve", lambda e: e.max(out=d8.ap, in_=Dm.ap), reads=[Dm], writes=[d8])
        de = dest_all[qb * 4:(qb + 1) * 4]
        ts("dve", de.ap, d8.ap[:, 0:4], -1.0, None, ALU.add, rd=[d8], wr=[de])
        for k in range(4):
            ga = gate_all[qb * 4 + k:qb * 4 + k + 1]
            stt("dve", junk.ap[:, 0:32], Dm.ap, d8.ap[:, k:k + 1], gf.ap, ALU.is_equal, ALU.mult, rd=[Dm, d8, gf],
                wr=[junk[0:32], ga], acc=ga.ap)
            P.op("pool", lambda e, k=k, de=de: e.indirect_dma_start(
                out=Xg, out_offset=bass.IndirectOffsetOnAxis(ap=de.ap[:, k:k + 1], axis=0), in_=x2b.ap, in_offset=None),
                reads=[x2b, de], writes=[("Xg", 0, NE * CAP)], dma=True)

    stageA(0)
    stageB1(0)
    stageB2(0)
    stageB3(0)
    stageB4(0)
    for grp in range(4):
        nxt = grp + 1 < 4
        if nxt:
            stageA(grp + 1)
        Bs = [stageB1, stageB2, stageB3, stageB4]
        for i in range(4):
            stageC1(grp, i)
            if nxt:
                Bs[i](grp + 1)
            stageC2(grp, i)
    P.release(m2)
    if stop < 4:
        return finish()

    m3 = P.mark()
    bguT = P.alloc(NE * 16)
    dma("sp", bguT.ap, bguT_d, writes=[bguT])
    bguS = P.alloc(NE * 16)
    ts("dve", bguS.ap, bguT.ap, 1.0 / 1.702, None, ALU.mult, rd=[bguT], wr=[bguS])
    wgu2 = [P.alloc(8 * 2048, BF16) for _ in range(2)]
    wd2 = [P.alloc(8 * 1024, BF16) for _ in range(2)]
    Xe2 = [[P.alloc(1024, BF16) for _ in range(3)] for _ in range(2)]
    XeT2 = [P.alloc(8 * CAP, BF16) for _ in range(2)]
    hidT = P.alloc(8 * CAP, BF16)
    gt2 = [P.alloc(CAP) for _ in range(2)]
    sg2 = [P.alloc(CAP) for _ in range(2)]
    ln2 = [P.alloc(CAP) for _ in range(2)]
    Ysb = [P.alloc(1024, BF16) for _ in range(2)]

    def load_wgu(en):
        if en >= NE:
            return
        wgu_ = wgu2[en % 2]
        for c4 in range(4):
            sl_ = wgu_[c4 * 4096:(c4 + 1) * 4096]
            dma("pool", sl_.ap.rearrange("p (c n) -> p c n", c=2),
                w_gu[en, c4 * 256:(c4 + 1) * 256, :].rearrange("(c p) n -> p c n", p=128), writes=[sl_])

    def load_wd(en):
        if en >= NE:
            return
        wd_ = wd2[en % 2]
        for c4 in range(2):
            sl_ = wd_[c4 * 4096:(c4 + 1) * 4096]
            dma("pool", sl_.ap.rearrange("p (c n) -> p c n", c=4),
                w_dn[en, c4 * 512:(c4 + 1) * 512, :].rearrange("(c p) n -> p c n", p=128), writes=[sl_])

    def load_xe(en):
        if en >= NE:
            return
        for s in range(3):
            r0 = en * CAP + s * 128
            dma("sp", Xe2[en % 2][s].ap, Xg[r0:r0 + 128, :], reads=[("Xg", r0, r0 + 128)], writes=[Xe2[en % 2][s]])

    def xe_transposes(en):
        if en >= NE:
            return
        Xe, XeT = Xe2[en % 2], XeT2[en % 2]
        for c in range(8):
            b = P.nb()
            for s in range(3):
                ps = P.psum(b, s * 128, 128, BF16)
                tr(ps, Xe[s][c * 128:(c + 1) * 128], Xe[s].ap[:, c * 128:(c + 1) * 128], identb, identb.ap)
            psf = P.psum(b, 0, CAP, BF16)
            evac(XeT[c * CAP:(c + 1) * CAP], XeT.ap[:, c * CAP:(c + 1) * CAP], psf, psf.ap)

    load_wgu(0)
    load_wd(0)
    load_wgu(1)
    load_wd(1)
    load_xe(0)
    xe_transposes(0)
    yi = 0
    for ex_ in range(NE):
        wgu = wgu2[ex_ % 2]
        wd = wd2[ex_ % 2]
        XeT = XeT2[ex_ % 2]
        load_xe(ex_ + 1)
        for g in range(8):
            psg = P.psum(P.nb(), 0, CAP)
            for c in range(8):
                mm(psg, wgu.ap[:, c * 2048 + g * 128:c * 2048 + (g + 1) * 128], XeT.ap[:, c * CAP:(c + 1) * CAP],
                   c == 0, c == 7, [wgu[c * 2048:(c + 1) * 2048], XeT])
            psl = P.psum(P.nb(), 0, CAP)
            for c in range(8):
                mm(psl, wgu.ap[:, c * 2048 + 1024 + g * 128:c * 2048 + 1024 + (g + 1) * 128], XeT.ap[:, c * CAP:(c + 1) * CAP],
                   c == 0, c == 7, [wgu[c * 2048:(c + 1) * 2048], XeT])
            gt, sg, ln = gt2[g % 2], sg2[g % 2], ln2[g % 2]
            bg = bguT[ex_ * 16 + g:ex_ * 16 + g + 1]
            bl = bguT[ex_ * 16 + 8 + g:ex_ * 16 + 8 + g + 1]
            ts("dve", gt.ap, psg.ap, bg.ap, 7.0, ALU.add, ALU.min, rd=[psg, bg], wr=[gt])
            act(sg.ap, gt.ap, AF.Silu, rd=[gt], wr=[sg], scale=1.702)
            bls = bguS[ex_ * 16 + 8 + g:ex_ * 16 + 8 + g + 1]
            act(ln.ap, psl.ap, AF.Identity, rd=[psl, bls], wr=[ln], bias=bls.ap, scale=1.0 / 1.702)
            ts("dve", ln.ap, ln.ap, 7.0 / 1.702, -7.0 / 1.702, ALU.min, ALU.max, rd=[ln], wr=[ln])
            o = hidT[g * CAP:(g + 1) * CAP]
            stt("dve", o.ap, ln.ap, 1.0 / 1.702, sg.ap, ALU.add, ALU.mult, rd=[ln, sg], wr=[o])
        load_wgu(ex_ + 2)
        xe_transposes(ex_ + 1)
        for s in range(3):
            ysb = Ysb[yi % 2]
            yi += 1
            for half in range(2):
                ps = P.psum(P.nb())
                for g in range(8):
                    mm(ps, hidT.ap[:, g * CAP + s * 128:g * CAP + (s + 1) * 128], wd.ap[:, g * 1024 + half * 512:g * 1024 + (half + 1) * 512],
                       g == 0, g == 7, [hidT, wd[g * 1024:(g + 1) * 1024]])
                evac(ysb[half * 512:(half + 1) * 512], ysb.ap[:, half * 512:(half + 1) * 512], ps, ps.ap)
            r0 = ex_ * CAP + s * 128
            dma("sp", Yg[r0:r0 + 128, :], ysb.ap, reads=[ysb], writes=[("Yg", r0, r0 + 128)])
        load_wd(ex_ + 2)
    P.release(m3)
    if stop < 5:
        return finish()

    lnp4 = P.alloc(2 * 1024)
    dma("sp", lnp4.ap, ln_d[:, 4096:6144].partition_broadcast(128), writes=[lnp4])
    lnp_box[0], lnp_box[1] = lnp4, 4
    bdn = P.alloc(1024)
    dma("sp", bdn.ap[0:32, :], b_dn, writes=[bdn])
    Gk2 = [[P.alloc(1024, BF16) for _ in range(4)] for _ in range(2)]
    acc2 = [P.alloc(1024) for _ in range(2)]
    x2r2 = [P.alloc(1024) for _ in range(2)]
    gT = P.alloc(128)
    outt2 = [P.alloc(1024) for _ in range(2)]
    lt4 = P.alloc(8)

    def comb_loads(qb):
        de = dest_all[qb * 4:(qb + 1) * 4]
        Gk = Gk2[qb % 2]
        for k in range(4):
            P.op("pool", lambda e, k=k, de=de, Gk=Gk: e.indirect_dma_start(
                out=Gk[k].ap, out_offset=None, in_=Yg, in_offset=bass.IndirectOffsetOnAxis(ap=de.ap[:, k:k + 1], axis=0)),
                reads=[("Yg", 0, NE * CAP), de], writes=[Gk[k]], dma=True)
        dma("sp", x2r2[qb % 2].ap, X2[qb * 128:(qb + 1) * 128, :], reads=[("X2", qb * 128, (qb + 1) * 128)], writes=[x2r2[qb % 2]])

    comb_loads(0)
    for qb in range(16):
        if qb + 1 < 16:
            comb_loads(qb + 1)
        Gk, acc, x2r, outt = Gk2[qb % 2], acc2[qb % 2], x2r2[qb % 2], outt2[qb % 2]
        ga = gate_all[qb * 4:(qb + 1) * 4]
        ts("dve", acc.ap, Gk[0].ap, ga.ap[:, 0:1], None, ALU.mult, rd=[Gk[0], ga], wr=[acc])
        for k in range(1, 4):
            stt("dve", acc.ap, Gk[k].ap, ga.ap[:, k:k + 1], acc.ap, ALU.mult, ALU.add, rd=[Gk[k], ga, acc], wr=[acc])
        gf = gfull[qb * 32:(qb + 1) * 32]
        ps = P.psum(P.nb(), 0, 128)
        P.op("pe", lambda e, ps=ps, gf=gf: e.transpose(ps.ap[0:32, :], gf.ap, ident.ap), reads=[gf, ident], writes=[ps])
        evac(gT, gT.ap[0:32, :], ps, ps.ap[0:32, :], "dve")
        for half in range(2):
            ps = P.psum(P.nb())
            mm(ps, gT.ap[0:32, :], bdn.ap[0:32, half * 512:(half + 1) * 512], True, True, [gT, bdn])
            tt("dve", acc.ap[:, half * 512:(half + 1) * 512], acc.ap[:, half * 512:(half + 1) * 512], ps.ap, ALU.add,
               rd=[acc[half * 512:(half + 1) * 512], ps], wr=[acc[half * 512:(half + 1) * 512]])
        stt("dve", acc.ap, x2r.ap, ALPHA, acc.ap, ALU.mult, ALU.add, rd=[x2r, acc], wr=[acc])
        layer_norm(acc, 4, outt, lt4)
        dma("sp", out_d[qb * 128:(qb + 1) * 128, :], outt.ap, reads=[outt])

    return finish()


_CACHE = {}


def _consts():
    c = np.zeros((128, 800), np.float32)
    idx = np.arange(128)
    c[:, 0:128] = np.eye(128)
    c[:, 128:256] = (idx[:, None] <= idx[None, :])
    c[:, 256:384] = (idx[:, None] > idx[None, :])
    c[:, 384:512] = 1.0
    c[:, 512:640] = np.where(idx[:, None] <= idx[None, :], 0.0, NEG)
    c[:, 640:768] = (idx[:, None] < idx[None, :])
    c[:, 768:800] = (np.arange(NE) * CAP + 1)[None, :]
    return c


def make_in_maps(x, mem, w_in, fox_f_bias, mlstm_conv_w, mlstm_i_bias, mlstm_f_bias, fox_norm_g, mlstm_norm_g,
                 w_mix_out, ln1_g, ln1_b, w_xq, w_xk, w_xv, w_xo, ln2_g, ln2_b, w_router, b_router, w_gate_up,
                 b_gate_up, w_down, b_down, ln3_g, ln3_b):
    f = lambda a: np.ascontiguousarray(np.asarray(a, dtype=np.float32))
    x = f(x)
    mem = f(mem)
    gb = np.concatenate([f(fox_f_bias)[0], f(mlstm_i_bias)[0], f(mlstm_f_bias)[0]])
    shared = {
        "consts": _consts(),
        "w_in": f(w_in)[0],
        "gbias": np.ascontiguousarray(np.tile(gb, 32)[None, :]),
        "convT": np.ascontiguousarray(f(mlstm_conv_w)[0].reshape(4, 4, 128).transpose(2, 1, 0).reshape(128, 16)),
        "foxg": f(fox_norm_g),
        "mlg": f(mlstm_norm_g),
        "w_mix": f(w_mix_out)[0],
        "lnp": np.ascontiguousarray(np.concatenate([f(ln1_g)[0], f(ln1_b)[0], f(ln2_g)[0], f(ln2_b)[0], f(ln3_g)[0], f(ln3_b)[0]])[None, :]),
        "w_xq": f(w_xq)[0], "w_xk": f(w_xk)[0], "w_xv": f(w_xv)[0], "w_xo": f(w_xo)[0],
        "w_r": f(w_router)[0], "b_r": f(b_router),
        "w_gu": f(w_gate_up)[0],
        "bguT": np.ascontiguousarray(f(b_gate_up)[0].reshape(NE, 16, 128).transpose(2, 0, 1).reshape(128, NE * 16)),
        "w_dn": f(w_down)[0], "b_dn": f(b_down)[0],
    }
    maps = []
    for c in range(8):
        b, h = c // 2, c % 2
        if h == 0:
            xc = np.concatenate([np.zeros((NTOK, D), np.float32), x[b, :NTOK]], axis=0)
            km = np.full((128, 1), NEG, np.float32)
        else:
            xc = x[b]
            km = np.zeros((128, 1), np.float32)
        m = dict(shared)
        m["xcat"] = np.ascontiguousarray(xc)
        m["memb"] = mem[b]
        m["kmask"] = km
        maps.append(m)
    return maps


def kernel(**inputs):
    if "nc" not in _CACHE:
        _CACHE["nc"] = build_program(False)
    nc = _CACHE["nc"]
    maps = make_in_maps(**inputs)
    res = run_bass_kernel_spmd(nc, maps, core_ids=list(range(8)))
    out = np.zeros((4, 4096, D), np.float32)
    for c in range(8):
        b, h = c // 2, c % 2
        out[b, h * NTOK:(h + 1) * NTOK] = res.results[c]["out"]
    return out
```
